# Optimizing a Trainium2 kernel written in Bass

```python
import jax
import jax.numpy as jnp
from jax import lax
import numpy as np

D_MODEL = 1024
BATCH = 8
SEQ = 2048
DEPTH = 4

GRID_W = 64
CTX_LEN = 256
HEAD_DIM = 64
NA_HEADS = 8
NA_WIN_ROWS = 8
NA_WIN_COLS = 16
GQA_Q_HEADS = 8
GQA_KV_HEADS = 2
Q_BLOCK = 128
ROPE_THETA = 10000.0
ROPE_AXIS_DIM = HEAD_DIM // 2
NA_DIM = NA_HEADS * HEAD_DIM
GQA_Q_DIM = GQA_Q_HEADS * HEAD_DIM
GQA_KV_DIM = GQA_KV_HEADS * HEAD_DIM
ATT_SPLITS = (NA_DIM, 2 * NA_DIM, 3 * NA_DIM, 3 * NA_DIM + GQA_Q_DIM, 3 * NA_DIM + GQA_Q_DIM + GQA_KV_DIM)
ATT_IN = 3 * NA_DIM + GQA_Q_DIM + 2 * GQA_KV_DIM
RWKV_HEADS = 8
RWKV_HEAD = 64
RWKV_DIM = RWKV_HEADS * RWKV_HEAD
DECAY_LORA = 32
ICLR_LORA = 32
GATE_LORA = 96
RWKV_GN_EPS = 64e-5
RWKV_IN = 4 * RWKV_DIM
MLSTM_HEADS = 4
MLSTM_HEAD = 128
MLSTM_DIM = MLSTM_HEADS * MLSTM_HEAD
MLSTM_CHUNK = 64
MLSTM_IN = 4 * MLSTM_DIM + 4 * MLSTM_HEADS
REC_IN = RWKV_IN + MLSTM_IN
D_MIX = NA_DIM + GQA_Q_DIM
N_EXPERTS = 64
TOP_K = 6
EXPERT_FF = 256
SHARED_FF = 256
ROUTED_SCALE = 2.5
MOE_BLOCK = 128
N_ATT_LAYERS = (DEPTH + 1) // 2
N_REC_LAYERS = DEPTH // 2
DN_ALPHA = (2 * DEPTH) ** 0.25
DN_BETA = (8 * DEPTH) ** -0.25
LN_EPS = 1e-5
NORM_EPS = 1e-6
F32 = jnp.float32

kernel_name = 'hybrid_na_gqa_rwkv7_mlstm_moe_dit'


def layer_norm(x, g, b):
    xf = x.astype(F32)
    mu = jnp.mean(xf, -1, keepdims=True)
    var = jnp.mean(jnp.square(xf - mu), -1, keepdims=True)
    return ((xf - mu) * lax.rsqrt(var + LN_EPS)).astype(x.dtype) * g + b


def rms_norm(x, g):
    xf = x.astype(F32)
    return (xf * lax.rsqrt(jnp.mean(jnp.square(xf), -1, keepdims=True) + NORM_EPS)).astype(x.dtype) * g


def swiglu(x, w1, w3, w2):
    return (jax.nn.silu(x @ w1) * (x @ w3)) @ w2


def centred_shift(x):
    xp = jnp.pad(x, ((0, 0), (1, 1), (0, 0)))
    return 0.5 * (xp[:, :-2] + xp[:, 2:])


def seg_flip(x, n_ctx, axis):
    a, b = jnp.split(x, [n_ctx], axis=axis)
    return jnp.concatenate([jnp.flip(a, axis), jnp.flip(b, axis)], axis=axis)


def both_dirs(x, n_ctx):
    return jnp.stack([x, seg_flip(x, n_ctx, 1)])


def own_dirs(x, n_ctx):
    return jnp.stack([x[0], seg_flip(x[1], n_ctx, 1)])


def axial_rope(n_tokens):
    t = jnp.arange(n_tokens)
    row = (t // GRID_W).astype(F32)
    col = (t % GRID_W).astype(F32)
    inv = ROPE_THETA ** (-jnp.arange(0, ROPE_AXIS_DIM, 2, dtype=F32) / ROPE_AXIS_DIM)
    ang = jnp.concatenate([row[:, None] * inv, col[:, None] * inv], -1)
    return jnp.cos(ang), jnp.sin(ang)


def apply_rope(x, cos, sin):
    xp = x.reshape(x.shape[:-1] + (HEAD_DIM // 2, 2))
    x0, x1 = xp[..., 0], xp[..., 1]
    c, s = cos[:, None, :], sin[:, None, :]
    return jnp.stack([x0 * c - x1 * s, x0 * s + x1 * c], -1).reshape(x.shape).astype(x.dtype)


def attend(q, k, v):
    B, Tq, Hq, Dh = q.shape
    hkv = k.shape[2]
    qg = q.reshape(B, Tq, hkv, Hq // hkv, Dh)
    s = jnp.einsum('bqhgd,bkhd->bhgqk', qg, k).astype(F32) * Dh ** -0.5
    p = jax.nn.softmax(s, -1).astype(v.dtype)
    return jnp.einsum('bhgqk,bkhd->bqhgd', p, v).reshape(B, Tq, Hq * Dh)


def blocked_attention(q, k, v):
    B, S, Hq, Dh = q.shape
    nb = S // Q_BLOCK
    qb = jnp.moveaxis(q.reshape(B, nb, Q_BLOCK, Hq, Dh), 1, 0)
    out = lax.map(lambda qi: attend(qi, k, v), qb)
    return jnp.moveaxis(out, 0, 1).reshape(B, S, Hq * Dh)


def neighbourhood_attention(q, k, v, k_ctx, v_ctx, rpb):
    B, S, H, Dh = q.shape
    rows = S // GRID_W
    kr, kc = min(NA_WIN_ROWS, rows), NA_WIN_COLS
    qg = q.reshape(B, rows, GRID_W, H, Dh)
    kg = k.reshape(B, rows, GRID_W, H, Dh)
    vg = v.reshape(B, rows, GRID_W, H, Dh)
    r = jnp.arange(rows)
    row_idx = jnp.clip(r - kr // 2, 0, rows - kr)[:, None] + jnp.arange(kr)[None, :]
    k_band = kg[:, row_idx]
    v_band = vg[:, row_idx]
    cidx = jnp.arange(GRID_W)
    col_start = jnp.clip(cidx - kc // 2, 0, GRID_W - kc)
    col_in = (cidx[None, :] >= col_start[:, None]) & (cidx[None, :] < col_start[:, None] + kc)
    d_row = row_idx - r[:, None] + NA_WIN_ROWS - 1
    d_col = jnp.clip(cidx[None, :] - cidx[:, None], -(kc - 1), kc - 1) + kc - 1
    bias = rpb[:, d_row[:, None, :, None], d_col[None, :, None, :]]
    scale = Dh ** -0.5
    s_lat = jnp.einsum('brqhd,brkwhd->bhrqkw', qg, k_band).astype(F32) * scale + bias
    s_lat = jnp.where(col_in[None, None, None, :, None, :], s_lat, -jnp.inf)
    s_ctx = jnp.einsum('brqhd,bchd->bhrqc', qg, k_ctx).astype(F32) * scale
    n_lat = kr * GRID_W
    s = jnp.concatenate([s_lat.reshape(B, H, rows, GRID_W, n_lat), s_ctx], -1)
    p = jax.nn.softmax(s, -1).astype(v.dtype)
    p_lat = p[..., :n_lat].reshape(B, H, rows, GRID_W, kr, GRID_W)
    out = jnp.einsum('bhrqkw,brkwhd->brqhd', p_lat, v_band) + jnp.einsum('bhrqc,bchd->brqhd', p[..., n_lat:], v_ctx)
    return out.reshape(B, S, H * Dh)


def attention_mixers(h_ctx, h_lat, w_in, rpb, qk_gain, keep_ctx):
    S = h_lat.shape[1]
    heads = lambda t, n: t.reshape(t.shape[:2] + (n, HEAD_DIM))

    def project(h):
        q_a, k_a, v_a, q_b, k_b, v_b = jnp.split(h @ w_in, ATT_SPLITS, axis=-1)
        return (heads(q_a, NA_HEADS), heads(k_a, NA_HEADS), heads(v_a, NA_HEADS),
                rms_norm(heads(q_b, GQA_Q_HEADS), qk_gain[0]), rms_norm(heads(k_b, GQA_KV_HEADS), qk_gain[1]),
                heads(v_b, GQA_KV_HEADS))

    qa_c, ka_c, va_c, qb_c, kb_c, vb_c = project(h_ctx)
    qa, ka, va, qb, kb, vb = project(h_lat)
    cos, sin = axial_rope(S)
    qb, kb = apply_rope(qb, cos, sin), apply_rope(kb, cos, sin)
    out_a = neighbourhood_attention(qa, ka, va, ka_c, va_c, rpb)
    out_b = blocked_attention(qb, jnp.concatenate([kb, kb_c], 1), jnp.concatenate([vb, vb_c], 1))
    out_lat = jnp.concatenate([out_a, out_b], -1)
    if not keep_ctx:
        return None, out_lat
    out_ctx = jnp.concatenate([attend(qa_c, ka_c, va_c), attend(qb_c, kb_c, vb_c)], -1)
    return out_ctx, out_lat


def rwkv7_step(state, inp):
    r, w, k, v, kk, a = inp
    sa = jnp.einsum('...vk,...k->...v', state, -kk)
    state = state * w[..., None, :] + sa[..., :, None] * (kk * a)[..., None, :] + v[..., :, None] * k[..., None, :]
    return state, jnp.einsum('...vk,...k->...v', state, r)


def rwkv7_bidirectional(p_ctx, p_lat, mu, w0, w1, w2, a0, a1, a2, g1, g2, kvec, r_k, gn, keep_ctx):
    n_ctx = p_ctx.shape[1]

    def shift_mix(p):
        d = centred_shift(p) - p
        (r, k, v, z), (dr, dk, dv, dz) = jnp.split(p, 4, -1), jnp.split(d, 4, -1)
        return r + dr * mu[0], k + dk * mu[1], v + dv * mu[2], z + dz * mu[3], z + dz * mu[4], z + dz * mu[5]

    r, k, v, z_w, z_a, z_g = [jnp.concatenate(pair, axis=1) for pair in zip(shift_mix(p_ctx), shift_mix(p_lat))]
    B, T, C = r.shape
    heads = lambda t: t.reshape(t.shape[:-1] + (RWKV_HEADS, RWKV_HEAD)).astype(F32)
    w_pre = w0[:, None, None] + jnp.einsum('dbtr,drc->dbtc', jnp.tanh(jnp.einsum('btc,dcr->dbtr', z_w, w1)), w2)
    decay = jnp.exp(-jnp.exp(-jax.nn.softplus(-w_pre.astype(F32)) - 0.5))
    iclr = jax.nn.sigmoid((a0[:, None, None] + jnp.einsum('dbtr,drc->dbtc', jnp.einsum('btc,dcr->dbtr', z_a, a1), a2)).astype(F32))
    gate = jax.nn.sigmoid(z_g @ g1) @ g2
    kk = heads(k * kvec[0])
    kk = kk * lax.rsqrt(jnp.maximum(jnp.sum(jnp.square(kk), -1, keepdims=True), 1e-24))
    k_eff = heads(k)[None] * (1.0 + (heads(iclr) - 1.0) * heads(kvec[1]))
    xs = (both_dirs(heads(r), n_ctx), own_dirs(heads(decay), n_ctx), own_dirs(k_eff, n_ctx),
          both_dirs(heads(v), n_ctx), both_dirs(kk, n_ctx), own_dirs(heads(iclr), n_ctx))
    xs = tuple(jnp.moveaxis(t, 2, 0) for t in xs)
    state0 = jnp.zeros((2, B, RWKV_HEADS, RWKV_HEAD, RWKV_HEAD), F32)
    _, y = lax.scan(rwkv7_step, state0, xs)
    y = own_dirs(jnp.moveaxis(y, 0, 2), n_ctx).sum(0)
    t0 = 0 if keep_ctx else n_ctx
    y, r_h, v_h, k_h, gate = y[:, t0:], heads(r)[:, t0:], heads(v)[:, t0:], k_eff[:, :, t0:], gate[:, t0:]
    mu_y = jnp.mean(y, -1, keepdims=True)
    var_y = jnp.mean(jnp.square(y - mu_y), -1, keepdims=True)
    y = (y - mu_y) * lax.rsqrt(var_y + RWKV_GN_EPS) * heads(gn[0]) + heads(gn[1])
    bonus = jnp.sum(r_h[None] * k_h * r_k, axis=-1, keepdims=True).sum(0) * v_h
    out = ((y + bonus).reshape(B, T - t0, C) * gate).astype(p_lat.dtype)
    if keep_ctx:
        return out[:, :n_ctx], out[:, n_ctx:]
    return None, out


def mlstm_chunk(carry, inp):
    c_state, n_state, m_state = carry
    q, k, v, ig, lf = inp
    L = q.shape[-2]
    in_order = jnp.tril(jnp.ones((L, L), bool))
    b = jnp.cumsum(lf, -1)
    d_intra = jnp.where(in_order, b[..., :, None] - b[..., None, :] + ig[..., None, :], -jnp.inf)
    d_inter = b + m_state[..., None]
    m_t = jnp.maximum(d_inter, jnp.max(d_intra, -1))
    s = jnp.einsum('...td,...sd->...ts', q, k) * jnp.exp(d_intra - m_t[..., None])
    w_inter = jnp.exp(d_inter - m_t)
    num = jnp.einsum('...ts,...sv->...tv', s, v) + w_inter[..., None] * jnp.einsum('...vd,...td->...tv', c_state, q)
    den = jnp.sum(s, -1) + w_inter * jnp.einsum('...d,...td->...t', n_state, q)
    h = num / jnp.maximum(jnp.abs(den), jnp.exp(-m_t))[..., None]
    b_end = b[..., -1]
    d_state = b_end[..., None] - b + ig
    m_new = jnp.maximum(b_end + m_state, jnp.max(d_state, -1))
    w_s = jnp.exp(d_state - m_new[..., None])
    w_c = jnp.exp(b_end + m_state - m_new)
    c_new = w_c[..., None, None] * c_state + jnp.einsum('...s,...sv,...sd->...vd', w_s, v, k)
    n_new = w_c[..., None] * n_state + jnp.einsum('...s,...sd->...d', w_s, k)
    return (c_new, n_new, m_new), h


def mlstm_bidirectional(p_ctx, p_lat, gate_b, norm_g, keep_ctx):
    n_ctx = p_ctx.shape[1]
    p = jnp.concatenate([p_ctx, p_lat], axis=1)
    B, T, _ = p.shape
    L, nc = MLSTM_CHUNK, T // MLSTM_CHUNK
    q, k, v, o, g = jnp.split(p, [MLSTM_DIM, 2 * MLSTM_DIM, 3 * MLSTM_DIM, 4 * MLSTM_DIM], axis=-1)
    heads = lambda t: t.reshape(B, T, MLSTM_HEADS, MLSTM_HEAD).astype(F32)
    g = g.reshape(B, T, 4, MLSTM_HEADS).astype(F32) + gate_b
    ig = jnp.stack([g[:, :, 0], seg_flip(g[:, :, 2], n_ctx, 1)])
    lf = jax.nn.log_sigmoid(jnp.stack([g[:, :, 1], seg_flip(g[:, :, 3], n_ctx, 1)]))

    def chunks(t):
        t = t.reshape((2, B, nc, L) + t.shape[3:])
        return jnp.moveaxis(jnp.moveaxis(t, 2, 0), 3, 4)

    xs = (chunks(both_dirs(heads(q) * MLSTM_HEAD ** -0.5, n_ctx)), chunks(both_dirs(heads(k), n_ctx)),
          chunks(both_dirs(heads(v), n_ctx)), chunks(ig), chunks(lf))
    carry0 = (jnp.zeros((2, B, MLSTM_HEADS, MLSTM_HEAD, MLSTM_HEAD), F32),
              jnp.zeros((2, B, MLSTM_HEADS, MLSTM_HEAD), F32), jnp.zeros((2, B, MLSTM_HEADS), F32))
    _, h = lax.scan(mlstm_chunk, carry0, xs)
    h = jnp.moveaxis(jnp.moveaxis(h, 4, 3), 0, 2).reshape(2, B, T, MLSTM_HEADS, MLSTM_HEAD)
    h = own_dirs(h, n_ctx).sum(0)
    t0 = 0 if keep_ctx else n_ctx
    h = rms_norm(h[:, t0:], norm_g.reshape(MLSTM_HEADS, MLSTM_HEAD))
    out = (h.reshape(B, T - t0, MLSTM_DIM) * jax.nn.sigmoid(o[:, t0:])).astype(p_lat.dtype)
    if keep_ctx:
        return out[:, :n_ctx], out[:, n_ctx:]
    return None, out


def recurrent_mixers(h_ctx, h_lat, w_in, mu, w0, w1, w2, a0, a1, a2, g1, g2, kvec, r_k, gn, gate_b, norm_g, keep_ctx):
    p_ctx, p_lat = h_ctx @ w_in, h_lat @ w_in
    rc, rl = rwkv7_bidirectional(p_ctx[..., :RWKV_IN], p_lat[..., :RWKV_IN], mu, w0, w1, w2, a0, a1, a2,
                                 g1, g2, kvec, r_k, gn, keep_ctx)
    mc, ml = mlstm_bidirectional(p_ctx[..., RWKV_IN:], p_lat[..., RWKV_IN:], gate_b, norm_g, keep_ctx)
    out_lat = jnp.concatenate([rl, ml], -1)
    if not keep_ctx:
        return None, out_lat
    return jnp.concatenate([rc, mc], -1), out_lat


def grouped_expert_ffn(xt, idx, gates, w1, w3, w2):
    n_tok, D = xt.shape
    n_assign = n_tok * TOP_K
    flat_e = idx.reshape(-1)
    flat_tok = jnp.repeat(jnp.arange(n_tok, dtype=jnp.int32), TOP_K)
    flat_g = gates.reshape(-1)
    order = jnp.argsort(flat_e)
    e_sorted = flat_e[order]
    counts = jnp.bincount(flat_e, length=N_EXPERTS)
    padded = (counts + MOE_BLOCK - 1) // MOE_BLOCK * MOE_BLOCK
    start = jnp.cumsum(counts) - counts
    p_end = jnp.cumsum(padded)
    dest = (p_end - padded)[e_sorted] + jnp.arange(n_assign) - start[e_sorted]
    n_blocks = -(-n_assign // MOE_BLOCK) + N_EXPERTS
    n_rows = n_blocks * MOE_BLOCK
    row_tok = jnp.full((n_rows,), n_tok, jnp.int32).at[dest].set(flat_tok[order])
    row_gate = jnp.zeros((n_rows,), gates.dtype).at[dest].set(flat_g[order])
    block_expert = jnp.minimum(jnp.searchsorted(p_end, jnp.arange(n_blocks) * MOE_BLOCK, side='right'), N_EXPERTS - 1)
    x_pad = jnp.concatenate([xt, jnp.zeros((1, D), xt.dtype)], 0)
    xb = x_pad[row_tok].reshape(n_blocks, MOE_BLOCK, D)
    yb = lax.map(lambda a: swiglu(a[0], w1[a[1]], w3[a[1]], w2[a[1]]), (xb, block_expert))
    y = yb.reshape(n_rows, D) * row_gate[:, None]
    return jax.ops.segment_sum(y, row_tok, num_segments=n_tok + 1)[:n_tok]


def moe_ffn(xt, router_w, router_b, w1, w3, w2, sw1, sw3, sw2):
    scores = jax.nn.sigmoid((xt @ router_w).astype(F32))
    _, idx = lax.top_k(scores + router_b.astype(F32), TOP_K)
    gates = jnp.take_along_axis(scores, idx, -1)
    gates = (gates / jnp.sum(gates, -1, keepdims=True) * ROUTED_SCALE).astype(xt.dtype)
    return grouped_expert_ffn(xt, idx, gates, w1, w3, w2) + swiglu(xt, sw1, sw3, sw2)


def setup_inputs(seed: int = 0) -> dict:
    key = jax.random.key(seed)
    ks = iter(jax.random.split(key, 40))
    nrm = lambda shape, scale: scale * jax.random.normal(next(ks), shape, F32)
    D = D_MODEL
    return {
        'x': nrm((BATCH, SEQ, D), 1.0),
        'c': nrm((BATCH, D), 1.0),
        'ctx': nrm((BATCH, CTX_LEN, D), 1.0),
        'c_ctx': nrm((D,), 1.0),
        'ada_w': nrm((DEPTH, D, 6 * D), 0.5 * D ** -0.5),
        'ada_b': nrm((DEPTH, 6 * D), 0.02),
        'ln_g': 1.0 + nrm((DEPTH, 2, D), 0.02),
        'ln_b': nrm((DEPTH, 2, D), 0.02),
        'mix_w_out': nrm((DEPTH, D_MIX, D), DN_BETA * D_MIX ** -0.5),
        'att_w_in': nrm((N_ATT_LAYERS, D, ATT_IN), D ** -0.5),
        'na_rpb': nrm((N_ATT_LAYERS, NA_HEADS, 2 * NA_WIN_ROWS - 1, 2 * NA_WIN_COLS - 1), 0.1),
        'qk_gain': 1.0 + nrm((N_ATT_LAYERS, 2, HEAD_DIM), 0.02),
        'rec_w_in': nrm((N_REC_LAYERS, D, REC_IN), D ** -0.5),
        'rwkv_mu': jax.random.uniform(next(ks), (N_REC_LAYERS, 6, RWKV_DIM), F32),
        'rwkv_w0': jax.random.uniform(next(ks), (N_REC_LAYERS, 2, RWKV_DIM), F32, minval=-5.5, maxval=0.5),
        'rwkv_w1': nrm((N_REC_LAYERS, 2, RWKV_DIM, DECAY_LORA), RWKV_DIM ** -0.5),
        'rwkv_w2': nrm((N_REC_LAYERS, 2, DECAY_LORA, RWKV_DIM), 0.5 * DECAY_LORA ** -0.5),
        'rwkv_a0': nrm((N_REC_LAYERS, 2, RWKV_DIM), 0.1),
        'rwkv_a1': nrm((N_REC_LAYERS, 2, RWKV_DIM, ICLR_LORA), RWKV_DIM ** -0.5),
        'rwkv_a2': nrm((N_REC_LAYERS, 2, ICLR_LORA, RWKV_DIM), ICLR_LORA ** -0.5),
        'rwkv_g1': nrm((N_REC_LAYERS, RWKV_DIM, GATE_LORA), RWKV_DIM ** -0.5),
        'rwkv_g2': nrm((N_REC_LAYERS, GATE_LORA, RWKV_DIM), GATE_LORA ** -0.5),
        'rwkv_kvec': jnp.array([0.85, 1.0], F32)[None, :, None] + nrm((N_REC_LAYERS, 2, RWKV_DIM), 0.05),
        'rwkv_rk': nrm((N_REC_LAYERS, RWKV_HEADS, RWKV_HEAD), 0.1),
        'rwkv_gn': jnp.array([1.0, 0.0], F32)[None, :, None] + nrm((N_REC_LAYERS, 2, RWKV_DIM), 0.02),
        'mlstm_gate_b': jnp.array([-2.0, 3.0, -2.0, 3.0], F32)[None, :, None] + nrm((N_REC_LAYERS, 4, MLSTM_HEADS), 0.3),
        'mlstm_norm': 1.0 + nrm((N_REC_LAYERS, MLSTM_DIM), 0.02),
        'moe_router': nrm((DEPTH, D, N_EXPERTS), D ** -0.5),
        'moe_bias': nrm((DEPTH, N_EXPERTS), 0.01),
        'moe_w1': nrm((DEPTH, N_EXPERTS, D, EXPERT_FF), D ** -0.5),
        'moe_w3': nrm((DEPTH, N_EXPERTS, D, EXPERT_FF), D ** -0.5),
        'moe_w2': nrm((DEPTH, N_EXPERTS, EXPERT_FF, D), DN_BETA * EXPERT_FF ** -0.5),
        'shared_w1': nrm((DEPTH, D, SHARED_FF), D ** -0.5),
        'shared_w3': nrm((DEPTH, D, SHARED_FF), D ** -0.5),
        'shared_w2': nrm((DEPTH, SHARED_FF, D), DN_BETA * SHARED_FF ** -0.5),
    }


def reference(x, c, ctx, c_ctx, ada_w, ada_b, ln_g, ln_b, mix_w_out, att_w_in, na_rpb, qk_gain,
              rec_w_in, rwkv_mu, rwkv_w0, rwkv_w1, rwkv_w2, rwkv_a0, rwkv_a1, rwkv_a2, rwkv_g1, rwkv_g2,
              rwkv_kvec, rwkv_rk, rwkv_gn, mlstm_gate_b, mlstm_norm, moe_router, moe_bias, moe_w1, moe_w3,
              moe_w2, shared_w1, shared_w3, shared_w2):
    B, S, D = x.shape
    xc = ctx
    for i in range(DEPTH):
        keep_ctx = i < DEPTH - 1
        j = i // 2
        mod = jax.nn.silu(c) @ ada_w[i] + ada_b[i]
        mod_c = jax.nn.silu(c_ctx) @ ada_w[i] + ada_b[i]
        sh1, sc1, g1, sh2, sc2, g2 = [t[:, None] for t in jnp.split(mod, 6, -1)]
        sh1c, sc1c, g1c, sh2c, sc2c, g2c = jnp.split(mod_c, 6, -1)
        h_lat = x * (1.0 + sc1) + sh1
        h_ctx = xc * (1.0 + sc1c) + sh1c
        if i % 2 == 0:
            m_ctx, m_lat = attention_mixers(h_ctx, h_lat, att_w_in[j], na_rpb[j], qk_gain[j], keep_ctx)
        else:
            m_ctx, m_lat = recurrent_mixers(h_ctx, h_lat, rec_w_in[j], rwkv_mu[j], rwkv_w0[j], rwkv_w1[j], rwkv_w2[j],
                                            rwkv_a0[j], rwkv_a1[j], rwkv_a2[j], rwkv_g1[j], rwkv_g2[j], rwkv_kvec[j],
                                            rwkv_rk[j], rwkv_gn[j], mlstm_gate_b[j], mlstm_norm[j], keep_ctx)
        x = layer_norm(DN_ALPHA * x + g1 * (m_lat @ mix_w_out[i]), ln_g[i, 0], ln_b[i, 0])
        h_lat = x * (1.0 + sc2) + sh2
        if keep_ctx:
            xc = layer_norm(DN_ALPHA * xc + g1c * (m_ctx @ mix_w_out[i]), ln_g[i, 0], ln_b[i, 0])
            h_ctx = xc * (1.0 + sc2c) + sh2c
            tokens = jnp.concatenate([h_lat.reshape(-1, D), h_ctx.reshape(-1, D)], 0)
        else:
            tokens = h_lat.reshape(-1, D)
        y = moe_ffn(tokens, moe_router[i], moe_bias[i], moe_w1[i], moe_w3[i], moe_w2[i],
                    shared_w1[i], shared_w3[i], shared_w2[i])
        x = layer_norm(DN_ALPHA * x + g2 * y[:B * S].reshape(B, S, D), ln_g[i, 1], ln_b[i, 1])
        if keep_ctx:
            xc = layer_norm(DN_ALPHA * xc + g2c * y[B * S:].reshape(xc.shape), ln_g[i, 1], ln_b[i, 1])
    return x
```

```python
import contextlib
import math
import numpy as np
import ml_dtypes
import concourse.bass as bass
import concourse.mybir as mybir
from concourse.bass_utils import run_bass_kernel_spmd

F32 = mybir.dt.float32
BF16 = mybir.dt.bfloat16
ALU = mybir.AluOpType
AF = mybir.ActivationFunctionType
AX = mybir.AxisListType

D = 1024
S = 2048
NCTX = 256
T = S + NCTX
NT = T // 128
DEPTH = 4
GRID_W = 64
HD = 64
ATT_IN = 2304
REC_IN = 4112
NEXP = 64
TOPK = 6
ROUTED_SCALE = 2.5
DN_ALPHA = (2 * DEPTH) ** 0.25
LN_EPS = 1e-5
NORM_EPS = 1e-6
RWKV_GN_EPS = 64e-5
NEG = -30000.0

CONST_NAMES = ("ident", "ropec", "ropes", "ropepm", "blk64", "ones", "m_le", "m_lt", "m_le128", "m_gt128")
NDMA_Q = {"sp": 16, "pool": 8}
SEM_EPOCH = 30000


class Op:
    __slots__ = ("eng", "fn", "dma", "deps", "needs_inc", "sem", "count", "slot", "emitted")

    def __init__(self, eng, fn, dma):
        self.eng = eng
        self.fn = fn
        self.dma = dma
        self.deps = []
        self.needs_inc = False
        self.sem = None
        self.count = 0
        self.slot = None
        self.emitted = False


class Prog:
    ENGS = ("pe", "dve", "act", "pool", "sp")

    def __init__(self, nc):
        self.nc = nc
        self.stack = contextlib.ExitStack()
        self.pending = {e: [] for e in self.ENGS}
        self.res = {}
        self.dma_last = {q: [None] * n for q, n in NDMA_Q.items()}
        self.dma_rr = {q: 0 for q in NDMA_Q}
        self.eng_obj = {"pe": nc.tensor, "dve": nc.vector, "act": nc.scalar,
                        "pool": nc.gpsimd, "sp": nc.sync}
        self.dma_sems = {q: [self.stack.enter_context(nc.semaphore(f"dq_{q}{i}")) for i in range(n)] for q, n in NDMA_Q.items()}
        self.eng_sem = {e: None for e in self.ENGS}
        self.eng_cnt = {e: 0 for e in self.ENGS}
        self.eng_nep = {e: 0 for e in self.ENGS}
        self.eng_last = {e: None for e in self.ENGS}
        self.waited = {e: {} for e in self.ENGS}
        self.out_dmas = []
        self.n_ops = 0

    def sbuf(self, name, shape, dtype, stack=None):
        self.n_alloc = getattr(self, "n_alloc", 0) + 1
        return (stack or self.stack).enter_context(self.nc.sbuf_tensor(f"{name}_{self.n_alloc}", list(shape), dtype))

    def psum(self, name, shape, dtype=F32):
        return self.stack.enter_context(self.nc.psum_tensor(name, list(shape), dtype))

    def add(self, eng, fn, reads=(), writes=(), dma=False):
        op = Op(eng, fn, dma)
        deps = {}
        for k in reads:
            r = self.res.get(k)
            if r is not None and r[0] is not None:
                deps[id(r[0])] = r[0]
            if r is not None and isinstance(k, tuple) and k and k[0] == "pb":
                for o in r[1]:
                    if o.eng != eng:
                        deps[id(o)] = o
        for k in writes:
            r = self.res.get(k)
            if r is not None:
                if r[0] is not None:
                    deps[id(r[0])] = r[0]
                for o in r[1]:
                    deps[id(o)] = o
        for k in reads:
            r = self.res.get(k)
            if r is None:
                r = [None, []]
                self.res[k] = r
            r[1].append(op)
        for k in writes:
            self.res[k] = [op, []]
        if dma:
            slot = self.dma_rr[eng]
            self.dma_rr[eng] = (slot + 1) % NDMA_Q[eng]
            prev = self.dma_last[eng][slot]
            op.slot = slot
            op.sem = self.dma_sems[eng][slot]
            op.count = (prev.count if prev is not None else 0) + 16
            if prev is not None:
                deps[id(prev)] = prev
            self.dma_last[eng][slot] = op
        deps.pop(id(op), None)
        for d in deps.values():
            if d.eng == "pe" and eng == "pe" and not d.dma and not dma:
                continue
            op.deps.append(d)
            d.needs_inc = True
        self.pending[eng].append(op)
        self.n_ops += 1
        return op

    def _wait(self, e, sem, count):
        w = self.waited[e]
        key = id(sem)
        if w.get(key, 0) >= count:
            return
        self.eng_obj[e].wait_ge(sem, count)
        w[key] = count

    def flush(self, barrier=True):
        nc = self.nc
        if barrier:
            for e in self.ENGS:
                for op in reversed(self.pending[e]):
                    if not op.dma:
                        op.needs_inc = True
                        break
        for e in self.ENGS:
            for op in self.pending[e]:
                if op.dma or not op.needs_inc:
                    continue
                if self.eng_sem[e] is None or self.eng_cnt[e] >= SEM_EPOCH:
                    self.eng_sem[e] = self.stack.enter_context(nc.semaphore(f"c_{e}_{self.eng_nep[e]}"))
                    self.eng_nep[e] += 1
                    self.eng_cnt[e] = 0
                self.eng_cnt[e] += 1
                op.sem = self.eng_sem[e]
                op.count = self.eng_cnt[e]
                self.eng_last[e] = op
        for e in self.ENGS:
            eng = self.eng_obj[e]
            for op in self.pending[e]:
                for d in op.deps:
                    self._wait(e, d.sem, d.count)
                ins = op.fn(eng)
                if op.dma:
                    ins.then_inc(op.sem, 16)
                elif op.needs_inc:
                    ins.then_inc(op.sem, 1)
                op.emitted = True
                op.fn = None
            self.pending[e] = []
        if barrier:
            for e in self.ENGS:
                for e2 in self.ENGS:
                    o = self.eng_last[e2]
                    if o is not None and not (e2 == e and e == "pe"):
                        self._wait(e, o.sem, o.count)
                for q in self.dma_last:
                    for o in self.dma_last[q]:
                        if o is not None:
                            self._wait(e, o.sem, o.count)
            self.res = {}

    @contextlib.contextmanager
    def phase(self):
        st = contextlib.ExitStack()
        try:
            yield st
            self.flush(barrier=True)
        finally:
            st.close()

    def finish(self):
        self.flush(barrier=True)
        self.stack.close()


def _bf(a):
    return np.ascontiguousarray(a.astype(ml_dtypes.bfloat16))


def host_constants():
    c = {}
    c["ident"] = np.eye(128, dtype=np.float32)
    t = np.arange(S)
    row = (t // GRID_W).astype(np.float64)
    col = (t % GRID_W).astype(np.float64)
    inv = 10000.0 ** (-np.arange(0, 32, 2, dtype=np.float64) / 32.0)
    ang = np.concatenate([row[:, None] * inv, col[:, None] * inv], -1)
    cosT = np.cos(ang).T
    sinT = np.sin(ang).T
    cos64 = np.repeat(cosT, 2, axis=0)
    sin64 = np.repeat(sinT, 2, axis=0)
    c["ropec"] = np.concatenate([cos64, cos64], 0).astype(np.float32)
    c["ropes"] = np.concatenate([sin64, sin64], 0).astype(np.float32)
    pm = np.zeros((128, 128), np.float32)
    for i in range(64):
        pm[2 * i + 1, 2 * i] = -1.0
        pm[2 * i, 2 * i + 1] = 1.0
    c["ropepm"] = pm
    bo = np.zeros((128, 128), np.float32)
    bo[:64, :64] = 1.0
    bo[64:, 64:] = 1.0
    c["blk64"] = bo
    c["ones"] = np.ones((128, 128), np.float32)
    s_ = np.arange(128)[:, None]
    t_ = np.arange(128)[None, :]
    same = (s_ // 64) == (t_ // 64)
    c["m_le"] = (same & (s_ <= t_)).astype(np.float32)
    c["m_lt"] = (same & (s_ < t_)).astype(np.float32)
    c["m_ge"] = (same & (s_ >= t_)).astype(np.float32)
    c["m_gt"] = (same & (s_ > t_)).astype(np.float32)
    c["m_le128"] = (s_ <= t_).astype(np.float32)
    c["m_ge128"] = (s_ >= t_).astype(np.float32)
    c["m_lt128"] = (s_ < t_).astype(np.float32)
    c["m_gt128"] = (s_ > t_).astype(np.float32)
    return c


def na_bias_tables(rpb):
    H = 8
    ext = np.concatenate([rpb.reshape(H, 15 * 31), np.full((H, 1), NEG, np.float32)], 1)
    cq = np.arange(64)[:, None]
    ck = np.arange(64)[None, :]
    cs = np.clip(cq - 8, 0, 48)
    col_in = (ck >= cs) & (ck < cs + 16)
    dcol = np.clip(ck - cq, -15, 15) + 15
    idxE = np.zeros((15, 64, 64), np.int64)
    for dr in range(15):
        idxE[dr] = np.where(col_in, dr * 31 + dcol, 465)
    tabE = np.zeros((128, 4, 15, 64), np.float32)
    tabO = np.zeros((128, 4, 10, 64), np.float32)
    mrow = np.full((64, 64), 465, np.int64)
    for h in range(H):
        pb = (h % 2) * 64
        hp = h // 2
        for dr in range(15):
            tabE[pb:pb + 64, hp, dr, :] = ext[h][idxE[dr]]
        tabO[pb:pb + 64, hp, 0, :] = ext[h][mrow]
        for q in range(8):
            tabO[pb:pb + 64, hp, 1 + q, :] = ext[h][idxE[3 + q]]
        tabO[pb:pb + 64, hp, 9, :] = ext[h][mrow]
    return tabE.reshape(128, 4 * 15 * 64), tabO.reshape(128, 4 * 10 * 64)


class K:
    def __init__(self, nc):
        self.nc = nc
        self.P = Prog(nc)
        self.bank_rr = 0

    def dma(self, eng, out, in_, reads, writes, **kw):
        return self.P.add(eng, lambda e: e.dma_start(out=out, in_=in_, **kw), reads, writes, dma=True)

    def mm(self, out, lhsT, rhs, start, stop, reads, writes):
        return self.P.add("pe", lambda e: e.matmul(out, lhsT=lhsT, rhs=rhs, start=start, stop=stop), reads, writes)

    def tr(self, out, in_, ident, reads, writes):
        return self.P.add("pe", lambda e: e.transpose(out, in_, ident), reads, writes)

    def act(self, out, in_, func, reads, writes, eng="act", **kw):
        return self.P.add("act", lambda e: e.activation(out=out, in_=in_, func=func, **kw), reads, writes)

    def tt(self, eng, out, in0, in1, op, reads, writes):
        return self.P.add(eng, lambda e: e.tensor_tensor(out=out, in0=in0, in1=in1, op=op), reads, writes)

    def ts(self, eng, out, in0, s1, s2, op0, op1, reads, writes, **kw):
        return self.P.add(eng, lambda e: e.tensor_scalar(out=out, in0=in0, scalar1=s1, scalar2=s2, op0=op0, op1=op1, **kw), reads, writes)

    def stt(self, eng, out, in0, scalar, in1, op0, op1, reads, writes):
        return self.P.add(eng, lambda e: e.scalar_tensor_tensor(out=out, in0=in0, scalar=scalar, in1=in1, op0=op0, op1=op1), reads, writes)

    def cp(self, eng, out, in_, reads, writes):
        if eng == "act":
            return self.P.add("act", lambda e: e.activation(out=out, in_=in_, func=AF.Copy), reads, writes)
        return self.P.add(eng, lambda e: e.tensor_copy(out=out, in_=in_), reads, writes)

    def bank(self, b):
        return self.pd[b // 2][:, (b % 2) * 512:(b % 2 + 1) * 512]

    def bk(self, b):
        return ("pb", b)


def bc_rows(ap1d, nparts):
    n = ap1d.shape[-1]
    return bass.AP(ap1d.tensor, ap1d.offset, [[0, nparts], [1, n]])


def build_program(n_layers=DEPTH, stop=None, dbg=False, start=0):
    nc = bass.Bass("TRN2", target_bir_lowering=False)
    NL = n_layers
    NLa = (n_layers + 1) // 2
    NLr = max(n_layers // 2, 1)
    k = K(nc)
    P = k.P

    def din(name, shape, dt=F32):
        return nc.dram_tensor(name, list(shape), dt, kind="ExternalInput").ap()

    I = {}
    I["x"] = din("x", [S, D]); I["ctx"] = din("ctx", [NCTX, D]); I["c2"] = din("c2", [2, D])
    I["ada_w"] = din("ada_w", [NL, D, 6 * D]); I["ada_b"] = din("ada_b", [NL, 6 * D])
    I["ln_g"] = din("ln_g", [NL, 2, D]); I["ln_b"] = din("ln_b", [NL, 2, D])
    I["mix_w_out"] = din("mix_w_out", [NL, D, D])
    I["att_w_in"] = din("att_w_in", [2, D, ATT_IN])
    I["na_tabE"] = din("na_tabE", [2, 128, 3840]); I["na_tabO"] = din("na_tabO", [2, 128, 2560])
    I["qk_gain"] = din("qk_gain", [2, 2, HD])
    I["rec_w_in"] = din("rec_w_in", [2, D, REC_IN])
    I["rwkv_mu"] = din("rwkv_mu", [2, 6, 512]); I["rwkv_w0"] = din("rwkv_w0", [2, 2, 512])
    I["rwkv_w1"] = din("rwkv_w1", [2, 2, 512, 32]); I["rwkv_w2"] = din("rwkv_w2", [2, 2, 32, 512])
    I["rwkv_a0"] = din("rwkv_a0", [2, 2, 512]); I["rwkv_a1"] = din("rwkv_a1", [2, 2, 512, 32])
    I["rwkv_a2"] = din("rwkv_a2", [2, 2, 32, 512]); I["rwkv_g1"] = din("rwkv_g1", [2, 512, 96])
    I["rwkv_g2"] = din("rwkv_g2", [2, 96, 512]); I["rwkv_kvec"] = din("rwkv_kvec", [2, 2, 512])
    I["rwkv_rk"] = din("rwkv_rk", [2, 512]); I["rwkv_gn"] = din("rwkv_gn", [2, 2, 512])
    I["mlstm_gate_b"] = din("mlstm_gate_b", [2, 16]); I["mlstm_norm"] = din("mlstm_norm", [2, 512])
    I["moe_router"] = din("moe_router", [NL, D, NEXP]); I["moe_bias"] = din("moe_bias", [NL, NEXP])
    I["moe_w1"] = din("moe_w1", [NL, NEXP, D, 256]); I["moe_w3"] = din("moe_w3", [NL, NEXP, D, 256])
    I["moe_w2"] = din("moe_w2", [NL, NEXP, 256, D])
    I["shared_w1"] = din("shared_w1", [NL, D, 256]); I["shared_w3"] = din("shared_w3", [NL, D, 256])
    I["shared_w2"] = din("shared_w2", [NL, 256, D])
    for cn in CONST_NAMES:
        shp = [128, 2048] if cn in ("ropec", "ropes") else [128, 128]
        I[cn] = din("c_" + cn, shp)
    out = nc.dram_tensor("out", [S, D], F32, kind="ExternalOutput").ap()
    k.Xd = nc.dram_tensor("Xd", [T, D], F32, kind="Internal").ap()
    k.modrow = nc.dram_tensor("modrow", [DEPTH, 2, 6 * D], F32, kind="Internal").ap()
    k.rkv_d = nc.dram_tensor("rkv_d", [3, 512, T], F32, kind="Internal").ap()
    k.dbg = nc.dram_tensor("dbg", [128, 8, T], BF16, kind="ExternalOutput").ap() if dbg else None
    k.I = I
    k.out = out

    k.identf = P.sbuf("identf", [128, 128], F32)
    k.identb = P.sbuf("identb", [128, 128], BF16)
    k.onesb = P.sbuf("onesb", [128, 128], BF16)
    k.epsln = P.sbuf("epsln", [128, 1], F32)
    k.epsnm = P.sbuf("epsnm", [128, 1], F32)
    k.modT = P.sbuf("modT", [128, DEPTH, 48, 2], F32)
    k.AB = P.sbuf("AB", [128, 8, T], BF16)
    k.Gtm = P.sbuf("Gtm", [128, NT, NEXP], F32)
    k.pd = [P.psum(f"pd{i}", [128, 1024], F32) for i in range(4)]

    k.dma("sp", k.identf[:], I["ident"], [], ["identf"])
    k.cp("dve", k.identb[:], k.identf[:], ["identf"], ["identb"])
    P.add("pool", lambda e: e.memset(k.onesb[:], 1.0), [], ["onesb"])
    P.add("pool", lambda e: e.memset(k.epsln[:], LN_EPS), [], ["epsln"])
    P.add("pool", lambda e: e.memset(k.epsnm[:], NORM_EPS), [], ["epsnm"])

    k.n_layers = n_layers
    k.start = start
    prologue(k, n_layers)
    if stop == ("pro", 0):
        o = k.dma("sp", out[0:12, :], k.modrow[0].rearrange("j (a b) -> (j a) b", b=1024), [], ["out"])
        P.out_dmas.append(o)
        n_layers = 0
    for i in range(start, n_layers):
        last = (i == DEPTH - 1)
        if i == start:
            phase1(k, i, ntile=(stop[1] if (stop and stop[0] in ("p1", "p1x") and stop[1] > 0) else NT))
            if stop and stop[0] == "p1x":
                phase1(k, 0, ntile=1) if False else None
                o = k.dma("pool", out[0:128, :].rearrange("p (k n) -> p k n", k=8), k.AB[:, :, 0:128], [], ["out"])
                P.out_dmas.append(o)
                break
            if dbg and stop[0] == "p1":
                k.dma("sp", k.dbg, k.AB[:], [], ["dbg"])
                break
        if i % 2 == 0:
            attention_layer(k, i)
        else:
            recurrent_layer(k, i)
        if dbg and stop == ("m", i):
            k.dma("sp", k.dbg, k.AB[:], [("AB", t) for t in range(NT)], ["dbg"])
            break
        post(k, i, 0, last)
        if stop == ("xa", i):
            break
        with P.phase() as st:
            k.Yacc = P.sbuf("Yacc", [128, NT, D], F32, st)
            moe(k, i, last, st)
            post(k, i, 1, last)
        if stop == ("xb", i):
            break
    if stop is not None and stop[0] in ("p1", "m"):
        o = k.dma("sp", out[0:12, :], k.modrow[0].rearrange("j (a b) -> (j a) b", b=1024), [], ["out"])
        P.out_dmas.append(o)
    if not (n_layers == DEPTH and stop is None) and (stop is None or stop[0] in ("xa", "xb")):
        o = k.dma("sp", out, k.Xd[0:S, :], [("Xd", t) for t in range(16)], ["out"])
        P.out_dmas.append(o)
    P.flush(barrier=True)
    for o in P.out_dmas:
        nc.sync.wait_ge(o.sem, o.count)
    P.stack.close()
    return nc


def prologue(k, n_layers):
    P, I = k.P, k.I
    with P.phase() as st:
        c2T = P.sbuf("c2T", [128, 8, 2], F32, st)
        sT = P.sbuf("sT", [128, 8, 2], F32, st)
        wch = [P.sbuf(f"adaw{j}", [128, 8, 512], F32, st) for j in range(4)]
        b2 = [P.sbuf(f"adab{j}", [2, 512], F32, st) for j in range(4)]
        mrow = [P.sbuf(f"mrow{j}", [2, 512], F32, st) for j in range(4)]
        for jj in range(2):
            k.dma("sp", c2T[:, :, jj], I["c2"][jj, :].rearrange("(k p) -> p k", p=128), [], [("c2T", jj)], allow_slow_non_contiguous=True)
        k.act(sT[:], c2T[:], AF.Silu, [("c2T", 0), ("c2T", 1)], ["sT"])
        n = 0
        for i in range(n_layers):
            for cc in range(12):
                j = n % 4
                n += 1
                cols = slice(cc * 512, (cc + 1) * 512)
                k.dma("sp" if n % 2 else "pool", wch[j][:], I["ada_w"][i, :, cols].rearrange("(k p) n -> p k n", p=128), [], [("wch", j)])
                k.dma("sp", b2[j][:], bc_rows(I["ada_b"][i, cols], 2), [], [("b2", j)])
                b0, b1 = 2 * j, 2 * j + 1
                for kk in range(8):
                    k.mm(k.bank(b0)[0:2, :], sT[:, kk, :], wch[j][:, kk, :], kk == 0, kk == 7,
                         ["sT", ("wch", j)], [k.bk(b0)])
                k.tt("dve", mrow[j][:], k.bank(b0)[0:2, :], b2[j][:], ALU.add, [k.bk(b0), ("b2", j)], [("mrow", j)])
                k.dma("sp", k.modrow[i, :, cols], mrow[j][:], [("mrow", j)], [("modrow", i, cc)])
                for q in range(4):
                    k.mm(k.bank(b1)[:, q * 2:(q + 1) * 2], mrow[j][0:2, q * 128:(q + 1) * 128], k.identf[0:2, 0:2],
                         True, True, [("mrow", j), "identf"], [k.bk(b1)])
                k.cp("act", k.modT[:, i, cc * 4:(cc + 1) * 4, :],
                     k.bank(b1)[:, 0:8].rearrange("p (q j) -> p q j", j=2), [k.bk(b1)], [("modT", i, cc)])
        for i in range(n_layers):
            for lo in (8, 32):
                cs = [("modT", i, cc) for cc in range(lo // 4, lo // 4 + 2)]
                k.ts("dve", k.modT[:, i, lo:lo + 8, :], k.modT[:, i, lo:lo + 8, :], 1.0, None, ALU.add, ALU.bypass, cs, cs)


def x_src(k, i, t, first):
    if first:
        return k.I["x"][t * 128:(t + 1) * 128, :] if t < 16 else k.I["ctx"][(t - 16) * 128:(t - 15) * 128, :]
    return k.Xd[t * 128:(t + 1) * 128, :]


def emit_hT(k, xt, xkey, i, scb, shb, t, hf=None, hfkey=None):
    j = 0 if t < 16 else 1
    for kq in range(2):
        b = 4 + (k.bank_rr % 4)
        k.bank_rr += 1
        for q in range(4):
            kk = kq * 4 + q
            k.tr(k.bank(b)[:, q * 128:(q + 1) * 128], xt[:, kk * 128:(kk + 1) * 128], k.identf[:], [xkey, "identf"], [k.bk(b)])
        for q in range(4):
            kk = kq * 4 + q
            sc = k.modT[:, i, scb + kk, j:j + 1]
            sh = k.modT[:, i, shb + kk, j:j + 1]
            src = k.bank(b)[:, q * 128:(q + 1) * 128]
            if hf is not None:
                dst = hf[:, kk, :]
                wk = [(hfkey, kk)]
            else:
                dst = k.AB[:, kk, t * 128:(t + 1) * 128]
                wk = [("AB", t, kk)]
            if b % 2 == 0:
                k.ts("dve", dst, src, sc, sh, ALU.mult, ALU.add, [k.bk(b)], wk)
            else:
                k.act(dst, src, AF.Identity, [k.bk(b)], wk, scale=sc, bias=sh)


def ABk(t):
    return [("AB", t, kk) for kk in range(8)]


def phase1(k, i, ntile=NT):
    P = k.P
    with P.phase() as st:
        xts = [P.sbuf(f"p1x{j}", [128, D], F32, st) for j in range(3)]
        for t in range(ntile):
            j = t % 3
            k.dma("sp", xts[j][:], x_src(k, i, t, True), [("Xd", t)], [("p1x", j)])
            emit_hT(k, xts[j], ("p1x", j), i, 8, 0, t)


def post(k, i, which, last):
    P, I = k.P, k.I
    ntile = 16 if last else NT
    first = (i == k.start and which == 0)
    with P.phase() as st:
        gb = P.sbuf("gb", [128, 2, D], F32, st)
        lng = P.sbuf("lng", [128, D], F32, st)
        lnb = P.sbuf("lnb", [128, D], F32, st)
        goff = 2 * D if which == 0 else 5 * D
        for j in range(2):
            k.dma("sp", gb[:, j, :], bc_rows(k.modrow[i, j, goff:goff + D], 128), [], [("gb", j)])
        k.dma("sp", lng[:], bc_rows(I["ln_g"][i, which, :], 128), [], ["lng"])
        k.dma("sp", lnb[:], bc_rows(I["ln_b"][i, which, :], 128), [], ["lnb"])
        if which == 0:
            Wo = P.sbuf("Wo", [128, 8, D], BF16, st)
            for h in range(2):
                k.dma("pool", Wo[:, h * 4:(h + 1) * 4], I["mix_w_out"][i, h * 512:(h + 1) * 512, :].rearrange("(k p) n -> p k n", p=128), [], [("Wo", h)])
            rw = P.sbuf("rw", [128, 8, NEXP], F32, st)
            k.dma("sp", rw[:], I["moe_router"][i].rearrange("(k p) e -> p k e", p=128), [], ["rw"])
            rb = P.sbuf("rb", [128, NEXP], F32, st)
            k.dma("sp", rb[:], bc_rows(I["moe_bias"][i], 128), [], ["rb"])
            hf = [P.sbuf(f"hf{r}", [128, 8, 128], F32, st) for r in range(2)]
            rt = {n: [P.sbuf(f"rt_{n}{r}", [128, w], F32, st) for r in range(2)]
                  for n, w in (("scs", 64), ("sel", 64), ("top8", 8), ("msk", 64), ("gs", 64), ("den", 1), ("rden", 1), ("G", 64))}
        xt = [P.sbuf(f"xt{r}", [128, D], F32, st) for r in range(2)]
        t1 = [P.sbuf(f"t1{r}", [128, D], F32, st) for r in range(2)]
        z = [P.sbuf(f"z{r}", [128, D], F32, st) for r in range(2)]
        xn = [P.sbuf(f"xn{r}", [128, D], F32, st) for r in range(2)]
        xo = [P.sbuf(f"xo{r}", [128, D], F32, st) for r in range(2)]
        bst = [P.sbuf(f"bst{r}", [128, 12], F32, st) for r in range(2)]
        mv = [P.sbuf(f"mv{r}", [128, 2], F32, st) for r in range(2)]
        sd = [P.sbuf(f"sd{r}", [128, 1], F32, st) for r in range(2)]
        rstd = [P.sbuf(f"rstd{r}", [128, 1], F32, st) for r in range(2)]
        k.bank_rr = 4

        def stage_a(t):
            j = 0 if t < 16 else 1
            r = t % 2
            tl = slice(t * 128, (t + 1) * 128)
            k.dma("sp", xt[r][:], x_src(k, i, t, first), [("Xd", t)], [("xt", r)])
            if which == 0:
                Yb = k.pd[r]
                yk = [k.bk(2 * r), k.bk(2 * r + 1)]
                for half in range(2):
                    for kk in range(8):
                        k.mm(Yb[:, half * 512:(half + 1) * 512], k.AB[:, kk, tl], Wo[:, kk, half * 512:(half + 1) * 512],
                             kk == 0, kk == 7, ABk(t) + [("Wo", kk // 4)], [yk[half]])
                k.tt("dve", t1[r][:], Yb[:], gb[:, j, :], ALU.mult, yk + [("gb", j)], [("t1", r)])
            else:
                k.tt("dve", t1[r][:], k.Yacc[:, t, :], gb[:, j, :], ALU.mult, [("Yacc", t), ("gb", j)], [("t1", r)])
            k.stt("dve", z[r][:], xt[r][:], float(DN_ALPHA), t1[r][:], ALU.mult, ALU.add, [("xt", r), ("t1", r)], [("z", r)])
            for h in range(2):
                P.add("dve", lambda e, r=r, h=h: e.bn_stats(out=bst[r][:, h * 6:(h + 1) * 6], in_=z[r][:, h * 512:(h + 1) * 512]),
                      [("z", r)], [("bst", r, h)])
            P.add("dve", lambda e, r=r: e.bn_aggr(out=mv[r][:], in_=bst[r][:]), [("bst", r, 0), ("bst", r, 1)], [("mv", r)])
            k.act(sd[r][:], mv[r][:, 1:2], AF.Sqrt, [("mv", r), "epsln"], [("sd", r)], bias=k.epsln[:, 0:1], scale=1.0)
            P.add("dve", lambda e, r=r: e.reciprocal(out=rstd[r][:], in_=sd[r][:]), [("sd", r)], [("rstd", r)])
            k.ts("dve", xn[r][:], z[r][:], mv[r][:, 0:1], rstd[r][:, 0:1], ALU.subtract, ALU.mult,
                 [("z", r), ("mv", r), ("rstd", r)], [("xn", r)])
            k.tt("pool", xn[r][:], xn[r][:], lng[:], ALU.mult, [("xn", r), "lng"], [("xn", r)])
            k.tt("pool", xo[r][:], xn[r][:], lnb[:], ALU.add, [("xn", r), "lnb"], [("xo", r)])

        def stage_b(t):
            j = 0 if t < 16 else 1
            r = t % 2
            tl = slice(t * 128, (t + 1) * 128)
            if last and which == 1:
                o = k.dma("sp", k.out[tl, :], xo[r][:], [("xo", r)], [("out", t)])
                P.out_dmas.append(o)
                return
            k.dma("sp", k.Xd[tl, :], xo[r][:], [("xo", r)], [("Xd", t)])
            if which == 1:
                if i + 1 < k.n_layers:
                    emit_hT(k, xo[r], ("xo", r), i + 1, 8, 0, t)
                return
            emit_hT(k, xo[r], ("xo", r), i, 32, 24, t, hf=hf[r], hfkey=("hf", r))
            hk = [(("hf", r), kk) for kk in range(8)]
            k.cp("pool", k.AB[:, :, tl], hf[r][:], hk, ABk(t))
            b = 4 + (k.bank_rr % 4)
            k.bank_rr += 1
            for kk in range(8):
                k.mm(k.bank(b)[:, 0:NEXP], hf[r][:, kk, :], rw[:, kk, :], kk == 0, kk == 7, hk + ["rw"], [k.bk(b)])
            R = {n: rt[n][r] for n in rt}
            K_ = lambda n: ("rt", n, r)
            k.act(R["scs"][:], k.bank(b)[:, 0:NEXP], AF.Sigmoid, [k.bk(b)], [K_("scs")])
            k.tt("dve", R["sel"][:], R["scs"][:], rb[:], ALU.add, [K_("scs"), "rb"], [K_("sel")])
            P.add("dve", lambda e, R=R: e.max(out=R["top8"][:], in_=R["sel"][:]), [K_("sel")], [K_("top8")])
            k.ts("dve", R["msk"][:], R["sel"][:], R["top8"][:, TOPK - 1:TOPK], None, ALU.is_ge, ALU.bypass, [K_("sel"), K_("top8")], [K_("msk")])
            k.tt("dve", R["gs"][:], R["scs"][:], R["msk"][:], ALU.mult, [K_("scs"), K_("msk")], [K_("gs")])
            P.add("dve", lambda e, R=R: e.reduce_sum(out=R["den"][:], in_=R["gs"][:], axis=AX.X), [K_("gs")], [K_("den")])
            P.add("dve", lambda e, R=R: e.reciprocal(out=R["rden"][:], in_=R["den"][:]), [K_("den")], [K_("rden")])
            k.ts("dve", R["G"][:], R["gs"][:], R["rden"][:, 0:1], float(ROUTED_SCALE), ALU.mult, ALU.mult, [K_("gs"), K_("rden")], [K_("G")])
            k.cp("pool", k.Gtm[:, t, :], R["G"][:], [K_("G")], [("Gtm", t)])

        stage_a(0)
        for t in range(ntile):
            if t + 1 < ntile:
                stage_a(t + 1)
            stage_b(t)


def moe(k, i, last, st_unused=None):
    P, I = k.P, k.I
    ntile = 16 if last else NT
    nchunk = ntile // 2
    G = 2
    groups = [list(range(g * G, (g + 1) * G)) for g in range(NEXP // G)] + [["s"]]
    with P.phase() as st:
        w1b = [P.sbuf(f"w1b{j}", [128, G, 8, 256], BF16, st) for j in range(2)]
        w3b = [P.sbuf(f"w3b{j}", [128, G, 8, 256], BF16, st) for j in range(2)]
        w2b = [P.sbuf(f"w2b{j}", [128, G, 2, D], BF16, st) for j in range(2)]
        s1 = [P.sbuf(f"ms1{j}", [128, 2, 256], BF16, st) for j in range(2)]
        u = [P.sbuf(f"mu{j}", [128, 2, 256], BF16, st) for j in range(2)]

        def load(gi):
            j = gi % 2
            for ei, e in enumerate(groups[gi]):
                s1_ = I["shared_w1"][i] if e == "s" else I["moe_w1"][i, e]
                s3_ = I["shared_w3"][i] if e == "s" else I["moe_w3"][i, e]
                s2_ = I["shared_w2"][i] if e == "s" else I["moe_w2"][i, e]
                k.dma("pool", w1b[j][:, ei], s1_.rearrange("(k p) f -> p k f", p=128), [], [("w1b", j, ei)])
                k.dma("pool", w3b[j][:, ei], s3_.rearrange("(k p) f -> p k f", p=128), [], [("w3b", j, ei)])
                k.dma("pool", w2b[j][:, ei], s2_.rearrange("(f p) n -> p f n", p=128), [], [("w2b", j, ei)])

        units = [(gi, ci, ei, e) for gi, grp in enumerate(groups) for ci in range(nchunk) for ei, e in enumerate(grp)]

        def up(n):
            gi, ci, ei, e = units[n]
            j, x, c0 = gi % 2, n % 2, ci * 256
            for (wb, bnk, nm) in ((w1b, 4 + x, "w1b"), (w3b, 6 + x, "w3b")):
                for f in range(2):
                    for kk in range(8):
                        k.mm(k.bank(bnk)[:, f * 256:(f + 1) * 256], wb[j][:, ei, kk, f * 128:(f + 1) * 128],
                             k.AB[:, kk, c0:c0 + 256], kk == 0, kk == 7, [(nm, j, ei)], [k.bk(bnk)])

        def rest(n):
            gi, ci, ei, e = units[n]
            j, x = gi % 2, n % 2
            k.act(s1[x][:], k.bank(4 + x)[:, 0:512].rearrange("p (f n) -> p f n", f=2), AF.Silu, [k.bk(4 + x)], [("ms1", x)])
            k.tt("dve", u[x][:], k.bank(6 + x)[:, 0:512].rearrange("p (f n) -> p f n", f=2), s1[x][:], ALU.mult,
                 [k.bk(6 + x), ("ms1", x)], [("mu", x)])
            for tt in range(2):
                t = ci * 2 + tt
                for half in range(2):
                    for f in range(2):
                        k.mm(k.pd[tt][:, half * 512:(half + 1) * 512], u[x][:, f, tt * 128:(tt + 1) * 128],
                             w2b[j][:, ei, f, half * 512:(half + 1) * 512], f == 0, f == 1, [("mu", x), ("w2b", j, ei)], [k.bk(2 * tt + half)])
                yk = [k.bk(2 * tt), k.bk(2 * tt + 1)]
                gsc = 1.0 if e == "s" else k.Gtm[:, t, e:e + 1]
                if gi == 0 and ei == 0:
                    k.ts("dve", k.Yacc[:, t, :], k.pd[tt][:], gsc, None, ALU.mult, ALU.bypass, yk, [("Yacc", t)])
                else:
                    k.stt("dve", k.Yacc[:, t, :], k.pd[tt][:], gsc, k.Yacc[:, t, :], ALU.mult, ALU.add, yk + [("Yacc", t)], [("Yacc", t)])

        load(0)
        up(0)
        for n in range(len(units)):
            gi, ci, ei, e = units[n]
            if ci == 0 and ei == 0 and gi + 1 < len(groups):
                load(gi + 1)
            if n + 1 < len(units):
                up(n + 1)
            rest(n)


def attention_layer(k, i):
    P, I = k.P, k.I
    j = i // 2
    keep_ctx = i < DEPTH - 1
    Win = I["att_w_in"][j]
    with P.phase() as sa:
        QTa = P.sbuf("QTa", [128, 4, T], BF16, sa)
        KTa = P.sbuf("KTa", [128, 4, T], BF16, sa)
        Va = P.sbuf("Va", [128, NT, 512], BF16, sa)
        QTb = P.sbuf("QTb", [128, 4, T], BF16, sa)
        KTb = P.sbuf("KTb", [128, 2, T], BF16, sa)
        Vb = P.sbuf("Vb", [128, NT, 128], BF16, sa)
        with P.phase() as st:
            W = P.sbuf("Win", [128, 8, ATT_IN], BF16, st)
            for kk in range(8):
                k.dma("pool", W[:, kk, :], Win[kk * 128:(kk + 1) * 128, :], [], [("W", kk)])
            ropec = P.sbuf("ropec", [128, S], F32, st)
            ropes = P.sbuf("ropes", [128, S], F32, st)
            pm = P.sbuf("ropepm", [128, 128], F32, st)
            blk = P.sbuf("blk64", [128, 128], F32, st)
            gq = P.sbuf("gq", [128, 1], F32, st)
            gk = P.sbuf("gk", [128, 1], F32, st)
            k.dma("sp", ropec[:], I["ropec"], [], ["ropec"])
            k.dma("sp", ropes[:], I["ropes"], [], ["ropes"])
            k.dma("sp", pm[:], I["ropepm"], [], ["pm"])
            k.dma("sp", blk[:], I["blk64"], [], ["blk"])
            for h in range(2):
                k.dma("sp", gq[h * 64:(h + 1) * 64, :], I["qk_gain"][j, 0, :].rearrange("(d o) -> d o", o=1), [], [("gq", h)], allow_slow_non_contiguous=True)
                k.dma("sp", gk[h * 64:(h + 1) * 64, :], I["qk_gain"][j, 1, :].rearrange("(d o) -> d o", o=1), [], [("gk", h)], allow_slow_non_contiguous=True)
            k.ts("dve", gq[:], gq[:], 0.125, None, ALU.mult, ALU.bypass, [("gq", 0), ("gq", 1)], [("gq", 0), ("gq", 1)])
            gqk = [("gq", 0), ("gq", 1)]
            gkk = [("gk", 0), ("gk", 1)]
            tmp = {n: [P.sbuf(f"at_{n}{r}", [128, 512], F32, st) for r in range(2)] for n in ("sq", "sd", "xg", "xn", "t1")}
            tmp["rstd"] = tmp["sd"]
            tmp["t2"] = tmp["sq"]
            Wk = [("W", kk) for kk in range(8)]
            nrot = 0
            brr = 0
            tcs = [(0, 512), (512, 512), (1024, 512), (1536, 512), (2048, 256)]
            for (c0, cn) in tcs:
                for cc in range(14):
                    b = brr % 8
                    brr += 1
                    if cc >= 12:
                        for hh in range(2):
                            for kk in range(8):
                                lhs = W[:, kk, 2048 + (cc - 12) * 64:2048 + (cc - 11) * 64]
                                k.mm(k.bank(b)[hh * 64:(hh + 1) * 64, 0:cn], lhs, k.AB[:, kk, c0:c0 + cn], kk == 0, kk == 7, [("W", kk)], [k.bk(b)])
                    for kk in range(8):
                        if cc < 4:
                            lhs = W[:, kk, cc * 128:(cc + 1) * 128]
                        elif cc < 8:
                            lhs = W[:, kk, 512 + (cc - 4) * 128:512 + (cc - 3) * 128]
                        elif cc < 12:
                            lhs = W[:, kk, 1536 + (cc - 8) * 128:1536 + (cc - 7) * 128]
                        else:
                            break
                        k.mm(k.bank(b)[:, 0:cn], lhs, k.AB[:, kk, c0:c0 + cn], kk == 0, kk == 7, [("W", kk)], [k.bk(b)])
                    src = k.bank(b)[:, 0:cn]
                    if cc < 4:
                        k.act(QTa[:, cc, c0:c0 + cn], src, AF.Copy, [k.bk(b)], [("QTa", cc, c0)], scale=0.125)
                        continue
                    if cc < 8:
                        k.cp("dve", KTa[:, cc - 4, c0:c0 + cn], src, [k.bk(b)], [("KTa", cc, c0)])
                        continue
                    is_q = cc < 12
                    dest = QTb[:, cc - 8, c0:c0 + cn] if is_q else KTb[:, cc - 12, c0:c0 + cn]
                    dk = [("QKb", cc, c0)]
                    gain, gkeys = (gq, gqk) if is_q else (gk, gkk)
                    r = nrot % 2
                    nrot += 1
                    tk = lambda n: ("at", {"rstd": "sd", "t2": "sq"}.get(n, n), r)
                    tv = lambda n: tmp[n][r][:, 0:cn]
                    k.act(tv("sq"), src, AF.Square, [k.bk(b)], [tk("sq")])
                    b2 = brr % 8
                    brr += 1
                    k.mm(k.bank(b2)[:, 0:cn], blk[:], tv("sq"), True, True, [tk("sq"), "blk"], [k.bk(b2)])
                    k.act(tv("sd"), k.bank(b2)[:, 0:cn], AF.Sqrt, [k.bk(b2), "epsnm"], [tk("sd")], scale=1.0 / 64, bias=k.epsnm[:, 0:1])
                    P.add("dve", lambda e, o=tv("rstd"), a=tv("sd"): e.reciprocal(out=o, in_=a), [tk("sd")], [tk("sd")])
                    k.act(tv("xg"), src, AF.Identity, [k.bk(b)] + gkeys, [tk("xg")], scale=gain[:, 0:1])
                    if c0 < S:
                        k.tt("pool", tv("xn"), tv("xg"), tv("rstd"), ALU.mult, [tk("xg"), tk("rstd")], [tk("xn")])
                        b3 = brr % 8
                        brr += 1
                        k.mm(k.bank(b3)[:, 0:cn], pm[:], tv("xn"), True, True, [tk("xn"), "pm"], [k.bk(b3)])
                        k.tt("pool", tv("t1"), tv("xn"), ropec[:, c0:c0 + cn], ALU.mult, [tk("xn"), "ropec"], [tk("t1")])
                        k.tt("dve", tv("t2"), k.bank(b3)[:, 0:cn], ropes[:, c0:c0 + cn], ALU.mult, [k.bk(b3), "ropes"], [tk("t2")])
                        k.tt("pool", dest, tv("t1"), tv("t2"), ALU.add, [tk("t1"), tk("t2")], dk)
                    else:
                        k.tt("pool", dest, tv("xg"), tv("rstd"), ALU.mult, [tk("xg"), tk("rstd")], dk)
            for t in range(NT):
                tl = slice(t * 128, (t + 1) * 128)
                b = brr % 8
                brr += 1
                for kk in range(8):
                    k.mm(k.bank(b)[:, 0:512], k.AB[:, kk, tl], W[:, kk, 1024:1536], kk == 0, kk == 7, [("W", kk)], [k.bk(b)])
                k.cp("act", Va[:, t, :], k.bank(b)[:, 0:512], [k.bk(b)], [("Va", t)])
                b = brr % 8
                brr += 1
                for kk in range(8):
                    k.mm(k.bank(b)[:, 0:128], k.AB[:, kk, tl], W[:, kk, 2176:2304], kk == 0, kk == 7, [("W", kk)], [k.bk(b)])
                k.cp("dve", Vb[:, t, :], k.bank(b)[:, 0:128], [k.bk(b)], [("Vb", t)])
        with P.phase() as st:
            tabE = P.sbuf("tabE", [128, 3840], F32, st)
            tabO = P.sbuf("tabO", [128, 2560], F32, st)
            k.dma("sp", tabE[:], I["na_tabE"][j], [], ["tabE"])
            k.dma("sp", tabO[:], I["na_tabO"][j], [], ["tabO"])
            Sb = [P.sbuf(f"naS{r}", [128, 896], F32, st) for r in range(2)]
            Pe = [P.sbuf(f"naP{r}", [128, 896], BF16, st) for r in range(2)]
            Pn = [P.sbuf(f"naN{r}", [128, 896], BF16, st) for r in range(2)]
            PT = [P.sbuf(f"naT{r}", [128, 896], BF16, st) for r in range(2)]
            sm = {n: [P.sbuf(f"na_{n}{r}", [128, 1], F32, st) for r in range(2)] for n in ("mx", "nmx", "rs", "ri")}
            units = []
            for r_ in range(32):
                sr = min(max(r_ - 4, 0), 24)
                if sr % 2 == 0:
                    a0, nrow, odd, d0 = sr, 8, False, sr - r_ + 7
                else:
                    a0, nrow, odd, d0 = sr - 1, 10, True, 0
                units.append((r_ * 64, a0 * 64, nrow * 64, odd, d0))
            if keep_ctx:
                for cq in range(4):
                    units.append((S + cq * 64, 0, 0, False, 0))
            ulist = [(q0, k0, nlat, odd, d0, hp) for (q0, k0, nlat, odd, d0) in units for hp in range(4)]

            def na_s(n):
                q0, k0, nlat, odd, d0, hp = ulist[n]
                nk = nlat + NCTX
                r = n % 2
                kS = ("naS", r)
                for hh in range(2):
                    ps = slice(hh * 64, (hh + 1) * 64)
                    bA, bB = 2 * hh, 2 * hh + 1
                    q = QTa[ps, hp, q0:q0 + 64]
                    if nlat:
                        k.mm(k.bank(bA)[ps, 0:512], q, KTa[ps, hp, k0:k0 + 512], True, True, [], [k.bk(bA)])
                        if nlat > 512:
                            k.mm(k.bank(bB)[ps, 0:128], q, KTa[ps, hp, k0 + 512:k0 + 640], True, True, [], [k.bk(bB)])
                        xo_ = nlat - 512
                        k.mm(k.bank(bB)[ps, xo_:xo_ + NCTX], q, KTa[ps, hp, S:T], True, True, [], [k.bk(bB)])
                        if odd:
                            bias = tabO[ps, hp * 640:(hp + 1) * 640]
                        else:
                            bias = tabE[ps, (hp * 15 + d0) * 64:(hp * 15 + d0 + 8) * 64]
                        k.tt("dve", Sb[r][ps, 0:512], k.bank(bA)[ps, 0:512], bias[:, 0:512], ALU.add,
                             [k.bk(bA), "tabE", "tabO"], [(kS, hh, 0)])
                        if nlat > 512:
                            k.tt("dve", Sb[r][ps, 512:640], k.bank(bB)[ps, 0:128], bias[:, 512:640], ALU.add,
                                 [k.bk(bB), "tabO"], [(kS, hh, 1)])
                        k.cp("dve", Sb[r][ps, nlat:nk], k.bank(bB)[ps, xo_:xo_ + NCTX], [k.bk(bB)], [(kS, hh, 2)])
                    else:
                        k.mm(k.bank(bA)[ps, 0:NCTX], q, KTa[ps, hp, S:T], True, True, [], [k.bk(bA)])
                        k.cp("act", Sb[r][ps, 0:NCTX], k.bank(bA)[ps, 0:NCTX], [k.bk(bA)], [(kS, hh, 2)])

            def na_rest(n):
                q0, k0, nlat, odd, d0, hp = ulist[n]
                nk = nlat + NCTX
                r = n % 2
                kS, kP, kN, kT = ("naS", r), ("naP", r), ("naN", r), ("naT", r)
                sk = [(kS, hh, x) for hh in range(2) for x in range(3)]
                M = {nm: sm[nm][r] for nm in sm}
                mk = lambda nm: ("nasm", nm, r)
                P.add("dve", lambda e, o=M["mx"], a=Sb[r], nk=nk: e.reduce_max(out=o[:], in_=a[:, 0:nk], axis=AX.X), sk, [mk("mx")])
                k.ts("dve", M["nmx"][:], M["mx"][:], -1.0, None, ALU.mult, ALU.bypass, [mk("mx")], [mk("nmx")])
                P.add("act", lambda e, o=Pe[r], a=Sb[r], nk=nk, nm=M["nmx"], rs=M["rs"]: e.activation(
                    out=o[:, 0:nk], in_=a[:, 0:nk], func=AF.Exp, bias=nm[:, 0:1], scale=1.0, accum_out=rs[:, 0:1]),
                    sk + [mk("nmx")], [kP, mk("rs")])
                P.add("dve", lambda e, o=M["ri"], a=M["rs"]: e.reciprocal(out=o[:], in_=a[:]), [mk("rs")], [mk("ri")])
                k.ts("dve", Pn[r][:, 0:nk], Pe[r][:, 0:nk], M["ri"][:, 0:1], None, ALU.mult, ALU.bypass, [kP, mk("ri")], [kN])
                nch = nk // 128
                bT = 4 + (n % 2)
                ptb = k.bank(bT).bitcast(BF16)
                for c in range(nch):
                    k.tr(ptb[:, c * 128:(c + 1) * 128], Pn[r][:, c * 128:(c + 1) * 128], k.identb[:], [kN], [k.bk(bT)])
                k.cp("act", PT[r][:, 0:nk], ptb[:, 0:nk], [k.bk(bT)], [kT])
                bO = 6 + (n % 2)
                for hh in range(2):
                    ps = slice(hh * 64, (hh + 1) * 64)
                    h = 2 * hp + hh
                    for c in range(nch):
                        if c < nlat // 128:
                            vt = k0 // 128 + c
                        else:
                            vt = 16 + (c - nlat // 128)
                        k.mm(k.bank(bO)[ps, 0:64], Va[:, vt, h * 64:(h + 1) * 64], PT[r][:, c * 128 + hh * 64:c * 128 + hh * 64 + 64],
                             c == 0, c == nch - 1, [kT], [k.bk(bO)])
                k.cp("act", k.AB[:, hp, q0:q0 + 64], k.bank(bO)[:, 0:64], [k.bk(bO)], [("mT", hp, q0)])

            na_s(0)
            for n in range(len(ulist)):
                if n + 1 < len(ulist):
                    na_s(n + 1)
                na_rest(n)
        with P.phase() as st:
            E = [[P.sbuf(f"gqE{r}{hh}", [128, 512], BF16, st) for hh in range(2)] for r in range(2)]
            rc = P.sbuf("gqrc", [128, 512], F32, st)
            gunits = [(hp, c * 512, 512, list(range(NT))) for hp in range(4) for c in range(4)]
            if keep_ctx:
                gunits += [(hp, S, NCTX, [16, 17]) for hp in range(4)]
            n = 0
            for (hp, q0, nq, kts) in gunits:
                g = hp // 2
                def gq_s(ki, r):
                    kt = kts[ki]
                    for hh in range(2):
                        ps = slice(hh * 64, (hh + 1) * 64)
                        bS = 2 * r + hh
                        k.mm(k.bank(bS)[:, 0:nq], KTb[ps, g, kt * 128:(kt + 1) * 128], QTb[ps, hp, q0:q0 + nq], True, True, [], [k.bk(bS)])
                        k.act(E[r][hh][:, 0:nq], k.bank(bS)[:, 0:nq], AF.Exp, [k.bk(bS)], [("gqE", r, hh)])

                def gq_pv(ki, r):
                    kt = kts[ki]
                    for hh in range(2):
                        ps = slice(hh * 64, (hh + 1) * 64)
                        k.mm(k.bank(4 + hh)[ps, 0:nq], Vb[:, kt, g * 64:(g + 1) * 64], E[r][hh][:, 0:nq], ki == 0, ki == len(kts) - 1,
                             [("gqE", r, hh)], [k.bk(4 + hh)])
                        k.mm(k.bank(6 + hh)[ps, 0:nq], k.onesb[:, 0:64], E[r][hh][:, 0:nq], ki == 0, ki == len(kts) - 1,
                             [("gqE", r, hh), "onesb"], [k.bk(6 + hh)])

                gq_s(0, n % 2)
                for ki in range(len(kts)):
                    r = n % 2
                    n += 1
                    if ki + 1 < len(kts):
                        gq_s(ki + 1, n % 2)
                    gq_pv(ki, r)
                for hh in range(2):
                    ps = slice(hh * 64, (hh + 1) * 64)
                    P.add("dve", lambda e, o=rc[ps, 0:nq], a=k.bank(6 + hh)[ps, 0:nq]: e.reciprocal(out=o, in_=a), [k.bk(6 + hh)], [("gqrc", hh)])
                    k.tt("dve", k.AB[ps, 4 + hp, q0:q0 + nq], k.bank(4 + hh)[ps, 0:nq], rc[ps, 0:nq], ALU.mult,
                         [k.bk(4 + hh), ("gqrc", hh)], [("mT", 4 + hp, q0, hh)])


_CACHE = {}


def make_in_maps(inputs):
    f = lambda a: np.ascontiguousarray(np.asarray(a, dtype=np.float32))
    shared = {}
    for n in ("ada_w", "ada_b", "ln_g", "ln_b", "mix_w_out", "att_w_in", "qk_gain", "rec_w_in", "rwkv_mu", "rwkv_w0",
              "rwkv_w1", "rwkv_w2", "rwkv_a0", "rwkv_a1", "rwkv_a2", "rwkv_g1", "rwkv_g2", "rwkv_kvec", "rwkv_gn",
              "mlstm_norm", "moe_router", "moe_bias", "moe_w1", "moe_w3", "moe_w2", "shared_w1", "shared_w3", "shared_w2"):
        shared[n] = f(inputs[n])
    shared["rwkv_rk"] = f(inputs["rwkv_rk"]).reshape(2, 512)
    shared["mlstm_gate_b"] = f(inputs["mlstm_gate_b"]).reshape(2, 16)
    rpb = f(inputs["na_rpb"])
    tabs = [na_bias_tables(rpb[j]) for j in range(rpb.shape[0])]
    shared["na_tabE"] = np.ascontiguousarray(np.stack([t[0] for t in tabs]))
    shared["na_tabO"] = np.ascontiguousarray(np.stack([t[1] for t in tabs]))
    hc = host_constants()
    for cn in CONST_NAMES:
        shared["c_" + cn] = np.ascontiguousarray(hc[cn])
    x = f(inputs["x"]); c = f(inputs["c"]); ctx = f(inputs["ctx"]); c_ctx = f(inputs["c_ctx"])
    maps = []
    for b in range(x.shape[0]):
        m = dict(shared)
        m["x"] = np.ascontiguousarray(x[b])
        m["ctx"] = np.ascontiguousarray(ctx[b])
        m["c2"] = np.ascontiguousarray(np.stack([c[b], c_ctx]))
        maps.append(m)
    return maps


def kernel(**inputs):
    maps = make_in_maps(inputs)
    if "nc" not in _CACHE:
        _CACHE["nc"] = build_program()
    nc = _CACHE["nc"]
    res = run_bass_kernel_spmd(nc, maps, core_ids=list(range(len(maps))))
    return np.stack([np.asarray(r["out"], dtype=np.float32) for r in res.results])


def tokview(t2d, row_elems, base, start, step, n, parts=128, p0=0):
    return bass.AP(t2d, p0 * row_elems + base + start, [[row_elems, parts], [step, n]])


def proc_tiles(d):
    if d == 0:
        return [(t * 128, 1) for t in (16, 17)] + [(t * 128, 1) for t in range(16)]
    return [(T - 1 - u * 128, -1) for u in range(NT)]


def recurrent_layer(k, i):
    P, I = k.P, k.I
    with P.phase() as sl:
        mTt = P.sbuf("mTt", [128, 8, T], BF16, sl) if False else None
        mTm = P.sbuf("mTm", [128, 4, T], BF16, sl)
        cm = {}
        for cn in ("m_le", "m_lt", "m_le128", "m_gt128", "ones", "blk64"):
            cm[cn] = P.sbuf("c" + cn, [128, 128], F32, sl)
            k.dma("sp", cm[cn][:], I[cn], [], [("cm", cn)])
        P.flush(barrier=True)
        mlstm_mixer(k, i, mTm, cm)
        rwkv_mixer(k, i, cm)
        with P.phase():
            for c in range(4):
                k.cp("pool" if c % 2 else "dve", k.AB[:, 4 + c, :], mTm[:, c, :], [], [("AB", 4 + c)])


def mlstm_mixer(k, i, mTt, cm):
    P, I = k.P, k.I
    j = i // 2
    Wr = I["rec_w_in"][j]
    ABt = k.AB
    RE = 8 * T
    with P.phase() as sm:
        Wg = P.sbuf("Wg", [128, 8, 16], BF16, sm)
        k.dma("pool", Wg[:], Wr[:, 4096:4112].rearrange("(k p) n -> p k n", p=128), [], ["Wg"])
        gb = P.sbuf("gateb", [128, 16], F32, sm)
        k.dma("sp", gb[:], bc_rows(I["mlstm_gate_b"][j, :], 128), [], ["gateb"])
        ng = P.sbuf("normg", [128, 4], F32, sm)
        k.dma("sp", ng[:], I["mlstm_norm"][j, :].rearrange("(h p) -> p h", p=128), [], ["normg"], allow_slow_non_contiguous=True)
        IG = [P.sbuf(f"IG{d}", [128, NT, 4], F32, sm) for d in range(2)]
        LF = [P.sbuf(f"LF{d}", [128, NT, 4], F32, sm) for d in range(2)]
        WI = [P.sbuf(f"WI{d}", [128, NT, 4], F32, sm) for d in range(2)]
        WS = [P.sbuf(f"WS{d}", [128, NT, 4], F32, sm) for d in range(2)]
        WC = [P.sbuf(f"WC{d}", [128, NT, 4], F32, sm) for d in range(2)]
        gt = [P.sbuf(f"gtmp{r}", [128, 8], F32, sm) for r in range(2)]
        hTr = P.sbuf("hTr", [128, 8, T], BF16, sm)
        for kk in range(8):
            k.cp("pool" if kk % 2 else "dve", hTr[:, kk, :], tokview(ABt, RE, kk * T, T - 1, -1, T), [], [("hTr", kk)])
        hkeys = [("hTr", kk) for kk in range(8)]

        def hview(d, u, st0, kk):
            return k.AB[:, kk, st0:st0 + 128] if d == 0 else hTr[:, kk, u * 128:(u + 1) * 128]
        n = 0
        for d in range(2):
            for u, (st0, step) in enumerate(proc_tiles(d)):
                r = n % 2
                b = n % 8
                n += 1
                for kk in range(8):
                    k.mm(k.bank(b)[:, 0:16], hview(d, u, st0, kk), Wg[:, kk, :], kk == 0, kk == 7, ["Wg"] + hkeys, [k.bk(b)])
                c0 = 8 * d
                k.tt("dve", IG[d][:, u, :], k.bank(b)[:, c0:c0 + 4], gb[:, c0:c0 + 4], ALU.add, [k.bk(b), "gateb"], [("IG", d, u)])
                k.tt("dve", gt[r][:, 0:4], k.bank(b)[:, c0 + 4:c0 + 8], gb[:, c0 + 4:c0 + 8], ALU.add, [k.bk(b), "gateb"], [("gt", r)])
                k.act(gt[r][:, 4:8], gt[r][:, 0:4], AF.Exp, [("gt", r)], [("gt2", r)], scale=-1.0)
                k.act(gt[r][:, 0:4], gt[r][:, 4:8], AF.Ln, [("gt2", r), ("gt", r)], [("gt", r)], bias=1.0, scale=1.0)
                k.ts("dve", LF[d][:, u, :], gt[r][:, 0:4], -1.0, None, ALU.mult, ALU.bypass, [("gt", r)], [("LF", d, u)])
                b2 = n % 8
                n += 1
                k.mm(k.bank(b2)[:, 0:4], cm["m_le128"][:], LF[d][:, u, :], True, True, [("LF", d, u), ("cm", "m_le128")], [k.bk(b2)])
                k.mm(k.bank(b2)[:, 4:8], cm["ones"][:], LF[d][:, u, :], True, True, [("LF", d, u), ("cm", "ones")], [k.bk(b2)])
                k.act(WI[d][:, u, :], k.bank(b2)[:, 0:4], AF.Exp, [k.bk(b2)], [("WI", d, u)])
                k.act(WC[d][:, u, :], k.bank(b2)[:, 4:8], AF.Exp, [k.bk(b2)], [("WC", d, u)])
                k.act(gt[r][:, 4:8], k.bank(b2)[:, 4:8], AF.Copy, [k.bk(b2), ("gt2", r)], [("gt2", r)])
                k.act(gt[r][:, 0:4], k.bank(b2)[:, 0:4], AF.Copy, [k.bk(b2), ("gt", r)], [("gt", r)])
                k.tt("dve", gt[r][:, 4:8], gt[r][:, 4:8], gt[r][:, 0:4], ALU.subtract, [("gt", r), ("gt2", r)], [("gt2", r)])
                k.tt("dve", gt[r][:, 4:8], gt[r][:, 4:8], IG[d][:, u, :], ALU.add, [("gt2", r), ("IG", d, u)], [("gt2", r)])
                k.act(WS[d][:, u, :], gt[r][:, 4:8], AF.Exp, [("gt2", r)], [("WS", d, u)])
        P.flush(barrier=True)
        for h in range(4):
            with P.phase() as st:
                Wq = P.sbuf("mWq", [128, 8, 128], BF16, st)
                Wk = P.sbuf("mWk", [128, 8, 128], BF16, st)
                Wv = P.sbuf("mWv", [128, 8, 128], BF16, st)
                Wo = P.sbuf("mWo", [128, 8, 128], BF16, st)
                for (w, off, nm) in ((Wq, 2048, "q"), (Wk, 2560, "k"), (Wv, 3072, "v"), (Wo, 3584, "o")):
                    k.dma("pool", w[:], Wr[:, off + h * 128:off + (h + 1) * 128].rearrange("(k p) n -> p k n", p=128), [], [("mW", nm)])
                QT = P.sbuf("mQT", [128, T], BF16, st)
                KT = P.sbuf("mKT", [128, T], BF16, st)
                SO = P.sbuf("mSO", [128, T], BF16, st)
                HS = P.sbuf("mHS", [128, T], F32, st)
                Kt = [P.sbuf(f"mKt{d}", [128, NT, 128], BF16, st) for d in range(2)]
                Vt = [P.sbuf(f"mVt{d}", [128, NT, 129], BF16, st) for d in range(2)]
                Cs = [P.sbuf(f"mC{d}", [128, 129], F32, st) for d in range(2)]
                Cbs = [P.sbuf(f"mCb{d}", [128, 129], BF16, st) for d in range(2)]
                tm = {nm: [P.sbuf(f"mt_{nm}{r}", [128, 132], F32, st) for r in range(2)] for nm in ("R", "eD", "eDm", "tmp", "num", "hq")}
                tb = {nm: [P.sbuf(f"mtb_{nm}{r}", [128, 128], BF16, st) for r in range(2)] for nm in ("Sg", "Kw")}
                sm1 = {nm: [P.sbuf(f"ms_{nm}{r}", [128, 1], F32, st) for r in range(2)] for nm in ("dn", "rdn")}
                fz = {nm: P.sbuf(f"mf_{nm}", [128, 512], F32, st) for nm in ("sq", "sd", "o1")}
                brr = 0
                for (c0, cn) in [(0, 512), (512, 512), (1024, 512), (1536, 512), (2048, 256)]:
                    for (w, nm) in ((Wq, "q"), (Wk, "k"), (Wo, "o")):
                        b = brr % 8
                        brr += 1
                        for kk in range(8):
                            k.mm(k.bank(b)[:, 0:cn], w[:, kk, :], k.AB[:, kk, c0:c0 + cn], kk == 0, kk == 7, [("mW", nm)], [k.bk(b)])
                        if nm == "q":
                            k.act(QT[:, c0:c0 + cn], k.bank(b)[:, 0:cn], AF.Copy, [k.bk(b)], [("mQT", c0)], scale=float(128 ** -0.5))
                        elif nm == "k":
                            k.cp("dve", KT[:, c0:c0 + cn], k.bank(b)[:, 0:cn], [k.bk(b)], [("mKT", c0)])
                        else:
                            k.act(SO[:, c0:c0 + cn], k.bank(b)[:, 0:cn], AF.Sigmoid, [k.bk(b)], [("mSO", c0)])
                for d in range(2):
                    P.add("pool", lambda e, d=d: e.memset(Vt[d][:, :, 128:129], 1.0), [], [("mVt1", d)])
                    for u, (st0, step) in enumerate(proc_tiles(d)):
                        for (w, nm) in ((Wk, "k"), (Wv, "v")):
                            b = brr % 8
                            brr += 1
                            for kk in range(8):
                                k.mm(k.bank(b)[:, 0:128], hview(d, u, st0, kk), w[:, kk, :], kk == 0, kk == 7, [("mW", nm)], [k.bk(b)])
                            if nm == "k":
                                k.cp("act", Kt[d][:, u, :], k.bank(b)[:, 0:128], [k.bk(b)], [("mKt", d, u)])
                            else:
                                k.cp("dve", Vt[d][:, u, 0:128], k.bank(b)[:, 0:128], [k.bk(b)], [("mVt", d, u)])
                qk_keys = [("mQT", c0) for c0 in (0, 512, 1024, 1536, 2048)] + [("mKT", c0) for c0 in (0, 512, 1024, 1536, 2048)]
                QTr = P.sbuf("mQTr", [128, T], BF16, st)
                KTr = P.sbuf("mKTr", [128, T], BF16, st)
                k.cp("pool", QTr[:], tokview(QT, T, 0, T - 1, -1, T), qk_keys, ["mQTr"])
                k.cp("pool", KTr[:], tokview(KT, T, 0, T - 1, -1, T), qk_keys, ["mKTr"])
                qk_keys = qk_keys + ["mQTr", "mKTr"]
                P.add("pool", lambda e: e.memset(HS[:], 0.0), [], [("mHS", t_) for t_ in range(NT)])
                for d in range(2):
                    P.add("dve", lambda e, d=d: e.memset(Cs[d][:], 0.0), [], [("mC", d)])
                    P.add("pool", lambda e, d=d: e.memset(Cbs[d][:], 0.0), [], [("mCb", d)])
                ptl = [proc_tiles(0), proc_tiles(1)]
                for u in range(NT):
                    for d in range(2):
                        st0, step = ptl[d][u]
                        r = d
                        C, Cb = Cs[d], Cbs[d]
                        stile = (st0 // 128) if d == 0 else (NT - 1 - u)
                        tk = lambda nm, r=r: ("mt", nm, r)
                        if d == 0:
                            qv, kv = QT[:, st0:st0 + 128], KT[:, st0:st0 + 128]
                        else:
                            qv, kv = QTr[:, u * 128:(u + 1) * 128], KTr[:, u * 128:(u + 1) * 128]
                        bS, bD, bN, bI = 4 * d, 4 * d + 1, 4 * d + 2, 4 * d + 3
                        bT, bC = bD, bS
                        k.mm(k.bank(bS)[:, 0:128], kv, qv, True, True, qk_keys, [k.bk(bS)])
                        k.ts("dve", tm["R"][r][:, 0:128], cm["m_le128"][:], LF[d][:, u, h:h + 1], None, ALU.mult, ALU.bypass, [], [tk("R")])
                        k.mm(k.bank(bD)[:, 0:128], cm["m_gt128"][:], tm["R"][r][:, 0:128], True, True, [tk("R")], [k.bk(bD)])
                        k.act(tm["eD"][r][:, 0:128], k.bank(bD)[:, 0:128], AF.Exp, [k.bk(bD)], [tk("eD")], bias=IG[d][:, u, h:h + 1], scale=1.0)
                        k.tt("pool", tm["eDm"][r][:, 0:128], tm["eD"][r][:, 0:128], cm["m_le128"][:], ALU.mult, [tk("eD")], [tk("eDm")])
                        k.tt("dve", tb["Sg"][r][:], k.bank(bS)[:, 0:128], tm["eDm"][r][:, 0:128], ALU.mult, [k.bk(bS), tk("eDm")], [tk("Sg")])
                        k.mm(k.bank(bN)[:, 0:129], tb["Sg"][r][:], Vt[d][:, u, :], True, True, [tk("Sg"), ("mVt", d, u), ("mVt1", d)], [k.bk(bN)])
                        k.mm(k.bank(bI)[:, 0:129], qv, Cb[:], True, True, qk_keys + [("mCb", d)], [k.bk(bI)])
                        k.ts("dve", tm["tmp"][r][:, 0:129], k.bank(bI)[:, 0:129], WI[d][:, u, h:h + 1], None, ALU.mult, ALU.bypass, [k.bk(bI)], [tk("tmp")])
                        k.tt("dve", tm["num"][r][:, 0:129], k.bank(bN)[:, 0:129], tm["tmp"][r][:, 0:129], ALU.add, [k.bk(bN), tk("tmp")], [tk("num")])
                        k.act(sm1["dn"][r][:], tm["num"][r][:, 128:129], AF.Abs, [tk("num")], [tk("dn")])
                        k.ts("dve", sm1["dn"][r][:], sm1["dn"][r][:], 1.0, None, ALU.max, ALU.bypass, [tk("dn")], [tk("dn")])
                        P.add("dve", lambda e, o=sm1["rdn"][r], a=sm1["dn"][r]: e.reciprocal(out=o[:], in_=a[:]), [tk("dn")], [tk("rdn")])
                        k.ts("dve", tm["hq"][r][:, 0:128], tm["num"][r][:, 0:128], sm1["rdn"][r][:, 0:1], None, ALU.mult, ALU.bypass, [tk("num"), tk("rdn")], [tk("hq")])
                        k.tr(k.bank(bT)[:, 0:128], tm["hq"][r][:, 0:128], k.identf[:], [tk("hq")], [k.bk(bT)])
                        hv = tokview(HS, T, 0, st0, step, 128)
                        k.tt("dve", hv, k.bank(bT)[:, 0:128], hv, ALU.add, [k.bk(bT), ("mHS", stile)], [("mHS", stile)])
                        k.ts("pool", tb["Kw"][r][:], Kt[d][:, u, :], WS[d][:, u, h:h + 1], None, ALU.mult, ALU.bypass, [("mKt", d, u)], [tk("Kw")])
                        k.mm(k.bank(bC)[:, 0:129], tb["Kw"][r][:], Vt[d][:, u, :], True, True, [tk("Kw"), ("mVt", d, u), ("mVt1", d)], [k.bk(bC)])
                        k.stt("dve", C[:], C[:], WC[d][:, u, h:h + 1], k.bank(bC)[:, 0:129], ALU.mult, ALU.add, [("mC", d), k.bk(bC)], [("mC", d)])
                        k.cp("pool", Cb[:], C[:], [("mC", d)], [("mCb", d)])
                hk = [("mHS", u) for u in range(NT)]
                for (c0, cn) in [(0, 512), (512, 512), (1024, 512), (1536, 512), (2048, 256)]:
                    b = brr % 2
                    brr += 1
                    k.act(fz["sq"][:, 0:cn], HS[:, c0:c0 + cn], AF.Square, hk, ["mfsq"])
                    k.mm(k.bank(b)[:, 0:cn], cm["ones"][:], fz["sq"][:, 0:cn], True, True, ["mfsq"], [k.bk(b)])
                    k.act(fz["sd"][:, 0:cn], k.bank(b)[:, 0:cn], AF.Sqrt, [k.bk(b)], ["mfsd"], scale=1.0 / 128, bias=k.epsnm[:, 0:1])
                    P.add("dve", lambda e, o=fz["sd"][:, 0:cn]: e.reciprocal(out=o, in_=o), ["mfsd"], ["mfsd"])
                    k.tt("pool", fz["o1"][:, 0:cn], HS[:, c0:c0 + cn], fz["sd"][:, 0:cn], ALU.mult, hk + ["mfsd"], ["mfo1"])
                    k.stt("dve", mTt[:, h, c0:c0 + cn], fz["o1"][:, 0:cn], ng[:, h:h + 1], SO[:, c0:c0 + cn], ALU.mult, ALU.mult,
                          ["mfo1", ("mSO", c0)], [("mTt", 4 + h, c0)])


DECAY_K = -math.exp(-0.5)


def col_vec(ap2d):
    return ap2d.rearrange("n (c p) -> p n c", p=128)


def rwkv_mixer(k, i, cm):
    P, I = k.P, k.I
    j = i // 2
    Wr = I["rec_w_in"][j]
    TCS = [(0, 512), (512, 512), (1024, 512), (1536, 512), (2048, 256)]
    with P.phase() as sr:
        L1 = P.sbuf("rL1", [128, T], BF16, sr)
        L2 = P.sbuf("rL2", [128, T], BF16, sr)
        L3 = P.sbuf("rL3", [128, T], BF16, sr)
        with P.phase() as st:
            W = P.sbuf("rW", [128, 8, 2048], BF16, st)
            for kk in range(8):
                k.dma("pool", W[:, kk, :], Wr[kk * 128:(kk + 1) * 128, 0:2048], [], [("rW", kk)])
            hd = P.sbuf("rhd", [128, 8, T], BF16, st)
            tmp = [P.sbuf(f"rhtmp{r}", [128, S], BF16, st) for r in range(2)]
            muT = P.sbuf("rmuT", [128, 6, 4], F32, st)
            k.dma("sp", muT[:], col_vec(I["rwkv_mu"][j]), [], ["muT"], allow_slow_non_contiguous=True)
            w1b = P.sbuf("rw1b", [128, 2, 4, 32], BF16, st)
            a1b = P.sbuf("ra1b", [128, 2, 4, 32], BF16, st)
            g1b = P.sbuf("rg1b", [128, 4, 96], BF16, st)
            for d in range(2):
                k.dma("pool", w1b[:, d], I["rwkv_w1"][j, d].rearrange("(c p) r -> p c r", p=128), [], [("w1b", d)])
                k.dma("pool", a1b[:, d], I["rwkv_a1"][j, d].rearrange("(c p) r -> p c r", p=128), [], [("a1b", d)])
            k.dma("pool", g1b[:], I["rwkv_g1"][j].rearrange("(c p) r -> p c r", p=128), [], ["g1b"])
            n = 0
            for kk in range(8):
                for (a, b) in ((0, S), (S, T)):
                    r = n % 2
                    e1 = "pool" if n % 2 else "dve"
                    n += 1
                    nn = b - a
                    k.tt(e1, tmp[r][:, 0:nn - 2], k.AB[:, kk, a:b - 2], k.AB[:, kk, a + 2:b], ALU.add, [], [("rhtmp", r)])
                    k.stt("dve", hd[:, kk, a + 1:b - 1], tmp[r][:, 0:nn - 2], 0.5, k.AB[:, kk, a + 1:b - 1], ALU.mult, ALU.subtract, [("rhtmp", r)], [("hd", kk, a, 0)])
                    k.stt("dve", hd[:, kk, a:a + 1], k.AB[:, kk, a + 1:a + 2], 0.5, k.AB[:, kk, a:a + 1], ALU.mult, ALU.subtract, [], [("hd", kk, a, 1)])
                    k.stt("dve", hd[:, kk, b - 1:b], k.AB[:, kk, b - 2:b - 1], 0.5, k.AB[:, kk, b - 1:b], ALU.mult, ALU.subtract, [], [("hd", kk, a, 2)])
            P.flush(barrier=True)
            ps = [P.sbuf(f"rps{r}", [128, 512], F32, st) for r in range(2)]
            zt = {nm: [P.sbuf(f"rz{nm}{r}", [128, 512], BF16, st) for r in range(2)] for nm in ("w", "a", "g")}
            o32 = [P.sbuf(f"ro32{r}", [128, 512], F32, st) for r in range(2)]
            n = 0
            for (c0, cn) in TCS:
                for c in range(4):
                    r = n % 2
                    n += 1
                    for kk in range(8):
                        k.mm(k.bank(0)[:, 0:cn], W[:, kk, 1536 + c * 128:1536 + (c + 1) * 128], k.AB[:, kk, c0:c0 + cn], kk == 0, kk == 7, [("rW", kk)], [k.bk(0)])
                    for kk in range(8):
                        k.mm(k.bank(1)[:, 0:cn], W[:, kk, 1536 + c * 128:1536 + (c + 1) * 128], hd[:, kk, c0:c0 + cn], kk == 0, kk == 7, [("rW", kk)], [k.bk(1)])
                    k.cp("act", ps[r][:, 0:cn], k.bank(0)[:, 0:cn], [k.bk(0)], [("rps", r)])
                    for (nm, m) in (("w", 3), ("a", 4), ("g", 5)):
                        k.stt("dve", zt[nm][r][:, 0:cn], k.bank(1)[:, 0:cn], muT[:, m, c:c + 1], ps[r][:, 0:cn], ALU.mult, ALU.add,
                              [k.bk(1), ("rps", r), "muT"], [("rz", nm, r)])
                    f, l = (c == 0), (c == 3)
                    k.mm(k.bank(2)[0:32, 0:cn], w1b[:, 0, c, :], zt["w"][r][:, 0:cn], f, l, [("rz", "w", r), ("w1b", 0)], [k.bk(2)])
                    k.mm(k.bank(3)[32:64, 0:cn], w1b[:, 1, c, :], zt["w"][r][:, 0:cn], f, l, [("rz", "w", r), ("w1b", 1)], [k.bk(3)])
                    k.mm(k.bank(4)[64:96, 0:cn], a1b[:, 0, c, :], zt["a"][r][:, 0:cn], f, l, [("rz", "a", r), ("a1b", 0)], [k.bk(4)])
                    k.mm(k.bank(5)[0:32, 0:cn], a1b[:, 1, c, :], zt["a"][r][:, 0:cn], f, l, [("rz", "a", r), ("a1b", 1)], [k.bk(5)])
                    k.mm(k.bank(6)[0:96, 0:cn], g1b[:, c, :], zt["g"][r][:, 0:cn], f, l, [("rz", "g", r), "g1b"], [k.bk(6)])
                k.act(L1[0:32, c0:c0 + cn], k.bank(2)[0:32, 0:cn], AF.Tanh, [k.bk(2)], [("L1", 0, c0)])
                k.act(L1[32:64, c0:c0 + cn], k.bank(3)[32:64, 0:cn], AF.Tanh, [k.bk(3)], [("L1", 1, c0)])
                k.cp("act", L1[64:96, c0:c0 + cn], k.bank(4)[64:96, 0:cn], [k.bk(4)], [("L1", 2, c0)])
                k.cp("act", L3[0:32, c0:c0 + cn], k.bank(5)[0:32, 0:cn], [k.bk(5)], [("L3", c0)])
                k.act(L2[0:96, c0:c0 + cn], k.bank(6)[0:96, 0:cn], AF.Sigmoid, [k.bk(6)], [("L2", c0)])
            for g in range(3):
                for (c0, cn) in TCS:
                    for c in range(4):
                        r = n % 2
                        n += 1
                        b0, b1 = (0, 1) if r == 0 else (2, 3)
                        for kk in range(8):
                            k.mm(k.bank(b0)[:, 0:cn], W[:, kk, g * 512 + c * 128:g * 512 + (c + 1) * 128], k.AB[:, kk, c0:c0 + cn], kk == 0, kk == 7, [("rW", kk)], [k.bk(b0)])
                        for kk in range(8):
                            k.mm(k.bank(b1)[:, 0:cn], W[:, kk, g * 512 + c * 128:g * 512 + (c + 1) * 128], hd[:, kk, c0:c0 + cn], kk == 0, kk == 7, [("rW", kk)], [k.bk(b1)])
                        k.cp("act", ps[r][:, 0:cn], k.bank(b0)[:, 0:cn], [k.bk(b0)], [("rps", r)])
                        k.stt("dve", o32[r][:, 0:cn], k.bank(b1)[:, 0:cn], muT[:, g, c:c + 1], ps[r][:, 0:cn], ALU.mult, ALU.add,
                              [k.bk(b1), ("rps", r), "muT"], [("ro32", r)])
                        k.dma("sp", k.rkv_d[g, c * 128:(c + 1) * 128, c0:c0 + cn], o32[r][:, 0:cn], [("ro32", r)], [("rkv_d", g, c, c0)])
        for hp in range(4):
            rwkv_stage_b(k, i, hp, cm, L1, L2, L3)


def rwkv_stage_b(k, i, hp, cm, L1, L2, L3):
    P, I = k.P, k.I
    j = i // 2
    TCS = [(0, 512), (512, 512), (1024, 512), (1536, 512), (2048, 256)]
    BN = 512
    cs_ = slice(hp * 128, (hp + 1) * 128)
    with P.phase() as st:
        pv = {}
        for nm, src, nrow in (("w0", "rwkv_w0", 2), ("a0", "rwkv_a0", 2), ("kvec", "rwkv_kvec", 2), ("gn", "rwkv_gn", 2)):
            pv[nm] = P.sbuf("rp_" + nm, [128, nrow], F32, st)
            k.dma("sp", pv[nm][:], I[src][j][:, cs_].rearrange("n p -> p n"), [], [("rp", nm)], allow_slow_non_contiguous=True)
        pv["rk"] = P.sbuf("rp_rk", [128, 1], F32, st)
        k.dma("sp", pv["rk"][:], I["rwkv_rk"][j, cs_].rearrange("(p o) -> p o", o=1), [], [("rp", "rk")], allow_slow_non_contiguous=True)
        pv["omk"] = P.sbuf("rp_omk", [128, 1], F32, st)
        k.ts("dve", pv["omk"][:], pv["kvec"][:, 1:2], -1.0, 1.0, ALU.mult, ALU.add, [("rp", "kvec")], [("rp", "omk")])
        pv["gne"] = P.sbuf("rp_gne", [128, 1], F32, st)
        P.add("pool", lambda e: e.memset(pv["gne"][:], RWKV_GN_EPS), [], [("rp", "gne")])
        w2b = P.sbuf("rw2b", [128, 128], BF16, st)
        a2b1 = P.sbuf("ra2b1", [32, 128], BF16, st)
        g2b = P.sbuf("rg2b", [96, 128], BF16, st)
        k.dma("pool", w2b[0:32, :], I["rwkv_w2"][j, 0][:, cs_], [], [("w2b", 0)])
        k.dma("pool", w2b[32:64, :], I["rwkv_w2"][j, 1][:, cs_], [], [("w2b", 1)])
        k.dma("pool", w2b[64:96, :], I["rwkv_a2"][j, 0][:, cs_], [], [("w2b", 2)])
        k.dma("pool", a2b1[:], I["rwkv_a2"][j, 1][:, cs_], [], ["a2b1"])
        k.dma("pool", g2b[:], I["rwkv_g2"][j][:, cs_], [], ["g2b"])
        R = P.sbuf("rR", [128, T], BF16, st)
        Kx = P.sbuf("rK", [128, T], BF16, st)
        V = P.sbuf("rV", [128, T], BF16, st)
        k.dma("pool", R[:], k.rkv_d[0, cs_, :], [], ["rR"])
        k.dma("pool", Kx[:], k.rkv_d[1, cs_, :], [], ["rK"])
        k.dma("pool", V[:], k.rkv_d[2, cs_, :], [], ["rV"])
        KK = P.sbuf("rKK", [128, T], F32, st)
        GA = P.sbuf("rGA", [128, T], BF16, st)
        LWr = [P.sbuf(f"rlw{d}", [128, T], F32, st) for d in range(2)]
        Aa = [P.sbuf(f"raa{d}", [128, T], BF16, st) for d in range(2)]
        YS = P.sbuf("rYS", [128, T], F32, st)
        ft = {nm: P.sbuf("rf_" + nm, [128, 512], F32, st) for nm in ("a", "b", "c")}
        brr = [0]

        def nb():
            brr[0] += 1
            return brr[0] % 8

        for (c0, cn) in TCS:
            sl = slice(c0, c0 + cn)
            k.act(ft["a"][:, 0:cn], Kx[:, sl], AF.Square, ["rK", ("rp", "kvec")], ["rfa"], scale=pv["kvec"][:, 0:1])
            b = nb()
            k.mm(k.bank(b)[:, 0:cn], cm["blk64"][:], ft["a"][:, 0:cn], True, True, ["rfa"], [k.bk(b)])
            k.ts("dve", ft["b"][:, 0:cn], k.bank(b)[:, 0:cn], 1e-24, None, ALU.max, ALU.bypass, [k.bk(b)], ["rfb"])
            k.act(ft["b"][:, 0:cn], ft["b"][:, 0:cn], AF.Sqrt, ["rfb"], ["rfb"])
            P.add("dve", lambda e, o=ft["b"][:, 0:cn]: e.reciprocal(out=o, in_=o), ["rfb"], ["rfb"])
            k.stt("dve", KK[:, sl], Kx[:, sl], pv["kvec"][:, 0:1], ft["b"][:, 0:cn], ALU.mult, ALU.mult, ["rK", "rfb", ("rp", "kvec")], [("rKK", c0)])
            b = nb()
            k.mm(k.bank(b)[:, 0:cn], g2b[:], L2[0:96, sl], True, True, ["g2b"], [k.bk(b)])
            k.cp("act", GA[:, sl], k.bank(b)[:, 0:cn], [k.bk(b)], [("rGA", c0)])
            for d in range(2):
                b = nb()
                k.mm(k.bank(b)[:, 0:cn], w2b[32 * d:32 * d + 32, :], L1[32 * d:32 * d + 32, sl], True, True, [("w2b", d)], [k.bk(b)])
                k.act(ft["c"][:, 0:cn], k.bank(b)[:, 0:cn], AF.Sigmoid, [k.bk(b), ("rp", "w0")], ["rfc"], bias=pv["w0"][:, d:d + 1], scale=1.0)
                k.ts("dve", LWr[d][:, sl], ft["c"][:, 0:cn], float(DECAY_K), None, ALU.mult, ALU.bypass, ["rfc"], [("rlw", d, c0)])
                b = nb()
                if d == 0:
                    k.mm(k.bank(b)[:, 0:cn], w2b[64:96, :], L1[64:96, sl], True, True, [("w2b", 2)], [k.bk(b)])
                else:
                    k.mm(k.bank(b)[:, 0:cn], a2b1[:], L3[0:32, sl], True, True, ["a2b1"], [k.bk(b)])
                k.act(Aa[d][:, sl], k.bank(b)[:, 0:cn], AF.Sigmoid, [k.bk(b), ("rp", "a0")], [("raa", d, c0)], bias=pv["a0"][:, d:d + 1], scale=1.0)
        P.flush(barrier=True)
        rst = P.sbuf("rrst", [128, BN], BF16, st)
        P.add("pool", lambda e: e.memset(rst[:], 1.0), [], ["rst"])
        P.add("pool", lambda e: e.memset(bass.AP(rst, 0, [[BN, 128], [64, BN // 64]]), 0.0), ["rst"], ["rst"])
        bl = {nm: P.sbuf("rb_" + nm, [128, BN], F32, st) for nm in ("LW", "E", "kka", "ke", "AH", "KH", "AW", "KW", "YT", "t", "Vb")}
        BR = P.sbuf("rBR", [128, BN // 128, 2, 128], F32, st)
        Hs = P.sbuf("rH", [128, 64], F32, st)
        tl = {nm: [P.sbuf(f"rt_{nm}{r}", [128, 128], F32 if nm in ("R1T", "Y0T") else BF16, st) for r in range(4)]
              for nm in ("Mm", "nAT", "Bm", "Btm", "MT", "Pv", "Q", "QT", "G", "nG2", "R1T", "Y0T")}
        TMs = [P.sbuf(f"rTM{r}", [128, 4, 128], BF16, st) for r in range(2)]
        FTs = [P.sbuf(f"rFT{r}", [128, 2, 64], F32, st) for r in range(4)]
        Zs = [P.sbuf(f"rZs{r}", [128, 2, 64], F32, st) for r in range(4)]
        for d in range(2):
            if d == 0:
                blocks = [(S, 1, NCTX), (0, 1, 512), (512, 1, 512), (1024, 1, 512), (1536, 1, 512)]
            else:
                blocks = [(T - 1, -1, NCTX), (S - 1, -1, 512), (S - 513, -1, 512), (S - 1025, -1, 512), (S - 1537, -1, 512)]
            P.add("dve", lambda e: e.memset(Hs[:], 0.0), [], [("rH", 0), ("rH", 1)])
            for (bs, step, bn) in blocks:
                Vw = lambda arr, bs=bs, step=step, bn=bn: tokview(arr, T, 0, bs, step, bn)
                B_ = lambda nm, bn=bn: bl[nm][:, 0:bn]
                nch = bn // 64
                P.add("dve", lambda e, o=B_("LW"), a=rst[:, 0:bn], b=Vw(LWr[d]): e.tensor_tensor_scan(
                    out=o, data0=a, data1=b, initial=0.0, op0=ALU.mult, op1=ALU.add), ["rst"], ["bLW"])
                lwend = bass.AP(bl["LW"], 63, [[BN, 128], [64, nch], [0, 64]])
                lw3 = bl["LW"][:, 0:bn].rearrange("p (c t) -> p c t", t=64)
                a_v, kk_v, k_v, r_v = Vw(Aa[d]), Vw(KK), Vw(Kx), Vw(R)
                k.cp("pool", B_("Vb"), Vw(V), [], ["bVb"])
                k.tt("pool", B_("kka"), kk_v, a_v, ALU.mult, [], ["bkka"])
                k.ts("dve", B_("t"), a_v, pv["kvec"][:, 1:2], pv["omk"][:, 0:1], ALU.mult, ALU.add, [("rp", "omk")], ["bt"])
                k.tt("pool", B_("ke"), B_("t"), k_v, ALU.mult, ["bt"], ["bke"])
                k.act(B_("E"), B_("LW"), AF.Exp, ["bLW"], ["bE"], scale=-1.0)
                k.tt("dve", B_("AH"), B_("kka"), B_("E"), ALU.mult, ["bkka", "bE"], ["bAH"])
                k.tt("pool", B_("KH"), B_("ke"), B_("E"), ALU.mult, ["bke", "bE"], ["bKH"])
                k.tt("dve", B_("t").rearrange("p (c t) -> p c t", t=64), lwend, lw3, ALU.subtract, ["bLW", "bt"], ["bt"])
                k.act(B_("E"), B_("t"), AF.Exp, ["bt", "bE", "bAH", "bKH"], ["bE"])
                k.tt("dve", B_("AW"), B_("kka"), B_("E"), ALU.mult, ["bkka", "bE"], ["bAW"])
                k.tt("pool", B_("KW"), B_("ke"), B_("E"), ALU.mult, ["bke", "bE"], ["bKW"])
                k.tt("dve", B_("t"), B_("LW"), Vw(LWr[d]), ALU.subtract, ["bLW", "bt"], ["bt"])
                k.act(B_("E"), B_("t"), AF.Exp, ["bt", "bE", "bAW", "bKW"], ["bE"])
                ntile = bn // 128
                brv = lambda q, ntile=ntile: bass.AP(BR, q * 128, [[(BN // 128) * 256, 128], [256, ntile], [1, 128]])
                k.tt("dve", brv(0), bass.AP(kk_v.tensor, kk_v.offset, [[T, 128], [128 * step, ntile], [step, 128]]),
                     B_("E").rearrange("p (c t) -> p c t", t=128), ALU.mult, ["bE"], ["bBR0"])
                k.act(B_("E"), B_("LW"), AF.Exp, ["bLW", "bE", "bBR0"], ["bE"])
                k.tt("pool", brv(1), bass.AP(r_v.tensor, r_v.offset, [[T, 128], [128 * step, ntile], [step, 128]]),
                     B_("E").rearrange("p (c t) -> p c t", t=128), ALU.mult, ["bE"], ["bBR1"])
                k.act(bl["t"][:, 0:nch], bass.AP(bl["LW"], 63, [[BN, 128], [64, nch]]), AF.Exp, ["bLW", "bt", "bE"], ["bWC", "bt"])
                for tg0 in range(0, ntile, 2):
                    tis = [ti for ti in (tg0, tg0 + 1) if ti < ntile]
                    chains = [(ti, hh) for ti in tis for hh in range(2)]
                    for ti in tis:
                        tsl = slice(ti * 128, (ti + 1) * 128)
                        r = ti % 2
                        bT = nb()
                        srcs = [BR[:, ti, 0, :], bl["Vb"][:, tsl], bl["AW"][:, tsl], bl["KW"][:, tsl]]
                        for q, s_ in enumerate(srcs):
                            k.tr(k.bank(bT)[:, q * 128:(q + 1) * 128], s_, k.identf[:], ["bBR0", "bAW", "bKW", "bVb"], [k.bk(bT)])
                        k.cp("act", TMs[r][:], k.bank(bT)[:, 0:512].rearrange("p (q n) -> p q n", q=4), [k.bk(bT)], [("rTM", r)])
                    CH = []
                    for x, (ti, hh) in enumerate(chains):
                        CH.append(dict(x=x, ti=ti, hh=hh, r=ti % 2, ps=slice(hh * 64, (hh + 1) * 64), tsl=slice(ti * 128, (ti + 1) * 128),
                                       L={nm: tl[nm][x] for nm in tl}, lk=(lambda nm, x=x: ("rtl", nm, x))))
                    for c_ in CH:
                        L, lk, ps_, tsl, ti = c_["L"], c_["lk"], c_["ps"], c_["tsl"], c_["ti"]
                        bX = nb()
                        k.mm(k.bank(bX)[:, 0:256], bl["AH"][ps_, tsl], BR[ps_, ti, :, :], True, True, ["bAH", "bBR0", "bBR1"], [k.bk(bX)])
                        k.mm(k.bank(bX)[:, 256:512], bl["KH"][ps_, tsl], BR[ps_, ti, :, :], True, True, ["bKH", "bBR0", "bBR1"], [k.bk(bX)])
                        k.tt("dve", L["Mm"][:], k.bank(bX)[:, 0:128], cm["m_lt"][:], ALU.mult, [k.bk(bX)], [lk("Mm")])
                        k.stt("dve", L["nAT"][:], k.bank(bX)[:, 128:256], -1.0, cm["m_le"][:], ALU.mult, ALU.mult, [k.bk(bX)], [lk("nAT")])
                        k.tt("dve", L["Bm"][:], k.bank(bX)[:, 256:384], cm["m_lt"][:], ALU.mult, [k.bk(bX)], [lk("Bm")])
                        k.tt("dve", L["Btm"][:], k.bank(bX)[:, 384:512], cm["m_le"][:], ALU.mult, [k.bk(bX)], [lk("Btm")])
                    for c_ in CH:
                        L, lk = c_["L"], c_["lk"]
                        bY = nb()
                        pyb = k.bank(bY).bitcast(BF16)
                        k.tr(pyb[:, 0:128], L["Mm"][:], k.identb[:], [lk("Mm")], [k.bk(bY)])
                        k.cp("act", L["MT"][:], pyb[:, 0:128], [k.bk(bY)], [lk("MT")])
                        k.tt("pool", L["Pv"][:], k.identf[:], L["Mm"][:], ALU.subtract, [lk("Mm")], [lk("Pv")])
                        c_["Q"], c_["QT"], c_["qk"], c_["qtk"] = L["Mm"], L["MT"], lk("Mm"), lk("MT")
                    for lvl in range(1, 6):
                        for c_ in CH:
                            L, lk = c_["L"], c_["lk"]
                            bq = nb()
                            c_["bq"] = bq
                            if lvl < 5:
                                k.mm(k.bank(bq)[:, 0:128], c_["QT"][:], c_["Q"][:], True, True, [c_["qk"], c_["qtk"]], [k.bk(bq)])
                            k.mm(k.bank(bq)[:, 128:256], c_["Q"][:], c_["QT"][:], True, True, [c_["qk"], c_["qtk"]], [k.bk(bq)])
                        for c_ in CH:
                            L, lk, bq = c_["L"], c_["lk"], c_["bq"]
                            if lvl < 5:
                                k.cp("act", L["Q"][:], k.bank(bq)[:, 0:128], [k.bk(bq)], [lk("Q")])
                            k.cp("act", L["QT"][:], k.bank(bq)[:, 128:256], [k.bk(bq)], [lk("QT")])
                            c_["Q"], c_["QT"], c_["qk"], c_["qtk"] = L["Q"], L["QT"], lk("Q"), lk("QT")
                        for c_ in CH:
                            L, lk = c_["L"], c_["lk"]
                            bp = nb()
                            c_["bp"] = bp
                            k.mm(k.bank(bp)[:, 0:128], c_["QT"][:], L["Pv"][:], True, True, [c_["qtk"], lk("Pv")], [k.bk(bp)])
                        for c_ in CH:
                            L, lk, bp = c_["L"], c_["lk"], c_["bp"]
                            k.tt("dve", L["Pv"][:], L["Pv"][:], k.bank(bp)[:, 0:128], ALU.add, [k.bk(bp), lk("Pv")], [lk("Pv")])
                    for c_ in CH:
                        L, lk, ps_, r = c_["L"], c_["lk"], c_["ps"], c_["r"]
                        bb = nb()
                        k.mm(k.bank(bb)[:, 0:64], L["Bm"][:], TMs[r][:, 1, ps_], True, True, [lk("Bm"), ("rTM", r)], [k.bk(bb)])
                        k.cp("act", L["Mm"][:, 64:128], k.bank(bb)[:, 0:64], [k.bk(bb)], [lk("Mm")])
                        k.cp("pool", L["Mm"][:, 0:64], TMs[r][:, 0, ps_], [("rTM", r)], [lk("Mm")])
                    for c_ in CH:
                        L, lk = c_["L"], c_["lk"]
                        bg = nb()
                        k.mm(k.bank(bg)[:, 0:128], L["Pv"][:], L["Mm"][:], True, True, [lk("Pv"), lk("Mm")], [k.bk(bg)])
                        k.cp("act", L["G"][:], k.bank(bg)[:, 0:128], [k.bk(bg)], [lk("G")])
                        k.ts("pool", L["nG2"][:, 0:64], L["G"][:, 64:128], -1.0, None, ALU.mult, ALU.bypass, [lk("G")], [lk("nG2")])
                    for c_ in CH:
                        L, lk, ps_, r, ti = c_["L"], c_["lk"], c_["ps"], c_["r"], c_["ti"]
                        b1 = nb()
                        k.mm(k.bank(b1)[ps_, 0:128], L["G"][:, 0:64], L["nAT"][:], True, True, [lk("G"), lk("nAT")], [k.bk(b1)])
                        k.tt("dve", L["R1T"][ps_, :], k.bank(b1)[ps_, 0:128], BR[ps_, ti, 1, :], ALU.add, [k.bk(b1), "bBR1"], [lk("R1T")])
                        b2 = nb()
                        k.mm(k.bank(b2)[ps_, 0:128], TMs[r][:, 1, ps_], L["Btm"][:], True, False, [("rTM", r), lk("Btm")], [k.bk(b2)])
                        k.mm(k.bank(b2)[ps_, 0:128], L["G"][:, 64:128], L["nAT"][:], False, True, [lk("G"), lk("nAT")], [k.bk(b2)])
                        k.cp("act", L["Y0T"][ps_, :], k.bank(b2)[ps_, 0:128], [k.bk(b2)], [lk("Y0T")])
                    for c in range(2):
                        rows = slice(c * 64, (c + 1) * 64)
                        for c_ in CH:
                            L, lk, ps_, r, ti, hh, x = c_["L"], c_["lk"], c_["ps"], c_["r"], c_["ti"], c_["hh"], c_["x"]
                            bf_ = nb()
                            k.mm(k.bank(bf_)[ps_, 0:64], L["G"][rows, 0:64], TMs[r][rows, 2, ps_], True, True, [lk("G"), ("rTM", r)], [k.bk(bf_)])
                            wc = bl["t"][ps_, ti * 2 + c:ti * 2 + c + 1]
                            k.stt("dve", FTs[x][ps_, c, :], k.identf[ps_, hh * 64:(hh + 1) * 64], wc, k.bank(bf_)[ps_, 0:64], ALU.mult, ALU.subtract,
                                  [k.bk(bf_), "bWC"], [("rFT", x, c)])
                            bz = nb()
                            k.mm(k.bank(bz)[ps_, 0:64], TMs[r][rows, 3, ps_], TMs[r][rows, 1, ps_], True, False, [("rTM", r)], [k.bk(bz)])
                            k.mm(k.bank(bz)[ps_, 0:64], TMs[r][rows, 2, ps_], L["nG2"][rows, 0:64], False, True, [("rTM", r), lk("nG2")], [k.bk(bz)])
                            k.cp("act", Zs[x][ps_, c, :], k.bank(bz)[ps_, 0:64], [k.bk(bz)], [("rZs", x, c)])
                    for ti in tis:
                        for c in range(2):
                            cc = slice(c * 64, (c + 1) * 64)
                            for c_ in CH:
                                if c_["ti"] != ti:
                                    continue
                                L, lk, ps_, hh, x = c_["L"], c_["lk"], c_["ps"], c_["hh"], c_["x"]
                                by = nb()
                                k.mm(k.bank(by)[ps_, 0:64], Hs[ps_, :], L["R1T"][ps_, cc], True, True, [("rH", hh), lk("R1T")], [k.bk(by)])
                                k.tt("dve", bl["YT"][ps_, ti * 128 + c * 64:ti * 128 + (c + 1) * 64], k.bank(by)[ps_, 0:64], L["Y0T"][ps_, cc], ALU.add,
                                     [k.bk(by), lk("Y0T")], [("bYT", hh, ti, c)])
                                bh = nb()
                                k.mm(k.bank(bh)[ps_, 0:64], FTs[x][ps_, c, :], Hs[ps_, :], True, True, [("rH", hh), ("rFT", x, c)], [k.bk(bh)])
                                k.tt("dve", Hs[ps_, :], k.bank(bh)[ps_, 0:64], Zs[x][ps_, c, :], ALU.add, [k.bk(bh), ("rZs", x, c), ("rH", hh)], [("rH", hh)])
                yk = [("bYT", hh, ti, c) for hh in range(2) for ti in range(ntile) for c in range(2)]
                ysv = Vw(YS)
                if d == 0:
                    k.cp("pool", ysv, B_("YT"), yk, [("rYS", bs)])
                else:
                    k.tt("pool", ysv, ysv, B_("YT"), ALU.add, yk, [("rYS", bs)])
                P.flush(barrier=True)
        for (c0, cn) in TCS:
            sl = slice(c0, c0 + cn)
            fa, fb, fc = ft["a"][:, 0:cn], ft["b"][:, 0:cn], ft["c"][:, 0:cn]
            b = nb()
            k.mm(k.bank(b)[:, 0:cn], cm["blk64"][:], YS[:, sl], True, True, [], [k.bk(b)])
            k.stt("dve", fa, k.bank(b)[:, 0:cn], -1.0 / 64, YS[:, sl], ALU.mult, ALU.add, [k.bk(b)], ["ffa"])
            k.act(fb, fa, AF.Square, ["ffa"], ["ffb"])
            b = nb()
            k.mm(k.bank(b)[:, 0:cn], cm["blk64"][:], fb, True, True, ["ffb"], [k.bk(b)])
            k.act(fb, k.bank(b)[:, 0:cn], AF.Sqrt, [k.bk(b), ("rp", "gne")], ["ffb"], scale=1.0 / 64, bias=pv["gne"][:, 0:1])
            P.add("dve", lambda e, o=fb: e.reciprocal(out=o, in_=o), ["ffb"], ["ffb"])
            k.tt("pool", fa, fa, fb, ALU.mult, ["ffa", "ffb"], ["ffa"])
            k.ts("dve", fa, fa, pv["gn"][:, 0:1], pv["gn"][:, 1:2], ALU.mult, ALU.add, ["ffa", ("rp", "gn")], ["ffa"])
            k.tt("pool", fc, Aa[0][:, sl], Aa[1][:, sl], ALU.add, [], ["ffc"])
            k.ts("dve", fc, fc, pv["kvec"][:, 1:2], pv["omk"][:, 0:1], ALU.mult, ALU.add, ["ffc"], ["ffc"])
            k.stt("dve", fc, fc, pv["omk"][:, 0:1], Kx[:, sl], ALU.add, ALU.mult, ["ffc"], ["ffc"])
            k.stt("dve", fc, R[:, sl], pv["rk"][:, 0:1], fc, ALU.mult, ALU.mult, ["ffc", ("rp", "rk")], ["ffc"])
            b = nb()
            k.mm(k.bank(b)[:, 0:cn], cm["blk64"][:], fc, True, True, ["ffc"], [k.bk(b)])
            k.tt("dve", fb, k.bank(b)[:, 0:cn], V[:, sl], ALU.mult, [k.bk(b), "ffb"], ["ffb"])
            k.tt("pool", fa, fa, fb, ALU.add, ["ffa", "ffb"], ["ffa"])
            k.tt("dve", k.AB[:, hp, sl], fa, GA[:, sl], ALU.mult, ["ffa"], [("ABo", hp, c0)])
```

```python
import contextlib
import math
import numpy as np
import ml_dtypes
import concourse.bass as bass
import concourse.mybir as mybir
from concourse.bass_utils import run_bass_kernel_spmd

F32 = mybir.dt.float32
BF16 = mybir.dt.bfloat16
ALU = mybir.AluOpType
AF = mybir.ActivationFunctionType
AX = mybir.AxisListType

D = 1024
S = 2048
NCTX = 256
T = S + NCTX
NT = T // 128
DEPTH = 4
GRID_W = 64
HD = 64
ATT_IN = 2304
REC_IN = 4112
NEXP = 64
TOPK = 6
ROUTED_SCALE = 2.5
DN_ALPHA = (2 * DEPTH) ** 0.25
LN_EPS = 1e-5
NORM_EPS = 1e-6
RWKV_GN_EPS = 64e-5
NEG = -30000.0

CONST_NAMES = ("ident", "ropec", "ropes", "ropepm", "blk64", "ones", "m_le", "m_lt", "m_le128", "m_gt128")
NDMA_Q = {"sp": 16, "pool": 8}
SEM_EPOCH = 30000


class Op:
    __slots__ = ("eng", "fn", "dma", "deps", "needs_inc", "sem", "count", "slot", "emitted")

    def __init__(self, eng, fn, dma):
        self.eng = eng
        self.fn = fn
        self.dma = dma
        self.deps = []
        self.needs_inc = False
        self.sem = None
        self.count = 0
        self.slot = None
        self.emitted = False


class Prog:
    ENGS = ("pe", "dve", "act", "pool", "sp")

    def __init__(self, nc):
        self.nc = nc
        self.stack = contextlib.ExitStack()
        self.pending = {e: [] for e in self.ENGS}
        self.res = {}
        self.dma_last = {q: [None] * n for q, n in NDMA_Q.items()}
        self.dma_rr = {q: 0 for q in NDMA_Q}
        self.eng_obj = {"pe": nc.tensor, "dve": nc.vector, "act": nc.scalar,
                        "pool": nc.gpsimd, "sp": nc.sync}
        self.dma_sems = {q: [self.stack.enter_context(nc.semaphore(f"dq_{q}{i}")) for i in range(n)] for q, n in NDMA_Q.items()}
        self.eng_sem = {e: None for e in self.ENGS}
        self.eng_cnt = {e: 0 for e in self.ENGS}
        self.eng_nep = {e: 0 for e in self.ENGS}
        self.eng_last = {e: None for e in self.ENGS}
        self.waited = {e: {} for e in self.ENGS}
        self.out_dmas = []
        self.n_ops = 0

    def sbuf(self, name, shape, dtype, stack=None):
        self.n_alloc = getattr(self, "n_alloc", 0) + 1
        return (stack or self.stack).enter_context(self.nc.sbuf_tensor(f"{name}_{self.n_alloc}", list(shape), dtype))

    def psum(self, name, shape, dtype=F32):
        return self.stack.enter_context(self.nc.psum_tensor(name, list(shape), dtype))

    def add(self, eng, fn, reads=(), writes=(), dma=False):
        op = Op(eng, fn, dma)
        deps = {}
        for k in reads:
            r = self.res.get(k)
            if r is not None and r[0] is not None:
                deps[id(r[0])] = r[0]
            if r is not None and isinstance(k, tuple) and k and k[0] == "pb":
                for o in r[1]:
                    if o.eng != eng:
                        deps[id(o)] = o
        for k in writes:
            r = self.res.get(k)
            if r is not None:
                if r[0] is not None:
                    deps[id(r[0])] = r[0]
                for o in r[1]:
                    deps[id(o)] = o
        for k in reads:
            r = self.res.get(k)
            if r is None:
                r = [None, []]
                self.res[k] = r
            r[1].append(op)
        for k in writes:
            self.res[k] = [op, []]
        if dma:
            slot = self.dma_rr[eng]
            self.dma_rr[eng] = (slot + 1) % NDMA_Q[eng]
            prev = self.dma_last[eng][slot]
            op.slot = slot
            op.sem = self.dma_sems[eng][slot]
            op.count = (prev.count if prev is not None else 0) + 16
            if prev is not None:
                deps[id(prev)] = prev
            self.dma_last[eng][slot] = op
        deps.pop(id(op), None)
        for d in deps.values():
            if d.eng == "pe" and eng == "pe" and not d.dma and not dma:
                continue
            op.deps.append(d)
            d.needs_inc = True
        self.pending[eng].append(op)
        self.n_ops += 1
        return op

    def _wait(self, e, sem, count):
        w = self.waited[e]
        key = id(sem)
        if w.get(key, 0) >= count:
            return
        self.eng_obj[e].wait_ge(sem, count)
        w[key] = count

    def flush(self, barrier=True):
        nc = self.nc
        if barrier:
            for e in self.ENGS:
                for op in reversed(self.pending[e]):
                    if not op.dma:
                        op.needs_inc = True
                        break
        for e in self.ENGS:
            for op in self.pending[e]:
                if op.dma or not op.needs_inc:
                    continue
                if self.eng_sem[e] is None or self.eng_cnt[e] >= SEM_EPOCH:
                    self.eng_sem[e] = self.stack.enter_context(nc.semaphore(f"c_{e}_{self.eng_nep[e]}"))
                    self.eng_nep[e] += 1
                    self.eng_cnt[e] = 0
                self.eng_cnt[e] += 1
                op.sem = self.eng_sem[e]
                op.count = self.eng_cnt[e]
                self.eng_last[e] = op
        for e in self.ENGS:
            eng = self.eng_obj[e]
            for op in self.pending[e]:
                for d in op.deps:
                    self._wait(e, d.sem, d.count)
                ins = op.fn(eng)
                if op.dma:
                    ins.then_inc(op.sem, 16)
                elif op.needs_inc:
                    ins.then_inc(op.sem, 1)
                op.emitted = True
                op.fn = None
            self.pending[e] = []
        if barrier:
            for e in self.ENGS:
                for e2 in self.ENGS:
                    o = self.eng_last[e2]
                    if o is not None and not (e2 == e and e == "pe"):
                        self._wait(e, o.sem, o.count)
                for q in self.dma_last:
                    for o in self.dma_last[q]:
                        if o is not None:
                            self._wait(e, o.sem, o.count)
            self.res = {}

    @contextlib.contextmanager
    def phase(self):
        st = contextlib.ExitStack()
        try:
            yield st
            self.flush(barrier=True)
        finally:
            st.close()

    def finish(self):
        self.flush(barrier=True)
        self.stack.close()


def _bf(a):
    return np.ascontiguousarray(a.astype(ml_dtypes.bfloat16))


def host_constants():
    c = {}
    c["ident"] = np.eye(128, dtype=np.float32)
    t = np.arange(S)
    row = (t // GRID_W).astype(np.float64)
    col = (t % GRID_W).astype(np.float64)
    inv = 10000.0 ** (-np.arange(0, 32, 2, dtype=np.float64) / 32.0)
    ang = np.concatenate([row[:, None] * inv, col[:, None] * inv], -1)
    cosT = np.cos(ang).T
    sinT = np.sin(ang).T
    cos64 = np.repeat(cosT, 2, axis=0)
    sin64 = np.repeat(sinT, 2, axis=0)
    c["ropec"] = np.concatenate([cos64, cos64], 0).astype(np.float32)
    c["ropes"] = np.concatenate([sin64, sin64], 0).astype(np.float32)
    pm = np.zeros((128, 128), np.float32)
    for i in range(64):
        pm[2 * i + 1, 2 * i] = -1.0
        pm[2 * i, 2 * i + 1] = 1.0
    c["ropepm"] = pm
    bo = np.zeros((128, 128), np.float32)
    bo[:64, :64] = 1.0
    bo[64:, 64:] = 1.0
    c["blk64"] = bo
    c["ones"] = np.ones((128, 128), np.float32)
    s_ = np.arange(128)[:, None]
    t_ = np.arange(128)[None, :]
    same = (s_ // 64) == (t_ // 64)
    c["m_le"] = (same & (s_ <= t_)).astype(np.float32)
    c["m_lt"] = (same & (s_ < t_)).astype(np.float32)
    c["m_ge"] = (same & (s_ >= t_)).astype(np.float32)
    c["m_gt"] = (same & (s_ > t_)).astype(np.float32)
    c["m_le128"] = (s_ <= t_).astype(np.float32)
    c["m_ge128"] = (s_ >= t_).astype(np.float32)
    c["m_lt128"] = (s_ < t_).astype(np.float32)
    c["m_gt128"] = (s_ > t_).astype(np.float32)
    return c


def na_bias_tables(rpb):
    H = 8
    ext = np.concatenate([rpb.reshape(H, 15 * 31), np.full((H, 1), NEG, np.float32)], 1)
    cq = np.arange(64)[:, None]
    ck = np.arange(64)[None, :]
    cs = np.clip(cq - 8, 0, 48)
    col_in = (ck >= cs) & (ck < cs + 16)
    dcol = np.clip(ck - cq, -15, 15) + 15
    idxE = np.zeros((15, 64, 64), np.int64)
    for dr in range(15):
        idxE[dr] = np.where(col_in, dr * 31 + dcol, 465)
    tabE = np.zeros((128, 4, 15, 64), np.float32)
    tabO = np.zeros((128, 4, 10, 64), np.float32)
    mrow = np.full((64, 64), 465, np.int64)
    for h in range(H):
        pb = (h % 2) * 64
        hp = h // 2
        for dr in range(15):
            tabE[pb:pb + 64, hp, dr, :] = ext[h][idxE[dr]]
        tabO[pb:pb + 64, hp, 0, :] = ext[h][mrow]
        for q in range(8):
            tabO[pb:pb + 64, hp, 1 + q, :] = ext[h][idxE[3 + q]]
        tabO[pb:pb + 64, hp, 9, :] = ext[h][mrow]
    return tabE.reshape(128, 4 * 15 * 64), tabO.reshape(128, 4 * 10 * 64)


class K:
    def __init__(self, nc):
        self.nc = nc
        self.P = Prog(nc)
        self.bank_rr = 0

    def dma(self, eng, out, in_, reads, writes, **kw):
        return self.P.add(eng, lambda e: e.dma_start(out=out, in_=in_, **kw), reads, writes, dma=True)

    def mm(self, out, lhsT, rhs, start, stop, reads, writes):
        return self.P.add("pe", lambda e: e.matmul(out, lhsT=lhsT, rhs=rhs, start=start, stop=stop), reads, writes)

    def tr(self, out, in_, ident, reads, writes):
        return self.P.add("pe", lambda e: e.transpose(out, in_, ident), reads, writes)

    def act(self, out, in_, func, reads, writes, eng="act", **kw):
        return self.P.add("act", lambda e: e.activation(out=out, in_=in_, func=func, **kw), reads, writes)

    def tt(self, eng, out, in0, in1, op, reads, writes):
        return self.P.add(eng, lambda e: e.tensor_tensor(out=out, in0=in0, in1=in1, op=op), reads, writes)

    def ts(self, eng, out, in0, s1, s2, op0, op1, reads, writes, **kw):
        return self.P.add(eng, lambda e: e.tensor_scalar(out=out, in0=in0, scalar1=s1, scalar2=s2, op0=op0, op1=op1, **kw), reads, writes)

    def stt(self, eng, out, in0, scalar, in1, op0, op1, reads, writes):
        return self.P.add(eng, lambda e: e.scalar_tensor_tensor(out=out, in0=in0, scalar=scalar, in1=in1, op0=op0, op1=op1), reads, writes)

    def cp(self, eng, out, in_, reads, writes):
        if eng == "act":
            return self.P.add("act", lambda e: e.activation(out=out, in_=in_, func=AF.Copy), reads, writes)
        return self.P.add(eng, lambda e: e.tensor_copy(out=out, in_=in_), reads, writes)

    def bank(self, b):
        return self.pd[b // 2][:, (b % 2) * 512:(b % 2 + 1) * 512]

    def bk(self, b):
        return ("pb", b)


def bc_rows(ap1d, nparts):
    n = ap1d.shape[-1]
    return bass.AP(ap1d.tensor, ap1d.offset, [[0, nparts], [1, n]])


def build_program(n_layers=DEPTH, stop=None, dbg=False, start=0):
    nc = bass.Bass("TRN2", target_bir_lowering=False)
    NL = n_layers
    NLa = (n_layers + 1) // 2
    NLr = max(n_layers // 2, 1)
    k = K(nc)
    P = k.P

    def din(name, shape, dt=F32):
        return nc.dram_tensor(name, list(shape), dt, kind="ExternalInput").ap()

    I = {}
    I["x"] = din("x", [S, D]); I["ctx"] = din("ctx", [NCTX, D]); I["c2"] = din("c2", [2, D])
    I["ada_w"] = din("ada_w", [NL, D, 6 * D]); I["ada_b"] = din("ada_b", [NL, 6 * D])
    I["ln_g"] = din("ln_g", [NL, 2, D]); I["ln_b"] = din("ln_b", [NL, 2, D])
    I["mix_w_out"] = din("mix_w_out", [NL, D, D])
    I["att_w_in"] = din("att_w_in", [2, D, ATT_IN])
    I["na_tabE"] = din("na_tabE", [2, 128, 3840]); I["na_tabO"] = din("na_tabO", [2, 128, 2560])
    I["qk_gain"] = din("qk_gain", [2, 2, HD])
    I["rec_w_in"] = din("rec_w_in", [2, D, REC_IN])
    I["rwkv_mu"] = din("rwkv_mu", [2, 6, 512]); I["rwkv_w0"] = din("rwkv_w0", [2, 2, 512])
    I["rwkv_w1"] = din("rwkv_w1", [2, 2, 512, 32]); I["rwkv_w2"] = din("rwkv_w2", [2, 2, 32, 512])
    I["rwkv_a0"] = din("rwkv_a0", [2, 2, 512]); I["rwkv_a1"] = din("rwkv_a1", [2, 2, 512, 32])
    I["rwkv_a2"] = din("rwkv_a2", [2, 2, 32, 512]); I["rwkv_g1"] = din("rwkv_g1", [2, 512, 96])
    I["rwkv_g2"] = din("rwkv_g2", [2, 96, 512]); I["rwkv_kvec"] = din("rwkv_kvec", [2, 2, 512])
    I["rwkv_rk"] = din("rwkv_rk", [2, 512]); I["rwkv_gn"] = din("rwkv_gn", [2, 2, 512])
    I["mlstm_gate_b"] = din("mlstm_gate_b", [2, 16]); I["mlstm_norm"] = din("mlstm_norm", [2, 512])
    I["moe_router"] = din("moe_router", [NL, D, NEXP]); I["moe_bias"] = din("moe_bias", [NL, NEXP])
    I["moe_w1"] = din("moe_w1", [NL, NEXP, D, 256]); I["moe_w3"] = din("moe_w3", [NL, NEXP, D, 256])
    I["moe_w2"] = din("moe_w2", [NL, NEXP, 256, D])
    I["shared_w1"] = din("shared_w1", [NL, D, 256]); I["shared_w3"] = din("shared_w3", [NL, D, 256])
    I["shared_w2"] = din("shared_w2", [NL, 256, D])
    for cn in CONST_NAMES:
        shp = [128, 2048] if cn in ("ropec", "ropes") else [128, 128]
        I[cn] = din("c_" + cn, shp)
    out = nc.dram_tensor("out", [S, D], F32, kind="ExternalOutput").ap()
    k.Xd = nc.dram_tensor("Xd", [T, D], F32, kind="Internal").ap()
    k.modrow = nc.dram_tensor("modrow", [DEPTH, 2, 6 * D], F32, kind="Internal").ap()
    k.rkv_d = nc.dram_tensor("rkv_d", [3, 512, T], F32, kind="Internal").ap()
    k.dbg = nc.dram_tensor("dbg", [128, 8, T], BF16, kind="ExternalOutput").ap() if dbg else None
    k.I = I
    k.out = out

    k.identf = P.sbuf("identf", [128, 128], F32)
    k.identb = P.sbuf("identb", [128, 128], BF16)
    k.onesb = P.sbuf("onesb", [128, 128], BF16)
    k.epsln = P.sbuf("epsln", [128, 1], F32)
    k.epsnm = P.sbuf("epsnm", [128, 1], F32)
    k.modT = P.sbuf("modT", [128, DEPTH, 48, 2], F32)
    k.AB = P.sbuf("AB", [128, 8, T], BF16)
    k.Gtm = P.sbuf("Gtm", [128, NT, NEXP], F32)
    k.pd = [P.psum(f"pd{i}", [128, 1024], F32) for i in range(4)]

    k.dma("sp", k.identf[:], I["ident"], [], ["identf"])
    k.cp("dve", k.identb[:], k.identf[:], ["identf"], ["identb"])
    P.add("pool", lambda e: e.memset(k.onesb[:], 1.0), [], ["onesb"])
    P.add("pool", lambda e: e.memset(k.epsln[:], LN_EPS), [], ["epsln"])
    P.add("pool", lambda e: e.memset(k.epsnm[:], NORM_EPS), [], ["epsnm"])

    k.n_layers = n_layers
    k.start = start
    prologue(k, n_layers)
    if stop == ("pro", 0):
        o = k.dma("sp", out[0:12, :], k.modrow[0].rearrange("j (a b) -> (j a) b", b=1024), [], ["out"])
        P.out_dmas.append(o)
        n_layers = 0
    for i in range(start, n_layers):
        last = (i == DEPTH - 1)
        if i == start:
            phase1(k, i, ntile=(stop[1] if (stop and stop[0] in ("p1", "p1x") and stop[1] > 0) else NT))
            if stop and stop[0] == "p1x":
                phase1(k, 0, ntile=1) if False else None
                o = k.dma("pool", out[0:128, :].rearrange("p (k n) -> p k n", k=8), k.AB[:, :, 0:128], [], ["out"])
                P.out_dmas.append(o)
                break
            if dbg and stop[0] == "p1":
                k.dma("sp", k.dbg, k.AB[:], [], ["dbg"])
                break
        if i % 2 == 0:
            attention_layer(k, i)
        else:
            recurrent_layer(k, i)
        if dbg and stop == ("m", i):
            k.dma("sp", k.dbg, k.AB[:], [("AB", t) for t in range(NT)], ["dbg"])
            break
        post(k, i, 0, last)
        if stop == ("xa", i):
            break
        with P.phase() as st:
            k.Yacc = P.sbuf("Yacc", [128, NT, D], F32, st)
            moe(k, i, last, st)
            post(k, i, 1, last)
        if stop == ("xb", i):
            break
    if stop is not None and stop[0] in ("p1", "m"):
        o = k.dma("sp", out[0:12, :], k.modrow[0].rearrange("j (a b) -> (j a) b", b=1024), [], ["out"])
        P.out_dmas.append(o)
    if not (n_layers == DEPTH and stop is None) and (stop is None or stop[0] in ("xa", "xb")):
        o = k.dma("sp", out, k.Xd[0:S, :], [("Xd", t) for t in range(16)], ["out"])
        P.out_dmas.append(o)
    P.flush(barrier=True)
    for o in P.out_dmas:
        nc.sync.wait_ge(o.sem, o.count)
    P.stack.close()
    return nc


def prologue(k, n_layers):
    P, I = k.P, k.I
    with P.phase() as st:
        c2T = P.sbuf("c2T", [128, 8, 2], F32, st)
        sT = P.sbuf("sT", [128, 8, 2], F32, st)
        wch = [P.sbuf(f"adaw{j}", [128, 8, 512], F32, st) for j in range(2)]
        b2 = [P.sbuf(f"adab{j}", [2, 512], F32, st) for j in range(2)]
        mrow = [P.sbuf(f"mrow{j}", [2, 512], F32, st) for j in range(2)]
        for jj in range(2):
            k.dma("sp", c2T[:, :, jj], I["c2"][jj, :].rearrange("(k p) -> p k", p=128), [], [("c2T", jj)], allow_slow_non_contiguous=True)
        k.act(sT[:], c2T[:], AF.Silu, [("c2T", 0), ("c2T", 1)], ["sT"])
        n = 0
        for i in range(n_layers):
            for cc in range(12):
                j = n % 2
                n += 1
                cols = slice(cc * 512, (cc + 1) * 512)
                k.dma("sp" if n % 2 else "pool", wch[j][:], I["ada_w"][i, :, cols].rearrange("(k p) n -> p k n", p=128), [], [("wch", j)])
                k.dma("sp", b2[j][:], bc_rows(I["ada_b"][i, cols], 2), [], [("b2", j)])
                b0, b1 = 2 * j, 2 * j + 1
                for kk in range(8):
                    k.mm(k.bank(b0)[0:2, :], sT[:, kk, :], wch[j][:, kk, :], kk == 0, kk == 7,
                         ["sT", ("wch", j)], [k.bk(b0)])
                k.tt("dve", mrow[j][:], k.bank(b0)[0:2, :], b2[j][:], ALU.add, [k.bk(b0), ("b2", j)], [("mrow", j)])
                k.dma("sp", k.modrow[i, :, cols], mrow[j][:], [("mrow", j)], [("modrow", i, cc)])
                for q in range(4):
                    k.mm(k.bank(b1)[:, q * 2:(q + 1) * 2], mrow[j][0:2, q * 128:(q + 1) * 128], k.identf[0:2, 0:2],
                         True, True, [("mrow", j), "identf"], [k.bk(b1)])
                k.cp("act", k.modT[:, i, cc * 4:(cc + 1) * 4, :],
                     k.bank(b1)[:, 0:8].rearrange("p (q j) -> p q j", j=2), [k.bk(b1)], [("modT", i, cc)])
        for i in range(n_layers):
            for lo in (8, 32):
                cs = [("modT", i, cc) for cc in range(lo // 4, lo // 4 + 2)]
                k.ts("dve", k.modT[:, i, lo:lo + 8, :], k.modT[:, i, lo:lo + 8, :], 1.0, None, ALU.add, ALU.bypass, cs, cs)


def x_src(k, i, t, first):
    if first:
        return k.I["x"][t * 128:(t + 1) * 128, :] if t < 16 else k.I["ctx"][(t - 16) * 128:(t - 15) * 128, :]
    return k.Xd[t * 128:(t + 1) * 128, :]


def emit_hT(k, xt, xkey, i, scb, shb, t, hf=None, hfkey=None):
    j = 0 if t < 16 else 1
    for kq in range(2):
        b = 4 + (k.bank_rr % 4)
        k.bank_rr += 1
        for q in range(4):
            kk = kq * 4 + q
            k.tr(k.bank(b)[:, q * 128:(q + 1) * 128], xt[:, kk * 128:(kk + 1) * 128], k.identf[:], [xkey, "identf"], [k.bk(b)])
        for q in range(4):
            kk = kq * 4 + q
            sc = k.modT[:, i, scb + kk, j:j + 1]
            sh = k.modT[:, i, shb + kk, j:j + 1]
            src = k.bank(b)[:, q * 128:(q + 1) * 128]
            if hf is not None:
                dst = hf[:, kk, :]
                wk = [(hfkey, kk)]
            else:
                dst = k.AB[:, kk, t * 128:(t + 1) * 128]
                wk = [("AB", t, kk)]
            if b % 2 == 0:
                k.ts("dve", dst, src, sc, sh, ALU.mult, ALU.add, [k.bk(b)], wk)
            else:
                k.act(dst, src, AF.Identity, [k.bk(b)], wk, scale=sc, bias=sh)


def ABk(t):
    return [("AB", t, kk) for kk in range(8)]


def phase1(k, i, ntile=NT):
    P = k.P
    with P.phase() as st:
        xts = [P.sbuf(f"p1x{j}", [128, D], F32, st) for j in range(3)]
        for t in range(ntile):
            j = t % 3
            k.dma("sp", xts[j][:], x_src(k, i, t, True), [("Xd", t)], [("p1x", j)])
            emit_hT(k, xts[j], ("p1x", j), i, 8, 0, t)


def post(k, i, which, last):
    P, I = k.P, k.I
    ntile = 16 if last else NT
    first = (i == k.start and which == 0)
    with P.phase() as st:
        gb = P.sbuf("gb", [128, 2, D], F32, st)
        lng = P.sbuf("lng", [128, D], F32, st)
        lnb = P.sbuf("lnb", [128, D], F32, st)
        goff = 2 * D if which == 0 else 5 * D
        for j in range(2):
            k.dma("sp", gb[:, j, :], bc_rows(k.modrow[i, j, goff:goff + D], 128), [], [("gb", j)])
        k.dma("sp", lng[:], bc_rows(I["ln_g"][i, which, :], 128), [], ["lng"])
        k.dma("sp", lnb[:], bc_rows(I["ln_b"][i, which, :], 128), [], ["lnb"])
        if which == 0:
            Wo = P.sbuf("Wo", [128, 8, D], BF16, st)
            for h in range(2):
                k.dma("pool", Wo[:, h * 4:(h + 1) * 4], I["mix_w_out"][i, h * 512:(h + 1) * 512, :].rearrange("(k p) n -> p k n", p=128), [], [("Wo", h)])
            rw = P.sbuf("rw", [128, 8, NEXP], F32, st)
            k.dma("sp", rw[:], I["moe_router"][i].rearrange("(k p) e -> p k e", p=128), [], ["rw"])
            rb = P.sbuf("rb", [128, NEXP], F32, st)
            k.dma("sp", rb[:], bc_rows(I["moe_bias"][i], 128), [], ["rb"])
            hf = [P.sbuf(f"hf{r}", [128, 8, 128], F32, st) for r in range(2)]
            rt = {n: [P.sbuf(f"rt_{n}{r}", [128, w], F32, st) for r in range(2)]
                  for n, w in (("scs", 64), ("sel", 64), ("top8", 8), ("msk", 64), ("gs", 64), ("den", 1), ("rden", 1), ("G", 64))}
        xt = [P.sbuf(f"xt{r}", [128, D], F32, st) for r in range(2)]
        t1 = [P.sbuf(f"t1{r}", [128, D], F32, st) for r in range(2)]
        z = [P.sbuf(f"z{r}", [128, D], F32, st) for r in range(2)]
        xn = [P.sbuf(f"xn{r}", [128, D], F32, st) for r in range(2)]
        xo = [P.sbuf(f"xo{r}", [128, D], F32, st) for r in range(2)]
        bst = [P.sbuf(f"bst{r}", [128, 12], F32, st) for r in range(2)]
        mv = [P.sbuf(f"mv{r}", [128, 2], F32, st) for r in range(2)]
        sd = [P.sbuf(f"sd{r}", [128, 1], F32, st) for r in range(2)]
        rstd = [P.sbuf(f"rstd{r}", [128, 1], F32, st) for r in range(2)]
        k.bank_rr = 4

        def stage_a(t):
            j = 0 if t < 16 else 1
            r = t % 2
            tl = slice(t * 128, (t + 1) * 128)
            k.dma("sp", xt[r][:], x_src(k, i, t, first), [("Xd", t)], [("xt", r)])
            if which == 0:
                Yb = k.pd[r]
                yk = [k.bk(2 * r), k.bk(2 * r + 1)]
                for half in range(2):
                    for kk in range(8):
                        k.mm(Yb[:, half * 512:(half + 1) * 512], k.AB[:, kk, tl], Wo[:, kk, half * 512:(half + 1) * 512],
                             kk == 0, kk == 7, ABk(t) + [("Wo", kk // 4)], [yk[half]])
                k.tt("dve", t1[r][:], Yb[:], gb[:, j, :], ALU.mult, yk + [("gb", j)], [("t1", r)])
            else:
                k.tt("dve", t1[r][:], k.Yacc[:, t, :], gb[:, j, :], ALU.mult, [("Yacc", t), ("gb", j)], [("t1", r)])
            k.stt("dve", z[r][:], xt[r][:], float(DN_ALPHA), t1[r][:], ALU.mult, ALU.add, [("xt", r), ("t1", r)], [("z", r)])
            for h in range(2):
                P.add("dve", lambda e, r=r, h=h: e.bn_stats(out=bst[r][:, h * 6:(h + 1) * 6], in_=z[r][:, h * 512:(h + 1) * 512]),
                      [("z", r)], [("bst", r, h)])
            P.add("dve", lambda e, r=r: e.bn_aggr(out=mv[r][:], in_=bst[r][:]), [("bst", r, 0), ("bst", r, 1)], [("mv", r)])
            k.act(sd[r][:], mv[r][:, 1:2], AF.Sqrt, [("mv", r), "epsln"], [("sd", r)], bias=k.epsln[:, 0:1], scale=1.0)
            P.add("dve", lambda e, r=r: e.reciprocal(out=rstd[r][:], in_=sd[r][:]), [("sd", r)], [("rstd", r)])
            k.ts("dve", xn[r][:], z[r][:], mv[r][:, 0:1], rstd[r][:, 0:1], ALU.subtract, ALU.mult,
                 [("z", r), ("mv", r), ("rstd", r)], [("xn", r)])
            k.tt("pool", xn[r][:], xn[r][:], lng[:], ALU.mult, [("xn", r), "lng"], [("xn", r)])
            k.tt("pool", xo[r][:], xn[r][:], lnb[:], ALU.add, [("xn", r), "lnb"], [("xo", r)])

        def stage_b(t):
            j = 0 if t < 16 else 1
            r = t % 2
            tl = slice(t * 128, (t + 1) * 128)
            if last and which == 1:
                o = k.dma("sp", k.out[tl, :], xo[r][:], [("xo", r)], [("out", t)])
                P.out_dmas.append(o)
                return
            k.dma("sp", k.Xd[tl, :], xo[r][:], [("xo", r)], [("Xd", t)])
            if which == 1:
                if i + 1 < k.n_layers:
                    emit_hT(k, xo[r], ("xo", r), i + 1, 8, 0, t)
                return
            emit_hT(k, xo[r], ("xo", r), i, 32, 24, t, hf=hf[r], hfkey=("hf", r))
            hk = [(("hf", r), kk) for kk in range(8)]
            k.cp("pool", k.AB[:, :, tl], hf[r][:], hk, ABk(t))
            b = 4 + (k.bank_rr % 4)
            k.bank_rr += 1
            for kk in range(8):
                k.mm(k.bank(b)[:, 0:NEXP], hf[r][:, kk, :], rw[:, kk, :], kk == 0, kk == 7, hk + ["rw"], [k.bk(b)])
            R = {n: rt[n][r] for n in rt}
            K_ = lambda n: ("rt", n, r)
            k.act(R["scs"][:], k.bank(b)[:, 0:NEXP], AF.Sigmoid, [k.bk(b)], [K_("scs")])
            k.tt("dve", R["sel"][:], R["scs"][:], rb[:], ALU.add, [K_("scs"), "rb"], [K_("sel")])
            P.add("dve", lambda e, R=R: e.max(out=R["top8"][:], in_=R["sel"][:]), [K_("sel")], [K_("top8")])
            k.ts("dve", R["msk"][:], R["sel"][:], R["top8"][:, TOPK - 1:TOPK], None, ALU.is_ge, ALU.bypass, [K_("sel"), K_("top8")], [K_("msk")])
            k.tt("dve", R["gs"][:], R["scs"][:], R["msk"][:], ALU.mult, [K_("scs"), K_("msk")], [K_("gs")])
            P.add("dve", lambda e, R=R: e.reduce_sum(out=R["den"][:], in_=R["gs"][:], axis=AX.X), [K_("gs")], [K_("den")])
            P.add("dve", lambda e, R=R: e.reciprocal(out=R["rden"][:], in_=R["den"][:]), [K_("den")], [K_("rden")])
            k.ts("dve", R["G"][:], R["gs"][:], R["rden"][:, 0:1], float(ROUTED_SCALE), ALU.mult, ALU.mult, [K_("gs"), K_("rden")], [K_("G")])
            k.cp("pool", k.Gtm[:, t, :], R["G"][:], [K_("G")], [("Gtm", t)])

        stage_a(0)
        for t in range(ntile):
            if t + 1 < ntile:
                stage_a(t + 1)
            stage_b(t)


def moe(k, i, last, st_unused=None):
    P, I = k.P, k.I
    ntile = 16 if last else NT
    nchunk = ntile // 2
    G = 2
    groups = [list(range(g * G, (g + 1) * G)) for g in range(NEXP // G)] + [["s"]]
    with P.phase() as st:
        w1b = [P.sbuf(f"w1b{j}", [128, G, 8, 256], BF16, st) for j in range(2)]
        w3b = [P.sbuf(f"w3b{j}", [128, G, 8, 256], BF16, st) for j in range(2)]
        w2b = [P.sbuf(f"w2b{j}", [128, G, 2, D], BF16, st) for j in range(2)]
        s1 = [P.sbuf(f"ms1{j}", [128, 2, 256], BF16, st) for j in range(2)]
        u = [P.sbuf(f"mu{j}", [128, 2, 256], BF16, st) for j in range(2)]

        def load(gi):
            j = gi % 2
            for ei, e in enumerate(groups[gi]):
                s1_ = I["shared_w1"][i] if e == "s" else I["moe_w1"][i, e]
                s3_ = I["shared_w3"][i] if e == "s" else I["moe_w3"][i, e]
                s2_ = I["shared_w2"][i] if e == "s" else I["moe_w2"][i, e]
                k.dma("pool", w1b[j][:, ei], s1_.rearrange("(k p) f -> p k f", p=128), [], [("w1b", j, ei)])
                k.dma("pool", w3b[j][:, ei], s3_.rearrange("(k p) f -> p k f", p=128), [], [("w3b", j, ei)])
                k.dma("pool", w2b[j][:, ei], s2_.rearrange("(f p) n -> p f n", p=128), [], [("w2b", j, ei)])

        units = [(gi, ci, ei, e) for gi, grp in enumerate(groups) for ci in range(nchunk) for ei, e in enumerate(grp)]

        def up(n):
            gi, ci, ei, e = units[n]
            j, x, c0 = gi % 2, n % 2, ci * 256
            for (wb, bnk, nm) in ((w1b, 4 + x, "w1b"), (w3b, 6 + x, "w3b")):
                for f in range(2):
                    for kk in range(8):
                        k.mm(k.bank(bnk)[:, f * 256:(f + 1) * 256], wb[j][:, ei, kk, f * 128:(f + 1) * 128],
                             k.AB[:, kk, c0:c0 + 256], kk == 0, kk == 7, [(nm, j, ei)], [k.bk(bnk)])

        def rest(n):
            gi, ci, ei, e = units[n]
            j, x = gi % 2, n % 2
            k.act(s1[x][:], k.bank(4 + x)[:, 0:512].rearrange("p (f n) -> p f n", f=2), AF.Silu, [k.bk(4 + x)], [("ms1", x)])
            k.tt("dve", u[x][:], k.bank(6 + x)[:, 0:512].rearrange("p (f n) -> p f n", f=2), s1[x][:], ALU.mult,
                 [k.bk(6 + x), ("ms1", x)], [("mu", x)])
            for tt in range(2):
                t = ci * 2 + tt
                for half in range(2):
                    for f in range(2):
                        k.mm(k.pd[tt][:, half * 512:(half + 1) * 512], u[x][:, f, tt * 128:(tt + 1) * 128],
                             w2b[j][:, ei, f, half * 512:(half + 1) * 512], f == 0, f == 1, [("mu", x), ("w2b", j, ei)], [k.bk(2 * tt + half)])
                yk = [k.bk(2 * tt), k.bk(2 * tt + 1)]
                gsc = 1.0 if e == "s" else k.Gtm[:, t, e:e + 1]
                if gi == 0 and ei == 0:
                    k.ts("dve", k.Yacc[:, t, :], k.pd[tt][:], gsc, None, ALU.mult, ALU.bypass, yk, [("Yacc", t)])
                else:
                    k.stt("dve", k.Yacc[:, t, :], k.pd[tt][:], gsc, k.Yacc[:, t, :], ALU.mult, ALU.add, yk + [("Yacc", t)], [("Yacc", t)])

        load(0)
        up(0)
        for n in range(len(units)):
            gi, ci, ei, e = units[n]
            if ci == 0 and ei == 0 and gi + 1 < len(groups):
                load(gi + 1)
            if n + 1 < len(units):
                up(n + 1)
            rest(n)


def attention_layer(k, i):
    P, I = k.P, k.I
    j = i // 2
    keep_ctx = i < DEPTH - 1
    Win = I["att_w_in"][j]
    with P.phase() as sa:
        QTa = P.sbuf("QTa", [128, 4, T], BF16, sa)
        KTa = P.sbuf("KTa", [128, 4, T], BF16, sa)
        Va = P.sbuf("Va", [128, NT, 512], BF16, sa)
        QTb = P.sbuf("QTb", [128, 4, T], BF16, sa)
        KTb = P.sbuf("KTb", [128, 2, T], BF16, sa)
        Vb = P.sbuf("Vb", [128, NT, 128], BF16, sa)
        with P.phase() as st:
            W = P.sbuf("Win", [128, 8, ATT_IN], BF16, st)
            for kk in range(8):
                k.dma("pool", W[:, kk, :], Win[kk * 128:(kk + 1) * 128, :], [], [("W", kk)])
            ropec = P.sbuf("ropec", [128, S], F32, st)
            ropes = P.sbuf("ropes", [128, S], F32, st)
            pm = P.sbuf("ropepm", [128, 128], F32, st)
            blk = P.sbuf("blk64", [128, 128], F32, st)
            gq = P.sbuf("gq", [128, 1], F32, st)
            gk = P.sbuf("gk", [128, 1], F32, st)
            k.dma("sp", ropec[:], I["ropec"], [], ["ropec"])
            k.dma("sp", ropes[:], I["ropes"], [], ["ropes"])
            k.dma("sp", pm[:], I["ropepm"], [], ["pm"])
            k.dma("sp", blk[:], I["blk64"], [], ["blk"])
            for h in range(2):
                k.dma("sp", gq[h * 64:(h + 1) * 64, :], I["qk_gain"][j, 0, :].rearrange("(d o) -> d o", o=1), [], [("gq", h)], allow_slow_non_contiguous=True)
                k.dma("sp", gk[h * 64:(h + 1) * 64, :], I["qk_gain"][j, 1, :].rearrange("(d o) -> d o", o=1), [], [("gk", h)], allow_slow_non_contiguous=True)
            k.ts("dve", gq[:], gq[:], 0.125, None, ALU.mult, ALU.bypass, [("gq", 0), ("gq", 1)], [("gq", 0), ("gq", 1)])
            gqk = [("gq", 0), ("gq", 1)]
            gkk = [("gk", 0), ("gk", 1)]
            tmp = {n: [P.sbuf(f"at_{n}{r}", [128, 512], F32, st) for r in range(2)] for n in ("sq", "sd", "xg", "xn", "t1")}
            tmp["rstd"] = tmp["sd"]
            tmp["t2"] = tmp["sq"]
            Wk = [("W", kk) for kk in range(8)]
            nrot = 0
            brr = 0
            tcs = [(0, 512), (512, 512), (1024, 512), (1536, 512), (2048, 256)]
            items = [(c0, cn, cc) for (c0, cn) in tcs for cc in range(14)]
            st_ = {"brr": 0, "nrot": 0}

            def a_proj(n):
                c0, cn, cc = items[n]
                b = n % 4
                if cc >= 12:
                    for hh in range(2):
                        for kk in range(8):
                            lhs = W[:, kk, 2048 + (cc - 12) * 64:2048 + (cc - 11) * 64]
                            k.mm(k.bank(b)[hh * 64:(hh + 1) * 64, 0:cn], lhs, k.AB[:, kk, c0:c0 + cn], kk == 0, kk == 7, [("W", kk)], [k.bk(b)])
                for kk in range(8):
                    if cc < 4:
                        lhs = W[:, kk, cc * 128:(cc + 1) * 128]
                    elif cc < 8:
                        lhs = W[:, kk, 512 + (cc - 4) * 128:512 + (cc - 3) * 128]
                    elif cc < 12:
                        lhs = W[:, kk, 1536 + (cc - 8) * 128:1536 + (cc - 7) * 128]
                    else:
                        break
                    k.mm(k.bank(b)[:, 0:cn], lhs, k.AB[:, kk, c0:c0 + cn], kk == 0, kk == 7, [("W", kk)], [k.bk(b)])

            def a_chain(n):
                nonlocal_brr = st_
                c0, cn, cc = items[n]
                b = n % 4
                src = k.bank(b)[:, 0:cn]
                if cc < 4:
                    k.act(QTa[:, cc, c0:c0 + cn], src, AF.Copy, [k.bk(b)], [("QTa", cc, c0)], scale=0.125)
                    return
                if cc < 8:
                    k.cp("dve", KTa[:, cc - 4, c0:c0 + cn], src, [k.bk(b)], [("KTa", cc, c0)])
                    return
                is_q = cc < 12
                dest = QTb[:, cc - 8, c0:c0 + cn] if is_q else KTb[:, cc - 12, c0:c0 + cn]
                dk = [("QKb", cc, c0)]
                gain, gkeys = (gq, gqk) if is_q else (gk, gkk)
                r = st_['nrot'] % 2
                st_['nrot'] += 1
                tk = lambda n: ("at", {"rstd": "sd", "t2": "sq"}.get(n, n), r)
                tv = lambda n: tmp[n][r][:, 0:cn]
                k.act(tv("sq"), src, AF.Square, [k.bk(b)], [tk("sq")])
                b2 = 4 + st_['brr'] % 4
                st_['brr'] += 1
                k.mm(k.bank(b2)[:, 0:cn], blk[:], tv("sq"), True, True, [tk("sq"), "blk"], [k.bk(b2)])
                k.act(tv("sd"), k.bank(b2)[:, 0:cn], AF.Sqrt, [k.bk(b2), "epsnm"], [tk("sd")], scale=1.0 / 64, bias=k.epsnm[:, 0:1])
                P.add("dve", lambda e, o=tv("rstd"), a=tv("sd"): e.reciprocal(out=o, in_=a), [tk("sd")], [tk("sd")])
                k.act(tv("xg"), src, AF.Identity, [k.bk(b)] + gkeys, [tk("xg")], scale=gain[:, 0:1])
                if c0 < S:
                    k.tt("pool", tv("xn"), tv("xg"), tv("rstd"), ALU.mult, [tk("xg"), tk("rstd")], [tk("xn")])
                    b3 = 4 + st_['brr'] % 4
                    st_['brr'] += 1
                    k.mm(k.bank(b3)[:, 0:cn], pm[:], tv("xn"), True, True, [tk("xn"), "pm"], [k.bk(b3)])
                    k.tt("pool", tv("t1"), tv("xn"), ropec[:, c0:c0 + cn], ALU.mult, [tk("xn"), "ropec"], [tk("t1")])
                    k.tt("dve", tv("t2"), k.bank(b3)[:, 0:cn], ropes[:, c0:c0 + cn], ALU.mult, [k.bk(b3), "ropes"], [tk("t2")])
                    k.tt("pool", dest, tv("t1"), tv("t2"), ALU.add, [tk("t1"), tk("t2")], dk)
                else:
                    k.tt("pool", dest, tv("xg"), tv("rstd"), ALU.mult, [tk("xg"), tk("rstd")], dk)

            a_proj(0)
            for n in range(len(items)):
                if n + 1 < len(items):
                    a_proj(n + 1)
                a_chain(n)
            for t in range(NT):
                tl = slice(t * 128, (t + 1) * 128)
                b = brr % 8
                brr += 1
                for kk in range(8):
                    k.mm(k.bank(b)[:, 0:512], k.AB[:, kk, tl], W[:, kk, 1024:1536], kk == 0, kk == 7, [("W", kk)], [k.bk(b)])
                k.cp("act", Va[:, t, :], k.bank(b)[:, 0:512], [k.bk(b)], [("Va", t)])
                b = brr % 8
                brr += 1
                for kk in range(8):
                    k.mm(k.bank(b)[:, 0:128], k.AB[:, kk, tl], W[:, kk, 2176:2304], kk == 0, kk == 7, [("W", kk)], [k.bk(b)])
                k.cp("dve", Vb[:, t, :], k.bank(b)[:, 0:128], [k.bk(b)], [("Vb", t)])
        with P.phase() as st:
            tabE = P.sbuf("tabE", [128, 3840], F32, st)
            tabO = P.sbuf("tabO", [128, 2560], F32, st)
            k.dma("sp", tabE[:], I["na_tabE"][j], [], ["tabE"])
            k.dma("sp", tabO[:], I["na_tabO"][j], [], ["tabO"])
            Sb = [P.sbuf(f"naS{r}", [128, 896], F32, st) for r in range(2)]
            Pe = [P.sbuf(f"naP{r}", [128, 896], BF16, st) for r in range(2)]
            Pn = [P.sbuf(f"naN{r}", [128, 896], BF16, st) for r in range(2)]
            PT = [P.sbuf(f"naT{r}", [128, 896], BF16, st) for r in range(2)]
            sm = {n: [P.sbuf(f"na_{n}{r}", [128, 1], F32, st) for r in range(2)] for n in ("mx", "nmx", "rs", "ri")}
            units = []
            for r_ in range(32):
                sr = min(max(r_ - 4, 0), 24)
                if sr % 2 == 0:
                    a0, nrow, odd, d0 = sr, 8, False, sr - r_ + 7
                else:
                    a0, nrow, odd, d0 = sr - 1, 10, True, 0
                units.append((r_ * 64, a0 * 64, nrow * 64, odd, d0))
            if keep_ctx:
                for cq in range(4):
                    units.append((S + cq * 64, 0, 0, False, 0))
            ulist = [(q0, k0, nlat, odd, d0, hp) for (q0, k0, nlat, odd, d0) in units for hp in range(4)]

            def na_s(n):
                q0, k0, nlat, odd, d0, hp = ulist[n]
                nk = nlat + NCTX
                r = n % 2
                kS = ("naS", r)
                for hh in range(2):
                    ps = slice(hh * 64, (hh + 1) * 64)
                    bA, bB = 2 * hh, 2 * hh + 1
                    q = QTa[ps, hp, q0:q0 + 64]
                    if nlat:
                        k.mm(k.bank(bA)[ps, 0:512], q, KTa[ps, hp, k0:k0 + 512], True, True, [], [k.bk(bA)])
                        if nlat > 512:
                            k.mm(k.bank(bB)[ps, 0:128], q, KTa[ps, hp, k0 + 512:k0 + 640], True, True, [], [k.bk(bB)])
                        xo_ = nlat - 512
                        k.mm(k.bank(bB)[ps, xo_:xo_ + NCTX], q, KTa[ps, hp, S:T], True, True, [], [k.bk(bB)])
                        if odd:
                            bias = tabO[ps, hp * 640:(hp + 1) * 640]
                        else:
                            bias = tabE[ps, (hp * 15 + d0) * 64:(hp * 15 + d0 + 8) * 64]
                        k.tt("dve", Sb[r][ps, 0:512], k.bank(bA)[ps, 0:512], bias[:, 0:512], ALU.add,
                             [k.bk(bA), "tabE", "tabO"], [(kS, hh, 0)])
                        if nlat > 512:
                            k.tt("dve", Sb[r][ps, 512:640], k.bank(bB)[ps, 0:128], bias[:, 512:640], ALU.add,
                                 [k.bk(bB), "tabO"], [(kS, hh, 1)])
                        k.cp("dve", Sb[r][ps, nlat:nk], k.bank(bB)[ps, xo_:xo_ + NCTX], [k.bk(bB)], [(kS, hh, 2)])
                    else:
                        k.mm(k.bank(bA)[ps, 0:NCTX], q, KTa[ps, hp, S:T], True, True, [], [k.bk(bA)])
                        k.cp("act", Sb[r][ps, 0:NCTX], k.bank(bA)[ps, 0:NCTX], [k.bk(bA)], [(kS, hh, 2)])

            def na_rest(n):
                q0, k0, nlat, odd, d0, hp = ulist[n]
                nk = nlat + NCTX
                r = n % 2
                kS, kP, kN, kT = ("naS", r), ("naP", r), ("naN", r), ("naT", r)
                sk = [(kS, hh, x) for hh in range(2) for x in range(3)]
                M = {nm: sm[nm][r] for nm in sm}
                mk = lambda nm: ("nasm", nm, r)
                P.add("dve", lambda e, o=M["mx"], a=Sb[r], nk=nk: e.reduce_max(out=o[:], in_=a[:, 0:nk], axis=AX.X), sk, [mk("mx")])
                k.ts("dve", M["nmx"][:], M["mx"][:], -1.0, None, ALU.mult, ALU.bypass, [mk("mx")], [mk("nmx")])
                P.add("act", lambda e, o=Pe[r], a=Sb[r], nk=nk, nm=M["nmx"], rs=M["rs"]: e.activation(
                    out=o[:, 0:nk], in_=a[:, 0:nk], func=AF.Exp, bias=nm[:, 0:1], scale=1.0, accum_out=rs[:, 0:1]),
                    sk + [mk("nmx")], [kP, mk("rs")])
                P.add("dve", lambda e, o=M["ri"], a=M["rs"]: e.reciprocal(out=o[:], in_=a[:]), [mk("rs")], [mk("ri")])
                k.ts("dve", Pn[r][:, 0:nk], Pe[r][:, 0:nk], M["ri"][:, 0:1], None, ALU.mult, ALU.bypass, [kP, mk("ri")], [kN])
                nch = nk // 128
                bT = 4 + (n % 2)
                ptb = k.bank(bT).bitcast(BF16)
                for c in range(nch):
                    k.tr(ptb[:, c * 128:(c + 1) * 128], Pn[r][:, c * 128:(c + 1) * 128], k.identb[:], [kN], [k.bk(bT)])
                k.cp("act", PT[r][:, 0:nk], ptb[:, 0:nk], [k.bk(bT)], [kT])
                bO = 6 + (n % 2)
                for hh in range(2):
                    ps = slice(hh * 64, (hh + 1) * 64)
                    h = 2 * hp + hh
                    for c in range(nch):
                        if c < nlat // 128:
                            vt = k0 // 128 + c
                        else:
                            vt = 16 + (c - nlat // 128)
                        k.mm(k.bank(bO)[ps, 0:64], Va[:, vt, h * 64:(h + 1) * 64], PT[r][:, c * 128 + hh * 64:c * 128 + hh * 64 + 64],
                             c == 0, c == nch - 1, [kT], [k.bk(bO)])
                k.cp("act", k.AB[:, hp, q0:q0 + 64], k.bank(bO)[:, 0:64], [k.bk(bO)], [("mT", hp, q0)])

            na_s(0)
            for n in range(len(ulist)):
                if n + 1 < len(ulist):
                    na_s(n + 1)
                na_rest(n)
        with P.phase() as st:
            E = [[P.sbuf(f"gqE{r}{hh}", [128, 512], BF16, st) for hh in range(2)] for r in range(2)]
            rc = P.sbuf("gqrc", [128, 512], F32, st)
            gunits = [(hp, c * 512, 512, list(range(NT))) for hp in range(4) for c in range(4)]
            if keep_ctx:
                gunits += [(hp, S, NCTX, [16, 17]) for hp in range(4)]
            n = 0
            for (hp, q0, nq, kts) in gunits:
                g = hp // 2
                def gq_s(ki, r):
                    kt = kts[ki]
                    for hh in range(2):
                        ps = slice(hh * 64, (hh + 1) * 64)
                        bS = 2 * r + hh
                        k.mm(k.bank(bS)[:, 0:nq], KTb[ps, g, kt * 128:(kt + 1) * 128], QTb[ps, hp, q0:q0 + nq], True, True, [], [k.bk(bS)])
                        k.act(E[r][hh][:, 0:nq], k.bank(bS)[:, 0:nq], AF.Exp, [k.bk(bS)], [("gqE", r, hh)])

                def gq_pv(ki, r):
                    kt = kts[ki]
                    for hh in range(2):
                        ps = slice(hh * 64, (hh + 1) * 64)
                        k.mm(k.bank(4 + hh)[ps, 0:nq], Vb[:, kt, g * 64:(g + 1) * 64], E[r][hh][:, 0:nq], ki == 0, ki == len(kts) - 1,
                             [("gqE", r, hh)], [k.bk(4 + hh)])
                        k.mm(k.bank(6 + hh)[ps, 0:nq], k.onesb[:, 0:64], E[r][hh][:, 0:nq], ki == 0, ki == len(kts) - 1,
                             [("gqE", r, hh), "onesb"], [k.bk(6 + hh)])

                gq_s(0, n % 2)
                for ki in range(len(kts)):
                    r = n % 2
                    n += 1
                    if ki + 1 < len(kts):
                        gq_s(ki + 1, n % 2)
                    gq_pv(ki, r)
                for hh in range(2):
                    ps = slice(hh * 64, (hh + 1) * 64)
                    P.add("dve", lambda e, o=rc[ps, 0:nq], a=k.bank(6 + hh)[ps, 0:nq]: e.reciprocal(out=o, in_=a), [k.bk(6 + hh)], [("gqrc", hh)])
                    k.tt("dve", k.AB[ps, 4 + hp, q0:q0 + nq], k.bank(4 + hh)[ps, 0:nq], rc[ps, 0:nq], ALU.mult,
                         [k.bk(4 + hh), ("gqrc", hh)], [("mT", 4 + hp, q0, hh)])


_CACHE = {}


def make_in_maps(inputs):
    f = lambda a: np.ascontiguousarray(np.asarray(a, dtype=np.float32))
    shared = {}
    for n in ("ada_w", "ada_b", "ln_g", "ln_b", "mix_w_out", "att_w_in", "qk_gain", "rec_w_in", "rwkv_mu", "rwkv_w0",
              "rwkv_w1", "rwkv_w2", "rwkv_a0", "rwkv_a1", "rwkv_a2", "rwkv_g1", "rwkv_g2", "rwkv_kvec", "rwkv_gn",
              "mlstm_norm", "moe_router", "moe_bias", "moe_w1", "moe_w3", "moe_w2", "shared_w1", "shared_w3", "shared_w2"):
        shared[n] = f(inputs[n])
    shared["rwkv_rk"] = f(inputs["rwkv_rk"]).reshape(2, 512)
    shared["mlstm_gate_b"] = f(inputs["mlstm_gate_b"]).reshape(2, 16)
    rpb = f(inputs["na_rpb"])
    tabs = [na_bias_tables(rpb[j]) for j in range(rpb.shape[0])]
    shared["na_tabE"] = np.ascontiguousarray(np.stack([t[0] for t in tabs]))
    shared["na_tabO"] = np.ascontiguousarray(np.stack([t[1] for t in tabs]))
    hc = host_constants()
    for cn in CONST_NAMES:
        shared["c_" + cn] = np.ascontiguousarray(hc[cn])
    x = f(inputs["x"]); c = f(inputs["c"]); ctx = f(inputs["ctx"]); c_ctx = f(inputs["c_ctx"])
    maps = []
    for b in range(x.shape[0]):
        m = dict(shared)
        m["x"] = np.ascontiguousarray(x[b])
        m["ctx"] = np.ascontiguousarray(ctx[b])
        m["c2"] = np.ascontiguousarray(np.stack([c[b], c_ctx]))
        maps.append(m)
    return maps


def kernel(**inputs):
    maps = make_in_maps(inputs)
    if "nc" not in _CACHE:
        _CACHE["nc"] = build_program()
    nc = _CACHE["nc"]
    res = run_bass_kernel_spmd(nc, maps, core_ids=list(range(len(maps))))
    return np.stack([np.asarray(r["out"], dtype=np.float32) for r in res.results])


def tokview(t2d, row_elems, base, start, step, n, parts=128, p0=0):
    return bass.AP(t2d, p0 * row_elems + base + start, [[row_elems, parts], [step, n]])


def proc_tiles(d):
    if d == 0:
        return [(t * 128, 1) for t in (16, 17)] + [(t * 128, 1) for t in range(16)]
    return [(T - 1 - u * 128, -1) for u in range(NT)]


def recurrent_layer(k, i):
    P, I = k.P, k.I
    with P.phase() as sl:
        mTt = P.sbuf("mTt", [128, 8, T], BF16, sl) if False else None
        mTm = P.sbuf("mTm", [128, 4, T], BF16, sl)
        cm = {}
        for cn in ("m_le", "m_lt", "m_le128", "m_gt128", "ones", "blk64"):
            cm[cn] = P.sbuf("c" + cn, [128, 128], F32, sl)
            k.dma("sp", cm[cn][:], I[cn], [], [("cm", cn)])
        P.flush(barrier=True)
        mlstm_mixer(k, i, mTm, cm)
        rwkv_mixer(k, i, cm)
        with P.phase():
            for c in range(4):
                k.cp("pool" if c % 2 else "dve", k.AB[:, 4 + c, :], mTm[:, c, :], [], [("AB", 4 + c)])


def mlstm_mixer(k, i, mTt, cm):
    P, I = k.P, k.I
    j = i // 2
    Wr = I["rec_w_in"][j]
    ABt = k.AB
    RE = 8 * T
    with P.phase() as sm:
        Wg = P.sbuf("Wg", [128, 8, 16], BF16, sm)
        k.dma("pool", Wg[:], Wr[:, 4096:4112].rearrange("(k p) n -> p k n", p=128), [], ["Wg"])
        gb = P.sbuf("gateb", [128, 16], F32, sm)
        k.dma("sp", gb[:], bc_rows(I["mlstm_gate_b"][j, :], 128), [], ["gateb"])
        ng = P.sbuf("normg", [128, 4], F32, sm)
        k.dma("sp", ng[:], I["mlstm_norm"][j, :].rearrange("(h p) -> p h", p=128), [], ["normg"], allow_slow_non_contiguous=True)
        IG = [P.sbuf(f"IG{d}", [128, NT, 4], F32, sm) for d in range(2)]
        LF = [P.sbuf(f"LF{d}", [128, NT, 4], F32, sm) for d in range(2)]
        WI = [P.sbuf(f"WI{d}", [128, NT, 4], F32, sm) for d in range(2)]
        WS = [P.sbuf(f"WS{d}", [128, NT, 4], F32, sm) for d in range(2)]
        WC = [P.sbuf(f"WC{d}", [128, NT, 4], F32, sm) for d in range(2)]
        gt = [P.sbuf(f"gtmp{r}", [128, 8], F32, sm) for r in range(2)]
        hTr = P.sbuf("hTr", [128, 8, T], BF16, sm)
        for kk in range(8):
            k.cp("pool" if kk % 2 else "dve", hTr[:, kk, :], tokview(ABt, RE, kk * T, T - 1, -1, T), [], [("hTr", kk)])
        hkeys = [("hTr", kk) for kk in range(8)]

        def hview(d, u, st0, kk):
            return k.AB[:, kk, st0:st0 + 128] if d == 0 else hTr[:, kk, u * 128:(u + 1) * 128]
        n = 0
        for d in range(2):
            for u, (st0, step) in enumerate(proc_tiles(d)):
                r = n % 2
                b = n % 8
                n += 1
                for kk in range(8):
                    k.mm(k.bank(b)[:, 0:16], hview(d, u, st0, kk), Wg[:, kk, :], kk == 0, kk == 7, ["Wg"] + hkeys, [k.bk(b)])
                c0 = 8 * d
                k.tt("dve", IG[d][:, u, :], k.bank(b)[:, c0:c0 + 4], gb[:, c0:c0 + 4], ALU.add, [k.bk(b), "gateb"], [("IG", d, u)])
                k.tt("dve", gt[r][:, 0:4], k.bank(b)[:, c0 + 4:c0 + 8], gb[:, c0 + 4:c0 + 8], ALU.add, [k.bk(b), "gateb"], [("gt", r)])
                k.act(gt[r][:, 4:8], gt[r][:, 0:4], AF.Exp, [("gt", r)], [("gt2", r)], scale=-1.0)
                k.act(gt[r][:, 0:4], gt[r][:, 4:8], AF.Ln, [("gt2", r), ("gt", r)], [("gt", r)], bias=1.0, scale=1.0)
                k.ts("dve", LF[d][:, u, :], gt[r][:, 0:4], -1.0, None, ALU.mult, ALU.bypass, [("gt", r)], [("LF", d, u)])
                b2 = n % 8
                n += 1
                k.mm(k.bank(b2)[:, 0:4], cm["m_le128"][:], LF[d][:, u, :], True, True, [("LF", d, u), ("cm", "m_le128")], [k.bk(b2)])
                k.mm(k.bank(b2)[:, 4:8], cm["ones"][:], LF[d][:, u, :], True, True, [("LF", d, u), ("cm", "ones")], [k.bk(b2)])
                k.act(WI[d][:, u, :], k.bank(b2)[:, 0:4], AF.Exp, [k.bk(b2)], [("WI", d, u)])
                k.act(WC[d][:, u, :], k.bank(b2)[:, 4:8], AF.Exp, [k.bk(b2)], [("WC", d, u)])
                k.act(gt[r][:, 4:8], k.bank(b2)[:, 4:8], AF.Copy, [k.bk(b2), ("gt2", r)], [("gt2", r)])
                k.act(gt[r][:, 0:4], k.bank(b2)[:, 0:4], AF.Copy, [k.bk(b2), ("gt", r)], [("gt", r)])
                k.tt("dve", gt[r][:, 4:8], gt[r][:, 4:8], gt[r][:, 0:4], ALU.subtract, [("gt", r), ("gt2", r)], [("gt2", r)])
                k.tt("dve", gt[r][:, 4:8], gt[r][:, 4:8], IG[d][:, u, :], ALU.add, [("gt2", r), ("IG", d, u)], [("gt2", r)])
                k.act(WS[d][:, u, :], gt[r][:, 4:8], AF.Exp, [("gt2", r)], [("WS", d, u)])
        P.flush(barrier=True)
        for h in range(4):
            with P.phase() as st:
                Wq = P.sbuf("mWq", [128, 8, 128], BF16, st)
                Wk = P.sbuf("mWk", [128, 8, 128], BF16, st)
                Wv = P.sbuf("mWv", [128, 8, 128], BF16, st)
                Wo = P.sbuf("mWo", [128, 8, 128], BF16, st)
                for (w, off, nm) in ((Wq, 2048, "q"), (Wk, 2560, "k"), (Wv, 3072, "v"), (Wo, 3584, "o")):
                    k.dma("pool", w[:], Wr[:, off + h * 128:off + (h + 1) * 128].rearrange("(k p) n -> p k n", p=128), [], [("mW", nm)])
                QT = P.sbuf("mQT", [128, T], BF16, st)
                KT = P.sbuf("mKT", [128, T], BF16, st)
                SO = P.sbuf("mSO", [128, T], BF16, st)
                HS = P.sbuf("mHS", [128, T], F32, st)
                Kt = [P.sbuf(f"mKt{d}", [128, NT, 128], BF16, st) for d in range(2)]
                Vt = [P.sbuf(f"mVt{d}", [128, NT, 129], BF16, st) for d in range(2)]
                Cs = [P.sbuf(f"mC{d}", [128, 129], F32, st) for d in range(2)]
                Cbs = [P.sbuf(f"mCb{d}", [128, 129], BF16, st) for d in range(2)]
                tm = {nm: [P.sbuf(f"mt_{nm}{r}", [128, 132], F32, st) for r in range(2)] for nm in ("R", "eD", "eDm", "tmp", "num", "hq")}
                tb = {nm: [P.sbuf(f"mtb_{nm}{r}", [128, 128], BF16, st) for r in range(2)] for nm in ("Sg", "Kw")}
                sm1 = {nm: [P.sbuf(f"ms_{nm}{r}", [128, 1], F32, st) for r in range(2)] for nm in ("dn", "rdn")}
                fz = {nm: P.sbuf(f"mf_{nm}", [128, 512], F32, st) for nm in ("sq", "sd", "o1")}
                brr = 0
                for (c0, cn) in [(0, 512), (512, 512), (1024, 512), (1536, 512), (2048, 256)]:
                    for (w, nm) in ((Wq, "q"), (Wk, "k"), (Wo, "o")):
                        b = brr % 8
                        brr += 1
                        for kk in range(8):
                            k.mm(k.bank(b)[:, 0:cn], w[:, kk, :], k.AB[:, kk, c0:c0 + cn], kk == 0, kk == 7, [("mW", nm)], [k.bk(b)])
                        if nm == "q":
                            k.act(QT[:, c0:c0 + cn], k.bank(b)[:, 0:cn], AF.Copy, [k.bk(b)], [("mQT", c0)], scale=float(128 ** -0.5))
                        elif nm == "k":
                            k.cp("dve", KT[:, c0:c0 + cn], k.bank(b)[:, 0:cn], [k.bk(b)], [("mKT", c0)])
                        else:
                            k.act(SO[:, c0:c0 + cn], k.bank(b)[:, 0:cn], AF.Sigmoid, [k.bk(b)], [("mSO", c0)])
                for d in range(2):
                    P.add("pool", lambda e, d=d: e.memset(Vt[d][:, :, 128:129], 1.0), [], [("mVt1", d)])
                    for u, (st0, step) in enumerate(proc_tiles(d)):
                        for (w, nm) in ((Wk, "k"), (Wv, "v")):
                            b = brr % 8
                            brr += 1
                            for kk in range(8):
                                k.mm(k.bank(b)[:, 0:128], hview(d, u, st0, kk), w[:, kk, :], kk == 0, kk == 7, [("mW", nm)], [k.bk(b)])
                            if nm == "k":
                                k.cp("act", Kt[d][:, u, :], k.bank(b)[:, 0:128], [k.bk(b)], [("mKt", d, u)])
                            else:
                                k.cp("dve", Vt[d][:, u, 0:128], k.bank(b)[:, 0:128], [k.bk(b)], [("mVt", d, u)])
                qk_keys = [("mQT", c0) for c0 in (0, 512, 1024, 1536, 2048)] + [("mKT", c0) for c0 in (0, 512, 1024, 1536, 2048)]
                QTr = P.sbuf("mQTr", [128, T], BF16, st)
                KTr = P.sbuf("mKTr", [128, T], BF16, st)
                k.cp("pool", QTr[:], tokview(QT, T, 0, T - 1, -1, T), qk_keys, ["mQTr"])
                k.cp("pool", KTr[:], tokview(KT, T, 0, T - 1, -1, T), qk_keys, ["mKTr"])
                qk_keys = qk_keys + ["mQTr", "mKTr"]
                P.add("pool", lambda e: e.memset(HS[:], 0.0), [], [("mHS", t_) for t_ in range(NT)])
                for d in range(2):
                    P.add("dve", lambda e, d=d: e.memset(Cs[d][:], 0.0), [], [("mC", d)])
                    P.add("pool", lambda e, d=d: e.memset(Cbs[d][:], 0.0), [], [("mCb", d)])
                ptl = [proc_tiles(0), proc_tiles(1)]
                for u in range(NT):
                    for d in range(2):
                        st0, step = ptl[d][u]
                        r = d
                        C, Cb = Cs[d], Cbs[d]
                        stile = (st0 // 128) if d == 0 else (NT - 1 - u)
                        tk = lambda nm, r=r: ("mt", nm, r)
                        if d == 0:
                            qv, kv = QT[:, st0:st0 + 128], KT[:, st0:st0 + 128]
                        else:
                            qv, kv = QTr[:, u * 128:(u + 1) * 128], KTr[:, u * 128:(u + 1) * 128]
                        bS, bD, bN, bI = 4 * d, 4 * d + 1, 4 * d + 2, 4 * d + 3
                        bT, bC = bD, bS
                        k.mm(k.bank(bS)[:, 0:128], kv, qv, True, True, qk_keys, [k.bk(bS)])
                        k.ts("dve", tm["R"][r][:, 0:128], cm["m_le128"][:], LF[d][:, u, h:h + 1], None, ALU.mult, ALU.bypass, [], [tk("R")])
                        k.mm(k.bank(bD)[:, 0:128], cm["m_gt128"][:], tm["R"][r][:, 0:128], True, True, [tk("R")], [k.bk(bD)])
                        k.act(tm["eD"][r][:, 0:128], k.bank(bD)[:, 0:128], AF.Exp, [k.bk(bD)], [tk("eD")], bias=IG[d][:, u, h:h + 1], scale=1.0)
                        k.tt("pool", tm["eDm"][r][:, 0:128], tm["eD"][r][:, 0:128], cm["m_le128"][:], ALU.mult, [tk("eD")], [tk("eDm")])
                        k.tt("dve", tb["Sg"][r][:], k.bank(bS)[:, 0:128], tm["eDm"][r][:, 0:128], ALU.mult, [k.bk(bS), tk("eDm")], [tk("Sg")])
                        k.mm(k.bank(bN)[:, 0:129], tb["Sg"][r][:], Vt[d][:, u, :], True, True, [tk("Sg"), ("mVt", d, u), ("mVt1", d)], [k.bk(bN)])
                        k.mm(k.bank(bI)[:, 0:129], qv, Cb[:], True, True, qk_keys + [("mCb", d)], [k.bk(bI)])
                        k.ts("dve", tm["tmp"][r][:, 0:129], k.bank(bI)[:, 0:129], WI[d][:, u, h:h + 1], None, ALU.mult, ALU.bypass, [k.bk(bI)], [tk("tmp")])
                        k.tt("dve", tm["num"][r][:, 0:129], k.bank(bN)[:, 0:129], tm["tmp"][r][:, 0:129], ALU.add, [k.bk(bN), tk("tmp")], [tk("num")])
                        k.act(sm1["dn"][r][:], tm["num"][r][:, 128:129], AF.Abs, [tk("num")], [tk("dn")])
                        k.ts("dve", sm1["dn"][r][:], sm1["dn"][r][:], 1.0, None, ALU.max, ALU.bypass, [tk("dn")], [tk("dn")])
                        P.add("dve", lambda e, o=sm1["rdn"][r], a=sm1["dn"][r]: e.reciprocal(out=o[:], in_=a[:]), [tk("dn")], [tk("rdn")])
                        k.ts("dve", tm["hq"][r][:, 0:128], tm["num"][r][:, 0:128], sm1["rdn"][r][:, 0:1], None, ALU.mult, ALU.bypass, [tk("num"), tk("rdn")], [tk("hq")])
                        k.tr(k.bank(bT)[:, 0:128], tm["hq"][r][:, 0:128], k.identf[:], [tk("hq")], [k.bk(bT)])
                        hv = tokview(HS, T, 0, st0, step, 128)
                        k.tt("dve", hv, k.bank(bT)[:, 0:128], hv, ALU.add, [k.bk(bT), ("mHS", stile)], [("mHS", stile)])
                        k.ts("pool", tb["Kw"][r][:], Kt[d][:, u, :], WS[d][:, u, h:h + 1], None, ALU.mult, ALU.bypass, [("mKt", d, u)], [tk("Kw")])
                        k.mm(k.bank(bC)[:, 0:129], tb["Kw"][r][:], Vt[d][:, u, :], True, True, [tk("Kw"), ("mVt", d, u), ("mVt1", d)], [k.bk(bC)])
                        k.stt("dve", C[:], C[:], WC[d][:, u, h:h + 1], k.bank(bC)[:, 0:129], ALU.mult, ALU.add, [("mC", d), k.bk(bC)], [("mC", d)])
                        k.cp("pool", Cb[:], C[:], [("mC", d)], [("mCb", d)])
                hk = [("mHS", u) for u in range(NT)]
                for (c0, cn) in [(0, 512), (512, 512), (1024, 512), (1536, 512), (2048, 256)]:
                    b = brr % 2
                    brr += 1
                    k.act(fz["sq"][:, 0:cn], HS[:, c0:c0 + cn], AF.Square, hk, ["mfsq"])
                    k.mm(k.bank(b)[:, 0:cn], cm["ones"][:], fz["sq"][:, 0:cn], True, True, ["mfsq"], [k.bk(b)])
                    k.act(fz["sd"][:, 0:cn], k.bank(b)[:, 0:cn], AF.Sqrt, [k.bk(b)], ["mfsd"], scale=1.0 / 128, bias=k.epsnm[:, 0:1])
                    P.add("dve", lambda e, o=fz["sd"][:, 0:cn]: e.reciprocal(out=o, in_=o), ["mfsd"], ["mfsd"])
                    k.tt("pool", fz["o1"][:, 0:cn], HS[:, c0:c0 + cn], fz["sd"][:, 0:cn], ALU.mult, hk + ["mfsd"], ["mfo1"])
                    k.stt("dve", mTt[:, h, c0:c0 + cn], fz["o1"][:, 0:cn], ng[:, h:h + 1], SO[:, c0:c0 + cn], ALU.mult, ALU.mult,
                          ["mfo1", ("mSO", c0)], [("mTt", 4 + h, c0)])


DECAY_K = -math.exp(-0.5)


def col_vec(ap2d):
    return ap2d.rearrange("n (c p) -> p n c", p=128)


def rwkv_mixer(k, i, cm):
    P, I = k.P, k.I
    j = i // 2
    Wr = I["rec_w_in"][j]
    TCS = [(0, 512), (512, 512), (1024, 512), (1536, 512), (2048, 256)]
    with P.phase() as sr:
        L1 = P.sbuf("rL1", [128, T], BF16, sr)
        L2 = P.sbuf("rL2", [128, T], BF16, sr)
        L3 = P.sbuf("rL3", [128, T], BF16, sr)
        with P.phase() as st:
            W = P.sbuf("rW", [128, 8, 2048], BF16, st)
            for kk in range(8):
                k.dma("pool", W[:, kk, :], Wr[kk * 128:(kk + 1) * 128, 0:2048], [], [("rW", kk)])
            hd = P.sbuf("rhd", [128, 8, T], BF16, st)
            tmp = [P.sbuf(f"rhtmp{r}", [128, S], BF16, st) for r in range(2)]
            muT = P.sbuf("rmuT", [128, 6, 4], F32, st)
            k.dma("sp", muT[:], col_vec(I["rwkv_mu"][j]), [], ["muT"], allow_slow_non_contiguous=True)
            w1b = P.sbuf("rw1b", [128, 2, 4, 32], BF16, st)
            a1b = P.sbuf("ra1b", [128, 2, 4, 32], BF16, st)
            g1b = P.sbuf("rg1b", [128, 4, 96], BF16, st)
            for d in range(2):
                k.dma("pool", w1b[:, d], I["rwkv_w1"][j, d].rearrange("(c p) r -> p c r", p=128), [], [("w1b", d)])
                k.dma("pool", a1b[:, d], I["rwkv_a1"][j, d].rearrange("(c p) r -> p c r", p=128), [], [("a1b", d)])
            k.dma("pool", g1b[:], I["rwkv_g1"][j].rearrange("(c p) r -> p c r", p=128), [], ["g1b"])
            n = 0
            for kk in range(8):
                for (a, b) in ((0, S), (S, T)):
                    r = n % 2
                    e1 = "pool" if n % 2 else "dve"
                    n += 1
                    nn = b - a
                    k.tt(e1, tmp[r][:, 0:nn - 2], k.AB[:, kk, a:b - 2], k.AB[:, kk, a + 2:b], ALU.add, [], [("rhtmp", r)])
                    k.stt("dve", hd[:, kk, a + 1:b - 1], tmp[r][:, 0:nn - 2], 0.5, k.AB[:, kk, a + 1:b - 1], ALU.mult, ALU.subtract, [("rhtmp", r)], [("hd", kk, a, 0)])
                    k.stt("dve", hd[:, kk, a:a + 1], k.AB[:, kk, a + 1:a + 2], 0.5, k.AB[:, kk, a:a + 1], ALU.mult, ALU.subtract, [], [("hd", kk, a, 1)])
                    k.stt("dve", hd[:, kk, b - 1:b], k.AB[:, kk, b - 2:b - 1], 0.5, k.AB[:, kk, b - 1:b], ALU.mult, ALU.subtract, [], [("hd", kk, a, 2)])
            P.flush(barrier=True)
            ps = [P.sbuf(f"rps{r}", [128, 512], F32, st) for r in range(2)]
            zt = {nm: [P.sbuf(f"rz{nm}{r}", [128, 512], BF16, st) for r in range(2)] for nm in ("w", "a", "g")}
            o32 = [P.sbuf(f"ro32{r}", [128, 512], F32, st) for r in range(2)]
            n = 0
            for (c0, cn) in TCS:
                for c in range(4):
                    r = n % 2
                    n += 1
                    for kk in range(8):
                        k.mm(k.bank(0)[:, 0:cn], W[:, kk, 1536 + c * 128:1536 + (c + 1) * 128], k.AB[:, kk, c0:c0 + cn], kk == 0, kk == 7, [("rW", kk)], [k.bk(0)])
                    for kk in range(8):
                        k.mm(k.bank(1)[:, 0:cn], W[:, kk, 1536 + c * 128:1536 + (c + 1) * 128], hd[:, kk, c0:c0 + cn], kk == 0, kk == 7, [("rW", kk)], [k.bk(1)])
                    k.cp("act", ps[r][:, 0:cn], k.bank(0)[:, 0:cn], [k.bk(0)], [("rps", r)])
                    for (nm, m) in (("w", 3), ("a", 4), ("g", 5)):
                        k.stt("dve", zt[nm][r][:, 0:cn], k.bank(1)[:, 0:cn], muT[:, m, c:c + 1], ps[r][:, 0:cn], ALU.mult, ALU.add,
                              [k.bk(1), ("rps", r), "muT"], [("rz", nm, r)])
                    f, l = (c == 0), (c == 3)
                    k.mm(k.bank(2)[0:32, 0:cn], w1b[:, 0, c, :], zt["w"][r][:, 0:cn], f, l, [("rz", "w", r), ("w1b", 0)], [k.bk(2)])
                    k.mm(k.bank(3)[32:64, 0:cn], w1b[:, 1, c, :], zt["w"][r][:, 0:cn], f, l, [("rz", "w", r), ("w1b", 1)], [k.bk(3)])
                    k.mm(k.bank(4)[64:96, 0:cn], a1b[:, 0, c, :], zt["a"][r][:, 0:cn], f, l, [("rz", "a", r), ("a1b", 0)], [k.bk(4)])
                    k.mm(k.bank(5)[0:32, 0:cn], a1b[:, 1, c, :], zt["a"][r][:, 0:cn], f, l, [("rz", "a", r), ("a1b", 1)], [k.bk(5)])
                    k.mm(k.bank(6)[0:96, 0:cn], g1b[:, c, :], zt["g"][r][:, 0:cn], f, l, [("rz", "g", r), "g1b"], [k.bk(6)])
                k.act(L1[0:32, c0:c0 + cn], k.bank(2)[0:32, 0:cn], AF.Tanh, [k.bk(2)], [("L1", 0, c0)])
                k.act(L1[32:64, c0:c0 + cn], k.bank(3)[32:64, 0:cn], AF.Tanh, [k.bk(3)], [("L1", 1, c0)])
                k.cp("act", L1[64:96, c0:c0 + cn], k.bank(4)[64:96, 0:cn], [k.bk(4)], [("L1", 2, c0)])
                k.cp("act", L3[0:32, c0:c0 + cn], k.bank(5)[0:32, 0:cn], [k.bk(5)], [("L3", c0)])
                k.act(L2[0:96, c0:c0 + cn], k.bank(6)[0:96, 0:cn], AF.Sigmoid, [k.bk(6)], [("L2", c0)])
            for g in range(3):
                for (c0, cn) in TCS:
                    for c in range(4):
                        r = n % 2
                        n += 1
                        b0, b1 = (0, 1) if r == 0 else (2, 3)
                        for kk in range(8):
                            k.mm(k.bank(b0)[:, 0:cn], W[:, kk, g * 512 + c * 128:g * 512 + (c + 1) * 128], k.AB[:, kk, c0:c0 + cn], kk == 0, kk == 7, [("rW", kk)], [k.bk(b0)])
                        for kk in range(8):
                            k.mm(k.bank(b1)[:, 0:cn], W[:, kk, g * 512 + c * 128:g * 512 + (c + 1) * 128], hd[:, kk, c0:c0 + cn], kk == 0, kk == 7, [("rW", kk)], [k.bk(b1)])
                        k.cp("act", ps[r][:, 0:cn], k.bank(b0)[:, 0:cn], [k.bk(b0)], [("rps", r)])
                        k.stt("dve", o32[r][:, 0:cn], k.bank(b1)[:, 0:cn], muT[:, g, c:c + 1], ps[r][:, 0:cn], ALU.mult, ALU.add,
                              [k.bk(b1), ("rps", r), "muT"], [("ro32", r)])
                        k.dma("sp", k.rkv_d[g, c * 128:(c + 1) * 128, c0:c0 + cn], o32[r][:, 0:cn], [("ro32", r)], [("rkv_d", g, c, c0)])
        for hp in range(4):
            rwkv_stage_b(k, i, hp, cm, L1, L2, L3)


def rwkv_stage_b(k, i, hp, cm, L1, L2, L3):
    P, I = k.P, k.I
    j = i // 2
    TCS = [(0, 512), (512, 512), (1024, 512), (1536, 512), (2048, 256)]
    BN = 512
    cs_ = slice(hp * 128, (hp + 1) * 128)
    with P.phase() as st:
        pv = {}
        for nm, src, nrow in (("w0", "rwkv_w0", 2), ("a0", "rwkv_a0", 2), ("kvec", "rwkv_kvec", 2), ("gn", "rwkv_gn", 2)):
            pv[nm] = P.sbuf("rp_" + nm, [128, nrow], F32, st)
            k.dma("sp", pv[nm][:], I[src][j][:, cs_].rearrange("n p -> p n"), [], [("rp", nm)], allow_slow_non_contiguous=True)
        pv["rk"] = P.sbuf("rp_rk", [128, 1], F32, st)
        k.dma("sp", pv["rk"][:], I["rwkv_rk"][j, cs_].rearrange("(p o) -> p o", o=1), [], [("rp", "rk")], allow_slow_non_contiguous=True)
        pv["omk"] = P.sbuf("rp_omk", [128, 1], F32, st)
        k.ts("dve", pv["omk"][:], pv["kvec"][:, 1:2], -1.0, 1.0, ALU.mult, ALU.add, [("rp", "kvec")], [("rp", "omk")])
        pv["gne"] = P.sbuf("rp_gne", [128, 1], F32, st)
        P.add("pool", lambda e: e.memset(pv["gne"][:], RWKV_GN_EPS), [], [("rp", "gne")])
        w2b = P.sbuf("rw2b", [128, 128], BF16, st)
        a2b1 = P.sbuf("ra2b1", [32, 128], BF16, st)
        g2b = P.sbuf("rg2b", [96, 128], BF16, st)
        k.dma("pool", w2b[0:32, :], I["rwkv_w2"][j, 0][:, cs_], [], [("w2b", 0)])
        k.dma("pool", w2b[32:64, :], I["rwkv_w2"][j, 1][:, cs_], [], [("w2b", 1)])
        k.dma("pool", w2b[64:96, :], I["rwkv_a2"][j, 0][:, cs_], [], [("w2b", 2)])
        k.dma("pool", a2b1[:], I["rwkv_a2"][j, 1][:, cs_], [], ["a2b1"])
        k.dma("pool", g2b[:], I["rwkv_g2"][j][:, cs_], [], ["g2b"])
        R = P.sbuf("rR", [128, T], BF16, st)
        Kx = P.sbuf("rK", [128, T], BF16, st)
        V = P.sbuf("rV", [128, T], BF16, st)
        k.dma("pool", R[:], k.rkv_d[0, cs_, :], [], ["rR"])
        k.dma("pool", Kx[:], k.rkv_d[1, cs_, :], [], ["rK"])
        k.dma("pool", V[:], k.rkv_d[2, cs_, :], [], ["rV"])
        KK = P.sbuf("rKK", [128, T], F32, st)
        GA = P.sbuf("rGA", [128, T], BF16, st)
        LWr = [P.sbuf(f"rlw{d}", [128, T], F32, st) for d in range(2)]
        Aa = [P.sbuf(f"raa{d}", [128, T], BF16, st) for d in range(2)]
        YS = P.sbuf("rYS", [128, T], F32, st)
        ft = {nm: P.sbuf("rf_" + nm, [128, 512], F32, st) for nm in ("a", "b", "c")}
        brr = [0]

        def nb():
            brr[0] += 1
            return brr[0] % 8

        for (c0, cn) in TCS:
            sl = slice(c0, c0 + cn)
            k.act(ft["a"][:, 0:cn], Kx[:, sl], AF.Square, ["rK", ("rp", "kvec")], ["rfa"], scale=pv["kvec"][:, 0:1])
            b = nb()
            k.mm(k.bank(b)[:, 0:cn], cm["blk64"][:], ft["a"][:, 0:cn], True, True, ["rfa"], [k.bk(b)])
            k.ts("dve", ft["b"][:, 0:cn], k.bank(b)[:, 0:cn], 1e-24, None, ALU.max, ALU.bypass, [k.bk(b)], ["rfb"])
            k.act(ft["b"][:, 0:cn], ft["b"][:, 0:cn], AF.Sqrt, ["rfb"], ["rfb"])
            P.add("dve", lambda e, o=ft["b"][:, 0:cn]: e.reciprocal(out=o, in_=o), ["rfb"], ["rfb"])
            k.stt("dve", KK[:, sl], Kx[:, sl], pv["kvec"][:, 0:1], ft["b"][:, 0:cn], ALU.mult, ALU.mult, ["rK", "rfb", ("rp", "kvec")], [("rKK", c0)])
            b = nb()
            k.mm(k.bank(b)[:, 0:cn], g2b[:], L2[0:96, sl], True, True, ["g2b"], [k.bk(b)])
            k.cp("act", GA[:, sl], k.bank(b)[:, 0:cn], [k.bk(b)], [("rGA", c0)])
            for d in range(2):
                b = nb()
                k.mm(k.bank(b)[:, 0:cn], w2b[32 * d:32 * d + 32, :], L1[32 * d:32 * d + 32, sl], True, True, [("w2b", d)], [k.bk(b)])
                k.act(ft["c"][:, 0:cn], k.bank(b)[:, 0:cn], AF.Sigmoid, [k.bk(b), ("rp", "w0")], ["rfc"], bias=pv["w0"][:, d:d + 1], scale=1.0)
                k.ts("dve", LWr[d][:, sl], ft["c"][:, 0:cn], float(DECAY_K), None, ALU.mult, ALU.bypass, ["rfc"], [("rlw", d, c0)])
                b = nb()
                if d == 0:
                    k.mm(k.bank(b)[:, 0:cn], w2b[64:96, :], L1[64:96, sl], True, True, [("w2b", 2)], [k.bk(b)])
                else:
                    k.mm(k.bank(b)[:, 0:cn], a2b1[:], L3[0:32, sl], True, True, ["a2b1"], [k.bk(b)])
                k.act(Aa[d][:, sl], k.bank(b)[:, 0:cn], AF.Sigmoid, [k.bk(b), ("rp", "a0")], [("raa", d, c0)], bias=pv["a0"][:, d:d + 1], scale=1.0)
        P.flush(barrier=True)
        rst = P.sbuf("rrst", [128, BN], BF16, st)
        P.add("pool", lambda e: e.memset(rst[:], 1.0), [], ["rst"])
        P.add("pool", lambda e: e.memset(bass.AP(rst, 0, [[BN, 128], [64, BN // 64]]), 0.0), ["rst"], ["rst"])
        bl = {nm: P.sbuf("rb_" + nm, [128, BN], F32, st) for nm in ("LW", "E", "kka", "ke", "AH", "KH", "AW", "KW", "YT", "t", "Vb")}
        BR = P.sbuf("rBR", [128, BN // 128, 2, 128], F32, st)
        Hs = P.sbuf("rH", [128, 64], F32, st)
        tl = {nm: [P.sbuf(f"rt_{nm}{r}", [128, 128], F32 if nm in ("R1T", "Y0T") else BF16, st) for r in range(4)]
              for nm in ("Mm", "nAT", "Bm", "Btm", "MT", "Pv", "Q", "QT", "G", "nG2", "R1T", "Y0T")}
        TMs = [P.sbuf(f"rTM{r}", [128, 4, 128], BF16, st) for r in range(2)]
        FTs = [P.sbuf(f"rFT{r}", [128, 2, 64], F32, st) for r in range(4)]
        Zs = [P.sbuf(f"rZs{r}", [128, 2, 64], F32, st) for r in range(4)]
        for d in range(2):
            if d == 0:
                blocks = [(S, 1, NCTX), (0, 1, 512), (512, 1, 512), (1024, 1, 512), (1536, 1, 512)]
            else:
                blocks = [(T - 1, -1, NCTX), (S - 1, -1, 512), (S - 513, -1, 512), (S - 1025, -1, 512), (S - 1537, -1, 512)]
            P.add("dve", lambda e: e.memset(Hs[:], 0.0), [], [("rH", 0), ("rH", 1)])
            for (bs, step, bn) in blocks:
                Vw = lambda arr, bs=bs, step=step, bn=bn: tokview(arr, T, 0, bs, step, bn)
                B_ = lambda nm, bn=bn: bl[nm][:, 0:bn]
                nch = bn // 64
                P.add("dve", lambda e, o=B_("LW"), a=rst[:, 0:bn], b=Vw(LWr[d]): e.tensor_tensor_scan(
                    out=o, data0=a, data1=b, initial=0.0, op0=ALU.mult, op1=ALU.add), ["rst"], ["bLW"])
                lwend = bass.AP(bl["LW"], 63, [[BN, 128], [64, nch], [0, 64]])
                lw3 = bl["LW"][:, 0:bn].rearrange("p (c t) -> p c t", t=64)
                a_v, kk_v, k_v, r_v = Vw(Aa[d]), Vw(KK), Vw(Kx), Vw(R)
                k.cp("pool", B_("Vb"), Vw(V), [], ["bVb"])
                k.tt("pool", B_("kka"), kk_v, a_v, ALU.mult, [], ["bkka"])
                k.ts("dve", B_("t"), a_v, pv["kvec"][:, 1:2], pv["omk"][:, 0:1], ALU.mult, ALU.add, [("rp", "omk")], ["bt"])
                k.tt("pool", B_("ke"), B_("t"), k_v, ALU.mult, ["bt"], ["bke"])
                k.act(B_("E"), B_("LW"), AF.Exp, ["bLW"], ["bE"], scale=-1.0)
                k.tt("dve", B_("AH"), B_("kka"), B_("E"), ALU.mult, ["bkka", "bE"], ["bAH"])
                k.tt("pool", B_("KH"), B_("ke"), B_("E"), ALU.mult, ["bke", "bE"], ["bKH"])
                k.tt("dve", B_("t").rearrange("p (c t) -> p c t", t=64), lwend, lw3, ALU.subtract, ["bLW", "bt"], ["bt"])
                k.act(B_("E"), B_("t"), AF.Exp, ["bt", "bE", "bAH", "bKH"], ["bE"])
                k.tt("dve", B_("AW"), B_("kka"), B_("E"), ALU.mult, ["bkka", "bE"], ["bAW"])
                k.tt("pool", B_("KW"), B_("ke"), B_("E"), ALU.mult, ["bke", "bE"], ["bKW"])
                k.tt("dve", B_("t"), B_("LW"), Vw(LWr[d]), ALU.subtract, ["bLW", "bt"], ["bt"])
                k.act(B_("E"), B_("t"), AF.Exp, ["bt", "bE", "bAW", "bKW"], ["bE"])
                ntile = bn // 128
                brv = lambda q, ntile=ntile: bass.AP(BR, q * 128, [[(BN // 128) * 256, 128], [256, ntile], [1, 128]])
                k.tt("dve", brv(0), bass.AP(kk_v.tensor, kk_v.offset, [[T, 128], [128 * step, ntile], [step, 128]]),
                     B_("E").rearrange("p (c t) -> p c t", t=128), ALU.mult, ["bE"], ["bBR0"])
                k.act(B_("E"), B_("LW"), AF.Exp, ["bLW", "bE", "bBR0"], ["bE"])
                k.tt("pool", brv(1), bass.AP(r_v.tensor, r_v.offset, [[T, 128], [128 * step, ntile], [step, 128]]),
                     B_("E").rearrange("p (c t) -> p c t", t=128), ALU.mult, ["bE"], ["bBR1"])
                k.act(bl["t"][:, 0:nch], bass.AP(bl["LW"], 63, [[BN, 128], [64, nch]]), AF.Exp, ["bLW", "bt", "bE"], ["bWC", "bt"])
                for tg0 in range(0, ntile, 2):
                    tis = [ti for ti in (tg0, tg0 + 1) if ti < ntile]
                    chains = [(ti, hh) for ti in tis for hh in range(2)]
                    for ti in tis:
                        tsl = slice(ti * 128, (ti + 1) * 128)
                        r = ti % 2
                        bT = nb()
                        srcs = [BR[:, ti, 0, :], bl["Vb"][:, tsl], bl["AW"][:, tsl], bl["KW"][:, tsl]]
                        for q, s_ in enumerate(srcs):
                            k.tr(k.bank(bT)[:, q * 128:(q + 1) * 128], s_, k.identf[:], ["bBR0", "bAW", "bKW", "bVb"], [k.bk(bT)])
                        k.cp("act", TMs[r][:], k.bank(bT)[:, 0:512].rearrange("p (q n) -> p q n", q=4), [k.bk(bT)], [("rTM", r)])
                    CH = []
                    for x, (ti, hh) in enumerate(chains):
                        CH.append(dict(x=x, ti=ti, hh=hh, r=ti % 2, ps=slice(hh * 64, (hh + 1) * 64), tsl=slice(ti * 128, (ti + 1) * 128),
                                       L={nm: tl[nm][x] for nm in tl}, lk=(lambda nm, x=x: ("rtl", nm, x))))
                    for c_ in CH:
                        L, lk, ps_, tsl, ti = c_["L"], c_["lk"], c_["ps"], c_["tsl"], c_["ti"]
                        bX = nb()
                        k.mm(k.bank(bX)[:, 0:256], bl["AH"][ps_, tsl], BR[ps_, ti, :, :], True, True, ["bAH", "bBR0", "bBR1"], [k.bk(bX)])
                        k.mm(k.bank(bX)[:, 256:512], bl["KH"][ps_, tsl], BR[ps_, ti, :, :], True, True, ["bKH", "bBR0", "bBR1"], [k.bk(bX)])
                        k.tt("dve", L["Mm"][:], k.bank(bX)[:, 0:128], cm["m_lt"][:], ALU.mult, [k.bk(bX)], [lk("Mm")])
                        k.stt("dve", L["nAT"][:], k.bank(bX)[:, 128:256], -1.0, cm["m_le"][:], ALU.mult, ALU.mult, [k.bk(bX)], [lk("nAT")])
                        k.tt("dve", L["Bm"][:], k.bank(bX)[:, 256:384], cm["m_lt"][:], ALU.mult, [k.bk(bX)], [lk("Bm")])
                        k.tt("dve", L["Btm"][:], k.bank(bX)[:, 384:512], cm["m_le"][:], ALU.mult, [k.bk(bX)], [lk("Btm")])
                    for c_ in CH:
                        L, lk = c_["L"], c_["lk"]
                        bY = nb()
                        pyb = k.bank(bY).bitcast(BF16)
                        k.tr(pyb[:, 0:128], L["Mm"][:], k.identb[:], [lk("Mm")], [k.bk(bY)])
                        k.cp("act", L["MT"][:], pyb[:, 0:128], [k.bk(bY)], [lk("MT")])
                        k.tt("pool", L["Pv"][:], k.identf[:], L["Mm"][:], ALU.subtract, [lk("Mm")], [lk("Pv")])
                        c_["Q"], c_["QT"], c_["qk"], c_["qtk"] = L["Mm"], L["MT"], lk("Mm"), lk("MT")
                    for lvl in range(1, 6):
                        for c_ in CH:
                            L, lk = c_["L"], c_["lk"]
                            bq = nb()
                            c_["bq"] = bq
                            if lvl < 5:
                                k.mm(k.bank(bq)[:, 0:128], c_["QT"][:], c_["Q"][:], True, True, [c_["qk"], c_["qtk"]], [k.bk(bq)])
                            k.mm(k.bank(bq)[:, 128:256], c_["Q"][:], c_["QT"][:], True, True, [c_["qk"], c_["qtk"]], [k.bk(bq)])
                        for c_ in CH:
                            L, lk, bq = c_["L"], c_["lk"], c_["bq"]
                            if lvl < 5:
                                k.cp("act", L["Q"][:], k.bank(bq)[:, 0:128], [k.bk(bq)], [lk("Q")])
                            k.cp("act", L["QT"][:], k.bank(bq)[:, 128:256], [k.bk(bq)], [lk("QT")])
                            c_["Q"], c_["QT"], c_["qk"], c_["qtk"] = L["Q"], L["QT"], lk("Q"), lk("QT")
                        for c_ in CH:
                            L, lk = c_["L"], c_["lk"]
                            bp = nb()
                            c_["bp"] = bp
                            k.mm(k.bank(bp)[:, 0:128], c_["QT"][:], L["Pv"][:], True, True, [c_["qtk"], lk("Pv")], [k.bk(bp)])
                        for c_ in CH:
                            L, lk, bp = c_["L"], c_["lk"], c_["bp"]
                            k.tt("dve", L["Pv"][:], L["Pv"][:], k.bank(bp)[:, 0:128], ALU.add, [k.bk(bp), lk("Pv")], [lk("Pv")])
                    for c_ in CH:
                        L, lk, ps_, r = c_["L"], c_["lk"], c_["ps"], c_["r"]
                        bb = nb()
                        k.mm(k.bank(bb)[:, 0:64], L["Bm"][:], TMs[r][:, 1, ps_], True, True, [lk("Bm"), ("rTM", r)], [k.bk(bb)])
                        k.cp("act", L["Mm"][:, 64:128], k.bank(bb)[:, 0:64], [k.bk(bb)], [lk("Mm")])
                        k.cp("pool", L["Mm"][:, 0:64], TMs[r][:, 0, ps_], [("rTM", r)], [lk("Mm")])
                    for c_ in CH:
                        L, lk = c_["L"], c_["lk"]
                        bg = nb()
                        k.mm(k.bank(bg)[:, 0:128], L["Pv"][:], L["Mm"][:], True, True, [lk("Pv"), lk("Mm")], [k.bk(bg)])
                        k.cp("act", L["G"][:], k.bank(bg)[:, 0:128], [k.bk(bg)], [lk("G")])
                        k.ts("pool", L["nG2"][:, 0:64], L["G"][:, 64:128], -1.0, None, ALU.mult, ALU.bypass, [lk("G")], [lk("nG2")])
                    for c_ in CH:
                        L, lk, ps_, r, ti = c_["L"], c_["lk"], c_["ps"], c_["r"], c_["ti"]
                        b1 = nb()
                        k.mm(k.bank(b1)[ps_, 0:128], L["G"][:, 0:64], L["nAT"][:], True, True, [lk("G"), lk("nAT")], [k.bk(b1)])
                        k.tt("dve", L["R1T"][ps_, :], k.bank(b1)[ps_, 0:128], BR[ps_, ti, 1, :], ALU.add, [k.bk(b1), "bBR1"], [lk("R1T")])
                        b2 = nb()
                        k.mm(k.bank(b2)[ps_, 0:128], TMs[r][:, 1, ps_], L["Btm"][:], True, False, [("rTM", r), lk("Btm")], [k.bk(b2)])
                        k.mm(k.bank(b2)[ps_, 0:128], L["G"][:, 64:128], L["nAT"][:], False, True, [lk("G"), lk("nAT")], [k.bk(b2)])
                        k.cp("act", L["Y0T"][ps_, :], k.bank(b2)[ps_, 0:128], [k.bk(b2)], [lk("Y0T")])
                    for c in range(2):
                        rows = slice(c * 64, (c + 1) * 64)
                        for c_ in CH:
                            L, lk, ps_, r, ti, hh, x = c_["L"], c_["lk"], c_["ps"], c_["r"], c_["ti"], c_["hh"], c_["x"]
                            bf_ = nb()
                            k.mm(k.bank(bf_)[ps_, 0:64], L["G"][rows, 0:64], TMs[r][rows, 2, ps_], True, True, [lk("G"), ("rTM", r)], [k.bk(bf_)])
                            wc = bl["t"][ps_, ti * 2 + c:ti * 2 + c + 1]
                            k.stt("dve", FTs[x][ps_, c, :], k.identf[ps_, hh * 64:(hh + 1) * 64], wc, k.bank(bf_)[ps_, 0:64], ALU.mult, ALU.subtract,
                                  [k.bk(bf_), "bWC"], [("rFT", x, c)])
                            bz = nb()
                            k.mm(k.bank(bz)[ps_, 0:64], TMs[r][rows, 3, ps_], TMs[r][rows, 1, ps_], True, False, [("rTM", r)], [k.bk(bz)])
                            k.mm(k.bank(bz)[ps_, 0:64], TMs[r][rows, 2, ps_], L["nG2"][rows, 0:64], False, True, [("rTM", r), lk("nG2")], [k.bk(bz)])
                            k.cp("act", Zs[x][ps_, c, :], k.bank(bz)[ps_, 0:64], [k.bk(bz)], [("rZs", x, c)])
                    for ti in tis:
                        for c in range(2):
                            cc = slice(c * 64, (c + 1) * 64)
                            for c_ in CH:
                                if c_["ti"] != ti:
                                    continue
                                L, lk, ps_, hh, x = c_["L"], c_["lk"], c_["ps"], c_["hh"], c_["x"]
                                by = nb()
                                k.mm(k.bank(by)[ps_, 0:64], Hs[ps_, :], L["R1T"][ps_, cc], True, True, [("rH", hh), lk("R1T")], [k.bk(by)])
                                k.tt("dve", bl["YT"][ps_, ti * 128 + c * 64:ti * 128 + (c + 1) * 64], k.bank(by)[ps_, 0:64], L["Y0T"][ps_, cc], ALU.add,
                                     [k.bk(by), lk("Y0T")], [("bYT", hh, ti, c)])
                                bh = nb()
                                k.mm(k.bank(bh)[ps_, 0:64], FTs[x][ps_, c, :], Hs[ps_, :], True, True, [("rH", hh), ("rFT", x, c)], [k.bk(bh)])
                                k.tt("dve", Hs[ps_, :], k.bank(bh)[ps_, 0:64], Zs[x][ps_, c, :], ALU.add, [k.bk(bh), ("rZs", x, c), ("rH", hh)], [("rH", hh)])
                yk = [("bYT", hh, ti, c) for hh in range(2) for ti in range(ntile) for c in range(2)]
                ysv = Vw(YS)
                if d == 0:
                    k.cp("pool", ysv, B_("YT"), yk, [("rYS", bs)])
                else:
                    k.tt("pool", ysv, ysv, B_("YT"), ALU.add, yk, [("rYS", bs)])
                P.flush(barrier=True)
        for (c0, cn) in TCS:
            sl = slice(c0, c0 + cn)
            fa, fb, fc = ft["a"][:, 0:cn], ft["b"][:, 0:cn], ft["c"][:, 0:cn]
            b = nb()
            k.mm(k.bank(b)[:, 0:cn], cm["blk64"][:], YS[:, sl], True, True, [], [k.bk(b)])
            k.stt("dve", fa, k.bank(b)[:, 0:cn], -1.0 / 64, YS[:, sl], ALU.mult, ALU.add, [k.bk(b)], ["ffa"])
            k.act(fb, fa, AF.Square, ["ffa"], ["ffb"])
            b = nb()
            k.mm(k.bank(b)[:, 0:cn], cm["blk64"][:], fb, True, True, ["ffb"], [k.bk(b)])
            k.act(fb, k.bank(b)[:, 0:cn], AF.Sqrt, [k.bk(b), ("rp", "gne")], ["ffb"], scale=1.0 / 64, bias=pv["gne"][:, 0:1])
            P.add("dve", lambda e, o=fb: e.reciprocal(out=o, in_=o), ["ffb"], ["ffb"])
            k.tt("pool", fa, fa, fb, ALU.mult, ["ffa", "ffb"], ["ffa"])
            k.ts("dve", fa, fa, pv["gn"][:, 0:1], pv["gn"][:, 1:2], ALU.mult, ALU.add, ["ffa", ("rp", "gn")], ["ffa"])
            k.tt("pool", fc, Aa[0][:, sl], Aa[1][:, sl], ALU.add, [], ["ffc"])
            k.ts("dve", fc, fc, pv["kvec"][:, 1:2], pv["omk"][:, 0:1], ALU.mult, ALU.add, ["ffc"], ["ffc"])
            k.stt("dve", fc, fc, pv["omk"][:, 0:1], Kx[:, sl], ALU.add, ALU.mult, ["ffc"], ["ffc"])
            k.stt("dve", fc, R[:, sl], pv["rk"][:, 0:1], fc, ALU.mult, ALU.mult, ["ffc", ("rp", "rk")], ["ffc"])
            b = nb()
            k.mm(k.bank(b)[:, 0:cn], cm["blk64"][:], fc, True, True, ["ffc"], [k.bk(b)])
            k.tt("dve", fb, k.bank(b)[:, 0:cn], V[:, sl], ALU.mult, [k.bk(b), "ffb"], ["ffb"])
            k.tt("pool", fa, fa, fb, ALU.add, ["ffa", "ffb"], ["ffa"])
            k.tt("dve", k.AB[:, hp, sl], fa, GA[:, sl], ALU.mult, ["ffa"], [("ABo", hp, c0)])
```

```python
import contextlib
import math
import numpy as np
import ml_dtypes
import concourse.bass as bass
import concourse.mybir as mybir
from concourse.bass_utils import run_bass_kernel_spmd

F32 = mybir.dt.float32
BF16 = mybir.dt.bfloat16
ALU = mybir.AluOpType
AF = mybir.ActivationFunctionType
AX = mybir.AxisListType

D = 1024
S = 2048
NCTX = 256
T = S + NCTX
NT = T // 128
DEPTH = 4
GRID_W = 64
HD = 64
ATT_IN = 2304
REC_IN = 4112
NEXP = 64
TOPK = 6
ROUTED_SCALE = 2.5
DN_ALPHA = (2 * DEPTH) ** 0.25
LN_EPS = 1e-5
NORM_EPS = 1e-6
RWKV_GN_EPS = 64e-5
NEG = -30000.0

CONST_NAMES = ("ident", "ropec", "ropes", "ropepm", "blk64", "ones", "m_le", "m_lt", "m_le128", "m_gt128")
NDMA_Q = {"sp": 16, "pool": 8}
SEM_EPOCH = 30000


class Op:
    __slots__ = ("eng", "fn", "dma", "deps", "needs_inc", "sem", "count", "slot", "emitted")

    def __init__(self, eng, fn, dma):
        self.eng = eng
        self.fn = fn
        self.dma = dma
        self.deps = []
        self.needs_inc = False
        self.sem = None
        self.count = 0
        self.slot = None
        self.emitted = False


class Prog:
    ENGS = ("pe", "dve", "act", "pool", "sp")

    def __init__(self, nc):
        self.nc = nc
        self.stack = contextlib.ExitStack()
        self.pending = {e: [] for e in self.ENGS}
        self.res = {}
        self.dma_last = {q: [None] * n for q, n in NDMA_Q.items()}
        self.dma_rr = {q: 0 for q in NDMA_Q}
        self.eng_obj = {"pe": nc.tensor, "dve": nc.vector, "act": nc.scalar,
                        "pool": nc.gpsimd, "sp": nc.sync}
        self.dma_sems = {q: [self.stack.enter_context(nc.semaphore(f"dq_{q}{i}")) for i in range(n)] for q, n in NDMA_Q.items()}
        self.eng_sem = {e: None for e in self.ENGS}
        self.eng_cnt = {e: 0 for e in self.ENGS}
        self.eng_nep = {e: 0 for e in self.ENGS}
        self.eng_last = {e: None for e in self.ENGS}
        self.waited = {e: {} for e in self.ENGS}
        self.out_dmas = []
        self.n_ops = 0

    def sbuf(self, name, shape, dtype, stack=None):
        self.n_alloc = getattr(self, "n_alloc", 0) + 1
        return (stack or self.stack).enter_context(self.nc.sbuf_tensor(f"{name}_{self.n_alloc}", list(shape), dtype))

    def psum(self, name, shape, dtype=F32):
        return self.stack.enter_context(self.nc.psum_tensor(name, list(shape), dtype))

    def add(self, eng, fn, reads=(), writes=(), dma=False):
        op = Op(eng, fn, dma)
        deps = {}
        for k in reads:
            r = self.res.get(k)
            if r is not None and r[0] is not None:
                deps[id(r[0])] = r[0]
            if r is not None and isinstance(k, tuple) and k and k[0] == "pb":
                for o in r[1]:
                    if o.eng != eng:
                        deps[id(o)] = o
        for k in writes:
            r = self.res.get(k)
            if r is not None:
                if r[0] is not None:
                    deps[id(r[0])] = r[0]
                for o in r[1]:
                    deps[id(o)] = o
        for k in reads:
            r = self.res.get(k)
            if r is None:
                r = [None, []]
                self.res[k] = r
            r[1].append(op)
        for k in writes:
            self.res[k] = [op, []]
        if dma:
            slot = self.dma_rr[eng]
            self.dma_rr[eng] = (slot + 1) % NDMA_Q[eng]
            prev = self.dma_last[eng][slot]
            op.slot = slot
            op.sem = self.dma_sems[eng][slot]
            op.count = (prev.count if prev is not None else 0) + 16
            if prev is not None:
                deps[id(prev)] = prev
            self.dma_last[eng][slot] = op
        deps.pop(id(op), None)
        for d in deps.values():
            if d.eng == "pe" and eng == "pe" and not d.dma and not dma:
                continue
            op.deps.append(d)
            d.needs_inc = True
        self.pending[eng].append(op)
        self.n_ops += 1
        return op

    def _wait(self, e, sem, count):
        w = self.waited[e]
        key = id(sem)
        if w.get(key, 0) >= count:
            return
        self.eng_obj[e].wait_ge(sem, count)
        w[key] = count

    def flush(self, barrier=True):
        nc = self.nc
        if barrier:
            for e in self.ENGS:
                for op in reversed(self.pending[e]):
                    if not op.dma:
                        op.needs_inc = True
                        break
        for e in self.ENGS:
            for op in self.pending[e]:
                if op.dma or not op.needs_inc:
                    continue
                if self.eng_sem[e] is None or self.eng_cnt[e] >= SEM_EPOCH:
                    self.eng_sem[e] = self.stack.enter_context(nc.semaphore(f"c_{e}_{self.eng_nep[e]}"))
                    self.eng_nep[e] += 1
                    self.eng_cnt[e] = 0
                self.eng_cnt[e] += 1
                op.sem = self.eng_sem[e]
                op.count = self.eng_cnt[e]
                self.eng_last[e] = op
        for e in self.ENGS:
            eng = self.eng_obj[e]
            for op in self.pending[e]:
                for d in op.deps:
                    self._wait(e, d.sem, d.count)
                ins = op.fn(eng)
                if op.dma:
                    ins.then_inc(op.sem, 16)
                elif op.needs_inc:
                    ins.then_inc(op.sem, 1)
                op.emitted = True
                op.fn = None
            self.pending[e] = []
        if barrier:
            for e in self.ENGS:
                for e2 in self.ENGS:
                    o = self.eng_last[e2]
                    if o is not None and not (e2 == e and e == "pe"):
                        self._wait(e, o.sem, o.count)
                for q in self.dma_last:
                    for o in self.dma_last[q]:
                        if o is not None:
                            self._wait(e, o.sem, o.count)
            self.res = {}

    @contextlib.contextmanager
    def phase(self):
        st = contextlib.ExitStack()
        try:
            yield st
            self.flush(barrier=True)
        finally:
            st.close()

    def finish(self):
        self.flush(barrier=True)
        self.stack.close()


def _bf(a):
    return np.ascontiguousarray(a.astype(ml_dtypes.bfloat16))


def host_constants():
    c = {}
    c["ident"] = np.eye(128, dtype=np.float32)
    t = np.arange(S)
    row = (t // GRID_W).astype(np.float64)
    col = (t % GRID_W).astype(np.float64)
    inv = 10000.0 ** (-np.arange(0, 32, 2, dtype=np.float64) / 32.0)
    ang = np.concatenate([row[:, None] * inv, col[:, None] * inv], -1)
    cosT = np.cos(ang).T
    sinT = np.sin(ang).T
    cos64 = np.repeat(cosT, 2, axis=0)
    sin64 = np.repeat(sinT, 2, axis=0)
    c["ropec"] = np.concatenate([cos64, cos64], 0).astype(np.float32)
    c["ropes"] = np.concatenate([sin64, sin64], 0).astype(np.float32)
    pm = np.zeros((128, 128), np.float32)
    for i in range(64):
        pm[2 * i + 1, 2 * i] = -1.0
        pm[2 * i, 2 * i + 1] = 1.0
    c["ropepm"] = pm
    bo = np.zeros((128, 128), np.float32)
    bo[:64, :64] = 1.0
    bo[64:, 64:] = 1.0
    c["blk64"] = bo
    c["ones"] = np.ones((128, 128), np.float32)
    s_ = np.arange(128)[:, None]
    t_ = np.arange(128)[None, :]
    same = (s_ // 64) == (t_ // 64)
    c["m_le"] = (same & (s_ <= t_)).astype(np.float32)
    c["m_lt"] = (same & (s_ < t_)).astype(np.float32)
    c["m_ge"] = (same & (s_ >= t_)).astype(np.float32)
    c["m_gt"] = (same & (s_ > t_)).astype(np.float32)
    c["m_le128"] = (s_ <= t_).astype(np.float32)
    c["m_ge128"] = (s_ >= t_).astype(np.float32)
    c["m_lt128"] = (s_ < t_).astype(np.float32)
    c["m_gt128"] = (s_ > t_).astype(np.float32)
    return c


def na_bias_tables(rpb):
    H = 8
    ext = np.concatenate([rpb.reshape(H, 15 * 31), np.full((H, 1), NEG, np.float32)], 1)
    cq = np.arange(64)[:, None]
    ck = np.arange(64)[None, :]
    cs = np.clip(cq - 8, 0, 48)
    col_in = (ck >= cs) & (ck < cs + 16)
    dcol = np.clip(ck - cq, -15, 15) + 15
    idxE = np.zeros((15, 64, 64), np.int64)
    for dr in range(15):
        idxE[dr] = np.where(col_in, dr * 31 + dcol, 465)
    tabE = np.zeros((128, 4, 15, 64), np.float32)
    tabO = np.zeros((128, 4, 10, 64), np.float32)
    mrow = np.full((64, 64), 465, np.int64)
    for h in range(H):
        pb = (h % 2) * 64
        hp = h // 2
        for dr in range(15):
            tabE[pb:pb + 64, hp, dr, :] = ext[h][idxE[dr]]
        tabO[pb:pb + 64, hp, 0, :] = ext[h][mrow]
        for q in range(8):
            tabO[pb:pb + 64, hp, 1 + q, :] = ext[h][idxE[3 + q]]
        tabO[pb:pb + 64, hp, 9, :] = ext[h][mrow]
    return tabE.reshape(128, 4 * 15 * 64), tabO.reshape(128, 4 * 10 * 64)


class K:
    def __init__(self, nc):
        self.nc = nc
        self.P = Prog(nc)
        self.bank_rr = 0

    def dma(self, eng, out, in_, reads, writes, **kw):
        return self.P.add(eng, lambda e: e.dma_start(out=out, in_=in_, **kw), reads, writes, dma=True)

    def mm(self, out, lhsT, rhs, start, stop, reads, writes):
        return self.P.add("pe", lambda e: e.matmul(out, lhsT=lhsT, rhs=rhs, start=start, stop=stop), reads, writes)

    def tr(self, out, in_, ident, reads, writes):
        return self.P.add("pe", lambda e: e.transpose(out, in_, ident), reads, writes)

    def act(self, out, in_, func, reads, writes, eng="act", **kw):
        return self.P.add("act", lambda e: e.activation(out=out, in_=in_, func=func, **kw), reads, writes)

    def tt(self, eng, out, in0, in1, op, reads, writes):
        return self.P.add(eng, lambda e: e.tensor_tensor(out=out, in0=in0, in1=in1, op=op), reads, writes)

    def ts(self, eng, out, in0, s1, s2, op0, op1, reads, writes, **kw):
        return self.P.add(eng, lambda e: e.tensor_scalar(out=out, in0=in0, scalar1=s1, scalar2=s2, op0=op0, op1=op1, **kw), reads, writes)

    def stt(self, eng, out, in0, scalar, in1, op0, op1, reads, writes):
        return self.P.add(eng, lambda e: e.scalar_tensor_tensor(out=out, in0=in0, scalar=scalar, in1=in1, op0=op0, op1=op1), reads, writes)

    def cp(self, eng, out, in_, reads, writes):
        if eng == "act":
            return self.P.add("act", lambda e: e.activation(out=out, in_=in_, func=AF.Copy), reads, writes)
        return self.P.add(eng, lambda e: e.tensor_copy(out=out, in_=in_), reads, writes)

    def bank(self, b):
        return self.pd[b // 2][:, (b % 2) * 512:(b % 2 + 1) * 512]

    def bk(self, b):
        return ("pb", b)


def bc_rows(ap1d, nparts):
    n = ap1d.shape[-1]
    return bass.AP(ap1d.tensor, ap1d.offset, [[0, nparts], [1, n]])


def build_program(n_layers=DEPTH, stop=None, dbg=False, start=0):
    nc = bass.Bass("TRN2", target_bir_lowering=False)
    NL = n_layers
    NLa = (n_layers + 1) // 2
    NLr = max(n_layers // 2, 1)
    k = K(nc)
    P = k.P

    def din(name, shape, dt=F32):
        return nc.dram_tensor(name, list(shape), dt, kind="ExternalInput").ap()

    I = {}
    I["x"] = din("x", [S, D]); I["ctx"] = din("ctx", [NCTX, D]); I["c2"] = din("c2", [2, D])
    I["ada_w"] = din("ada_w", [NL, D, 6 * D]); I["ada_b"] = din("ada_b", [NL, 6 * D])
    I["ln_g"] = din("ln_g", [NL, 2, D]); I["ln_b"] = din("ln_b", [NL, 2, D])
    I["mix_w_out"] = din("mix_w_out", [NL, D, D])
    I["att_w_in"] = din("att_w_in", [2, D, ATT_IN])
    I["na_tabE"] = din("na_tabE", [2, 128, 3840]); I["na_tabO"] = din("na_tabO", [2, 128, 2560])
    I["qk_gain"] = din("qk_gain", [2, 2, HD])
    I["rec_w_in"] = din("rec_w_in", [2, D, REC_IN])
    I["rwkv_mu"] = din("rwkv_mu", [2, 6, 512]); I["rwkv_w0"] = din("rwkv_w0", [2, 2, 512])
    I["rwkv_w1"] = din("rwkv_w1", [2, 2, 512, 32]); I["rwkv_w2"] = din("rwkv_w2", [2, 2, 32, 512])
    I["rwkv_a0"] = din("rwkv_a0", [2, 2, 512]); I["rwkv_a1"] = din("rwkv_a1", [2, 2, 512, 32])
    I["rwkv_a2"] = din("rwkv_a2", [2, 2, 32, 512]); I["rwkv_g1"] = din("rwkv_g1", [2, 512, 96])
    I["rwkv_g2"] = din("rwkv_g2", [2, 96, 512]); I["rwkv_kvec"] = din("rwkv_kvec", [2, 2, 512])
    I["rwkv_rk"] = din("rwkv_rk", [2, 512]); I["rwkv_gn"] = din("rwkv_gn", [2, 2, 512])
    I["mlstm_gate_b"] = din("mlstm_gate_b", [2, 16]); I["mlstm_norm"] = din("mlstm_norm", [2, 512])
    I["moe_router"] = din("moe_router", [NL, D, NEXP]); I["moe_bias"] = din("moe_bias", [NL, NEXP])
    I["moe_w1"] = din("moe_w1", [NL, NEXP, D, 256]); I["moe_w3"] = din("moe_w3", [NL, NEXP, D, 256])
    I["moe_w2"] = din("moe_w2", [NL, NEXP, 256, D])
    I["shared_w1"] = din("shared_w1", [NL, D, 256]); I["shared_w3"] = din("shared_w3", [NL, D, 256])
    I["shared_w2"] = din("shared_w2", [NL, 256, D])
    for cn in CONST_NAMES:
        shp = [128, 2048] if cn in ("ropec", "ropes") else [128, 128]
        I[cn] = din("c_" + cn, shp)
    out = nc.dram_tensor("out", [S, D], F32, kind="ExternalOutput").ap()
    k.Xd = nc.dram_tensor("Xd", [T, D], F32, kind="Internal").ap()
    k.modrow = nc.dram_tensor("modrow", [DEPTH, 2, 6 * D], F32, kind="Internal").ap()
    k.rkv_d = nc.dram_tensor("rkv_d", [3, 512, T], F32, kind="Internal").ap()
    k.dbg = nc.dram_tensor("dbg", [128, 8, T], BF16, kind="ExternalOutput").ap() if dbg else None
    k.I = I
    k.out = out

    k.identf = P.sbuf("identf", [128, 128], F32)
    k.identb = P.sbuf("identb", [128, 128], BF16)
    k.onesb = P.sbuf("onesb", [128, 128], BF16)
    k.epsln = P.sbuf("epsln", [128, 1], F32)
    k.epsnm = P.sbuf("epsnm", [128, 1], F32)
    k.modT = P.sbuf("modT", [128, DEPTH, 48, 2], F32)
    k.AB = P.sbuf("AB", [128, 8, T], BF16)
    k.Gtm = P.sbuf("Gtm", [128, NT, NEXP], F32)
    k.pd = [P.psum(f"pd{i}", [128, 1024], F32) for i in range(4)]

    k.dma("sp", k.identf[:], I["ident"], [], ["identf"])
    k.cp("dve", k.identb[:], k.identf[:], ["identf"], ["identb"])
    P.add("pool", lambda e: e.memset(k.onesb[:], 1.0), [], ["onesb"])
    P.add("pool", lambda e: e.memset(k.epsln[:], LN_EPS), [], ["epsln"])
    P.add("pool", lambda e: e.memset(k.epsnm[:], NORM_EPS), [], ["epsnm"])

    k.n_layers = n_layers
    k.start = start
    prologue(k, n_layers)
    if stop == ("pro", 0):
        o = k.dma("sp", out[0:12, :], k.modrow[0].rearrange("j (a b) -> (j a) b", b=1024), [], ["out"])
        P.out_dmas.append(o)
        n_layers = 0
    for i in range(start, n_layers):
        last = (i == DEPTH - 1)
        if i == start:
            phase1(k, i, ntile=(stop[1] if (stop and stop[0] in ("p1", "p1x") and stop[1] > 0) else NT))
            if stop and stop[0] == "p1x":
                phase1(k, 0, ntile=1) if False else None
                o = k.dma("pool", out[0:128, :].rearrange("p (k n) -> p k n", k=8), k.AB[:, :, 0:128], [], ["out"])
                P.out_dmas.append(o)
                break
            if dbg and stop[0] == "p1":
                k.dma("sp", k.dbg, k.AB[:], [], ["dbg"])
                break
        if i % 2 == 0:
            attention_layer(k, i)
        else:
            recurrent_layer(k, i)
        if dbg and stop == ("m", i):
            k.dma("sp", k.dbg, k.AB[:], [("AB", t) for t in range(NT)], ["dbg"])
            break
        post(k, i, 0, last)
        if stop == ("xa", i):
            break
        with P.phase() as st:
            k.Yacc = P.sbuf("Yacc", [128, NT, D], F32, st)
            moe(k, i, last, st)
            post(k, i, 1, last)
        if stop == ("xb", i):
            break
    if stop is not None and stop[0] in ("p1", "m"):
        o = k.dma("sp", out[0:12, :], k.modrow[0].rearrange("j (a b) -> (j a) b", b=1024), [], ["out"])
        P.out_dmas.append(o)
    if not (n_layers == DEPTH and stop is None) and (stop is None or stop[0] in ("xa", "xb")):
        o = k.dma("sp", out, k.Xd[0:S, :], [("Xd", t) for t in range(16)], ["out"])
        P.out_dmas.append(o)
    P.flush(barrier=True)
    for o in P.out_dmas:
        nc.sync.wait_ge(o.sem, o.count)
    P.stack.close()
    return nc


def prologue(k, n_layers):
    P, I = k.P, k.I
    with P.phase() as st:
        c2T = P.sbuf("c2T", [128, 8, 2], F32, st)
        sT = P.sbuf("sT", [128, 8, 2], F32, st)
        wch = [P.sbuf(f"adaw{j}", [128, 8, 512], F32, st) for j in range(2)]
        b2 = [P.sbuf(f"adab{j}", [2, 512], F32, st) for j in range(2)]
        mrow = [P.sbuf(f"mrow{j}", [2, 512], F32, st) for j in range(2)]
        for jj in range(2):
            k.dma("sp", c2T[:, :, jj], I["c2"][jj, :].rearrange("(k p) -> p k", p=128), [], [("c2T", jj)], allow_slow_non_contiguous=True)
        k.act(sT[:], c2T[:], AF.Silu, [("c2T", 0), ("c2T", 1)], ["sT"])
        n = 0
        for i in range(n_layers):
            for cc in range(12):
                j = n % 2
                n += 1
                cols = slice(cc * 512, (cc + 1) * 512)
                k.dma("sp" if n % 2 else "pool", wch[j][:], I["ada_w"][i, :, cols].rearrange("(k p) n -> p k n", p=128), [], [("wch", j)])
                k.dma("sp", b2[j][:], bc_rows(I["ada_b"][i, cols], 2), [], [("b2", j)])
                b0, b1 = 2 * j, 2 * j + 1
                for kk in range(8):
                    k.mm(k.bank(b0)[0:2, :], sT[:, kk, :], wch[j][:, kk, :], kk == 0, kk == 7,
                         ["sT", ("wch", j)], [k.bk(b0)])
                k.tt("dve", mrow[j][:], k.bank(b0)[0:2, :], b2[j][:], ALU.add, [k.bk(b0), ("b2", j)], [("mrow", j)])
                k.dma("sp", k.modrow[i, :, cols], mrow[j][:], [("mrow", j)], [("modrow", i, cc)])
                for q in range(4):
                    k.mm(k.bank(b1)[:, q * 2:(q + 1) * 2], mrow[j][0:2, q * 128:(q + 1) * 128], k.identf[0:2, 0:2],
                         True, True, [("mrow", j), "identf"], [k.bk(b1)])
                k.cp("act", k.modT[:, i, cc * 4:(cc + 1) * 4, :],
                     k.bank(b1)[:, 0:8].rearrange("p (q j) -> p q j", j=2), [k.bk(b1)], [("modT", i, cc)])
        for i in range(n_layers):
            for lo in (8, 32):
                cs = [("modT", i, cc) for cc in range(lo // 4, lo // 4 + 2)]
                k.ts("dve", k.modT[:, i, lo:lo + 8, :], k.modT[:, i, lo:lo + 8, :], 1.0, None, ALU.add, ALU.bypass, cs, cs)


def x_src(k, i, t, first):
    if first:
        return k.I["x"][t * 128:(t + 1) * 128, :] if t < 16 else k.I["ctx"][(t - 16) * 128:(t - 15) * 128, :]
    return k.Xd[t * 128:(t + 1) * 128, :]


def emit_hT(k, xt, xkey, i, scb, shb, t, hf=None, hfkey=None):
    j = 0 if t < 16 else 1
    for kq in range(2):
        b = 4 + (k.bank_rr % 4)
        k.bank_rr += 1
        for q in range(4):
            kk = kq * 4 + q
            k.tr(k.bank(b)[:, q * 128:(q + 1) * 128], xt[:, kk * 128:(kk + 1) * 128], k.identf[:], [xkey, "identf"], [k.bk(b)])
        for q in range(4):
            kk = kq * 4 + q
            sc = k.modT[:, i, scb + kk, j:j + 1]
            sh = k.modT[:, i, shb + kk, j:j + 1]
            src = k.bank(b)[:, q * 128:(q + 1) * 128]
            if hf is not None:
                dst = hf[:, kk, :]
                wk = [(hfkey, kk)]
            else:
                dst = k.AB[:, kk, t * 128:(t + 1) * 128]
                wk = [("AB", t, kk)]
            if b % 2 == 0:
                k.ts("dve", dst, src, sc, sh, ALU.mult, ALU.add, [k.bk(b)], wk)
            else:
                k.act(dst, src, AF.Identity, [k.bk(b)], wk, scale=sc, bias=sh)


def ABk(t):
    return [("AB", t, kk) for kk in range(8)]


def phase1(k, i, ntile=NT):
    P = k.P
    with P.phase() as st:
        xts = [P.sbuf(f"p1x{j}", [128, D], F32, st) for j in range(3)]
        for t in range(ntile):
            j = t % 3
            k.dma("sp", xts[j][:], x_src(k, i, t, True), [("Xd", t)], [("p1x", j)])
            emit_hT(k, xts[j], ("p1x", j), i, 8, 0, t)


def post(k, i, which, last):
    P, I = k.P, k.I
    ntile = 16 if last else NT
    first = (i == k.start and which == 0)
    with P.phase() as st:
        gb = P.sbuf("gb", [128, 2, D], F32, st)
        lng = P.sbuf("lng", [128, D], F32, st)
        lnb = P.sbuf("lnb", [128, D], F32, st)
        goff = 2 * D if which == 0 else 5 * D
        for j in range(2):
            k.dma("sp", gb[:, j, :], bc_rows(k.modrow[i, j, goff:goff + D], 128), [], [("gb", j)])
        k.dma("sp", lng[:], bc_rows(I["ln_g"][i, which, :], 128), [], ["lng"])
        k.dma("sp", lnb[:], bc_rows(I["ln_b"][i, which, :], 128), [], ["lnb"])
        if which == 0:
            Wo = P.sbuf("Wo", [128, 8, D], BF16, st)
            for h in range(2):
                k.dma("pool", Wo[:, h * 4:(h + 1) * 4], I["mix_w_out"][i, h * 512:(h + 1) * 512, :].rearrange("(k p) n -> p k n", p=128), [], [("Wo", h)])
            rw = P.sbuf("rw", [128, 8, NEXP], F32, st)
            k.dma("sp", rw[:], I["moe_router"][i].rearrange("(k p) e -> p k e", p=128), [], ["rw"])
            rb = P.sbuf("rb", [128, NEXP], F32, st)
            k.dma("sp", rb[:], bc_rows(I["moe_bias"][i], 128), [], ["rb"])
            hf = [P.sbuf(f"hf{r}", [128, 8, 128], F32, st) for r in range(2)]
            rt = {n: [P.sbuf(f"rt_{n}{r}", [128, w], F32, st) for r in range(2)]
                  for n, w in (("scs", 64), ("sel", 64), ("top8", 8), ("msk", 64), ("gs", 64), ("den", 1), ("rden", 1), ("G", 64))}
        xt = [P.sbuf(f"xt{r}", [128, D], F32, st) for r in range(2)]
        t1 = [P.sbuf(f"t1{r}", [128, D], F32, st) for r in range(2)]
        z = [P.sbuf(f"z{r}", [128, D], F32, st) for r in range(2)]
        xn = [P.sbuf(f"xn{r}", [128, D], F32, st) for r in range(2)]
        xo = [P.sbuf(f"xo{r}", [128, D], F32, st) for r in range(2)]
        bst = [P.sbuf(f"bst{r}", [128, 12], F32, st) for r in range(2)]
        mv = [P.sbuf(f"mv{r}", [128, 2], F32, st) for r in range(2)]
        sd = [P.sbuf(f"sd{r}", [128, 1], F32, st) for r in range(2)]
        rstd = [P.sbuf(f"rstd{r}", [128, 1], F32, st) for r in range(2)]
        k.bank_rr = 4

        def stage_a(t):
            j = 0 if t < 16 else 1
            r = t % 2
            tl = slice(t * 128, (t + 1) * 128)
            k.dma("sp", xt[r][:], x_src(k, i, t, first), [("Xd", t)], [("xt", r)])
            if which == 0:
                Yb = k.pd[r]
                yk = [k.bk(2 * r), k.bk(2 * r + 1)]
                for half in range(2):
                    for kk in range(8):
                        k.mm(Yb[:, half * 512:(half + 1) * 512], k.AB[:, kk, tl], Wo[:, kk, half * 512:(half + 1) * 512],
                             kk == 0, kk == 7, ABk(t) + [("Wo", kk // 4)], [yk[half]])
                k.tt("dve", t1[r][:], Yb[:], gb[:, j, :], ALU.mult, yk + [("gb", j)], [("t1", r)])
            else:
                k.tt("dve", t1[r][:], k.Yacc[:, t, :], gb[:, j, :], ALU.mult, [("Yacc", t), ("gb", j)], [("t1", r)])
            k.stt("dve", z[r][:], xt[r][:], float(DN_ALPHA), t1[r][:], ALU.mult, ALU.add, [("xt", r), ("t1", r)], [("z", r)])
            for h in range(2):
                P.add("dve", lambda e, r=r, h=h: e.bn_stats(out=bst[r][:, h * 6:(h + 1) * 6], in_=z[r][:, h * 512:(h + 1) * 512]),
                      [("z", r)], [("bst", r, h)])
            P.add("dve", lambda e, r=r: e.bn_aggr(out=mv[r][:], in_=bst[r][:]), [("bst", r, 0), ("bst", r, 1)], [("mv", r)])
            k.act(sd[r][:], mv[r][:, 1:2], AF.Sqrt, [("mv", r), "epsln"], [("sd", r)], bias=k.epsln[:, 0:1], scale=1.0)
            P.add("dve", lambda e, r=r: e.reciprocal(out=rstd[r][:], in_=sd[r][:]), [("sd", r)], [("rstd", r)])
            k.ts("dve", xn[r][:], z[r][:], mv[r][:, 0:1], rstd[r][:, 0:1], ALU.subtract, ALU.mult,
                 [("z", r), ("mv", r), ("rstd", r)], [("xn", r)])
            k.tt("pool", xn[r][:], xn[r][:], lng[:], ALU.mult, [("xn", r), "lng"], [("xn", r)])
            k.tt("pool", xo[r][:], xn[r][:], lnb[:], ALU.add, [("xn", r), "lnb"], [("xo", r)])

        def stage_b(t):
            j = 0 if t < 16 else 1
            r = t % 2
            tl = slice(t * 128, (t + 1) * 128)
            if last and which == 1:
                o = k.dma("sp", k.out[tl, :], xo[r][:], [("xo", r)], [("out", t)])
                P.out_dmas.append(o)
                return
            k.dma("sp", k.Xd[tl, :], xo[r][:], [("xo", r)], [("Xd", t)])
            if which == 1:
                if i + 1 < k.n_layers:
                    emit_hT(k, xo[r], ("xo", r), i + 1, 8, 0, t)
                return
            emit_hT(k, xo[r], ("xo", r), i, 32, 24, t, hf=hf[r], hfkey=("hf", r))
            hk = [(("hf", r), kk) for kk in range(8)]
            k.cp("pool", k.AB[:, :, tl], hf[r][:], hk, ABk(t))
            b = 4 + (k.bank_rr % 4)
            k.bank_rr += 1
            for kk in range(8):
                k.mm(k.bank(b)[:, 0:NEXP], hf[r][:, kk, :], rw[:, kk, :], kk == 0, kk == 7, hk + ["rw"], [k.bk(b)])
            R = {n: rt[n][r] for n in rt}
            K_ = lambda n: ("rt", n, r)
            k.act(R["scs"][:], k.bank(b)[:, 0:NEXP], AF.Sigmoid, [k.bk(b)], [K_("scs")])
            k.tt("dve", R["sel"][:], R["scs"][:], rb[:], ALU.add, [K_("scs"), "rb"], [K_("sel")])
            P.add("dve", lambda e, R=R: e.max(out=R["top8"][:], in_=R["sel"][:]), [K_("sel")], [K_("top8")])
            k.ts("dve", R["msk"][:], R["sel"][:], R["top8"][:, TOPK - 1:TOPK], None, ALU.is_ge, ALU.bypass, [K_("sel"), K_("top8")], [K_("msk")])
            k.tt("dve", R["gs"][:], R["scs"][:], R["msk"][:], ALU.mult, [K_("scs"), K_("msk")], [K_("gs")])
            P.add("dve", lambda e, R=R: e.reduce_sum(out=R["den"][:], in_=R["gs"][:], axis=AX.X), [K_("gs")], [K_("den")])
            P.add("dve", lambda e, R=R: e.reciprocal(out=R["rden"][:], in_=R["den"][:]), [K_("den")], [K_("rden")])
            k.ts("dve", R["G"][:], R["gs"][:], R["rden"][:, 0:1], float(ROUTED_SCALE), ALU.mult, ALU.mult, [K_("gs"), K_("rden")], [K_("G")])
            k.cp("pool", k.Gtm[:, t, :], R["G"][:], [K_("G")], [("Gtm", t)])

        stage_a(0)
        for t in range(ntile):
            if t + 1 < ntile:
                stage_a(t + 1)
            stage_b(t)


def moe(k, i, last, st_unused=None):
    P, I = k.P, k.I
    ntile = 16 if last else NT
    nchunk = ntile // 2
    G = 2
    groups = [list(range(g * G, (g + 1) * G)) for g in range(NEXP // G)] + [["s"]]
    with P.phase() as st:
        w1b = [P.sbuf(f"w1b{j}", [128, G, 8, 256], BF16, st) for j in range(2)]
        w3b = [P.sbuf(f"w3b{j}", [128, G, 8, 256], BF16, st) for j in range(2)]
        w2b = [P.sbuf(f"w2b{j}", [128, G, 2, D], BF16, st) for j in range(2)]
        s1 = [P.sbuf(f"ms1{j}", [128, 2, 256], BF16, st) for j in range(2)]
        u = [P.sbuf(f"mu{j}", [128, 2, 256], BF16, st) for j in range(2)]
        h3s = [P.sbuf(f"mh3{j}", [128, 2, 256], BF16, st) for j in range(2)]

        def load(gi):
            j = gi % 2
            for ei, e in enumerate(groups[gi]):
                s1_ = I["shared_w1"][i] if e == "s" else I["moe_w1"][i, e]
                s3_ = I["shared_w3"][i] if e == "s" else I["moe_w3"][i, e]
                s2_ = I["shared_w2"][i] if e == "s" else I["moe_w2"][i, e]
                k.dma("pool", w1b[j][:, ei], s1_.rearrange("(k p) f -> p k f", p=128), [], [("w1b", j, ei)])
                k.dma("pool", w3b[j][:, ei], s3_.rearrange("(k p) f -> p k f", p=128), [], [("w3b", j, ei)])
                k.dma("pool", w2b[j][:, ei], s2_.rearrange("(f p) n -> p f n", p=128), [], [("w2b", j, ei)])

        units = [(gi, ci, ei, e) for gi, grp in enumerate(groups) for ci in range(nchunk) for ei, e in enumerate(grp)]

        def up(n):
            gi, ci, ei, e = units[n]
            j, x, c0 = gi % 2, n % 2, ci * 256
            for (wb, bnk, nm) in ((w1b, 4 + x, "w1b"), (w3b, 6 + x, "w3b")):
                for f in range(2):
                    for kk in range(8):
                        k.mm(k.bank(bnk)[:, f * 256:(f + 1) * 256], wb[j][:, ei, kk, f * 128:(f + 1) * 128],
                             k.AB[:, kk, c0:c0 + 256], kk == 0, kk == 7, [(nm, j, ei)], [k.bk(bnk)])

        def rest(n):
            gi, ci, ei, e = units[n]
            j, x = gi % 2, n % 2
            k.act(s1[x][:], k.bank(4 + x)[:, 0:512].rearrange("p (f n) -> p f n", f=2), AF.Silu, [k.bk(4 + x)], [("ms1", x)])
            k.cp("act", h3s[x][:], k.bank(6 + x)[:, 0:512].rearrange("p (f n) -> p f n", f=2), [k.bk(6 + x)], [("mh3", x)])
            k.tt("dve", u[x][:], h3s[x][:], s1[x][:], ALU.mult, [("mh3", x), ("ms1", x)], [("mu", x)])
            for tt in range(2):
                t = ci * 2 + tt
                for half in range(2):
                    for f in range(2):
                        k.mm(k.pd[tt][:, half * 512:(half + 1) * 512], u[x][:, f, tt * 128:(tt + 1) * 128],
                             w2b[j][:, ei, f, half * 512:(half + 1) * 512], f == 0, f == 1, [("mu", x), ("w2b", j, ei)], [k.bk(2 * tt + half)])
                yk = [k.bk(2 * tt), k.bk(2 * tt + 1)]
                gsc = 1.0 if e == "s" else k.Gtm[:, t, e:e + 1]
                if gi == 0 and ei == 0:
                    k.ts("dve", k.Yacc[:, t, :], k.pd[tt][:], gsc, None, ALU.mult, ALU.bypass, yk, [("Yacc", t)])
                else:
                    k.stt("dve", k.Yacc[:, t, :], k.pd[tt][:], gsc, k.Yacc[:, t, :], ALU.mult, ALU.add, yk + [("Yacc", t)], [("Yacc", t)])

        load(0)
        up(0)
        for n in range(len(units)):
            gi, ci, ei, e = units[n]
            if ci == 0 and ei == 0 and gi + 1 < len(groups):
                load(gi + 1)
            if n + 1 < len(units):
                up(n + 1)
            rest(n)


def attention_layer(k, i):
    P, I = k.P, k.I
    j = i // 2
    keep_ctx = i < DEPTH - 1
    Win = I["att_w_in"][j]
    with P.phase() as sa:
        QTa = P.sbuf("QTa", [128, 4, T], BF16, sa)
        KTa = P.sbuf("KTa", [128, 4, T], BF16, sa)
        Va = P.sbuf("Va", [128, NT, 512], BF16, sa)
        QTb = P.sbuf("QTb", [128, 4, T], BF16, sa)
        KTb = P.sbuf("KTb", [128, 2, T], BF16, sa)
        Vb = P.sbuf("Vb", [128, NT, 128], BF16, sa)
        with P.phase() as st:
            W = P.sbuf("Win", [128, 8, ATT_IN], BF16, st)
            for kk in range(8):
                k.dma("pool", W[:, kk, :], Win[kk * 128:(kk + 1) * 128, :], [], [("W", kk)])
            ropec = P.sbuf("ropec", [128, S], F32, st)
            ropes = P.sbuf("ropes", [128, S], F32, st)
            pm = P.sbuf("ropepm", [128, 128], F32, st)
            blk = P.sbuf("blk64", [128, 128], F32, st)
            gq = P.sbuf("gq", [128, 1], F32, st)
            gk = P.sbuf("gk", [128, 1], F32, st)
            k.dma("sp", ropec[:], I["ropec"], [], ["ropec"])
            k.dma("sp", ropes[:], I["ropes"], [], ["ropes"])
            k.dma("sp", pm[:], I["ropepm"], [], ["pm"])
            k.dma("sp", blk[:], I["blk64"], [], ["blk"])
            for h in range(2):
                k.dma("sp", gq[h * 64:(h + 1) * 64, :], I["qk_gain"][j, 0, :].rearrange("(d o) -> d o", o=1), [], [("gq", h)], allow_slow_non_contiguous=True)
                k.dma("sp", gk[h * 64:(h + 1) * 64, :], I["qk_gain"][j, 1, :].rearrange("(d o) -> d o", o=1), [], [("gk", h)], allow_slow_non_contiguous=True)
            k.ts("dve", gq[:], gq[:], 0.125, None, ALU.mult, ALU.bypass, [("gq", 0), ("gq", 1)], [("gq", 0), ("gq", 1)])
            gqk = [("gq", 0), ("gq", 1)]
            gkk = [("gk", 0), ("gk", 1)]
            tmp = {n: [P.sbuf(f"at_{n}{r}", [128, 512], F32, st) for r in range(2)] for n in ("sq", "sd", "xg", "xn", "t1")}
            tmp["rstd"] = tmp["sd"]
            tmp["t2"] = tmp["sq"]
            Wk = [("W", kk) for kk in range(8)]
            nrot = 0
            brr = 0
            tcs = [(0, 512), (512, 512), (1024, 512), (1536, 512), (2048, 256)]
            for (c0, cn) in tcs:
                for cc in range(14):
                    b = brr % 8
                    brr += 1
                    if cc >= 12:
                        for hh in range(2):
                            for kk in range(8):
                                lhs = W[:, kk, 2048 + (cc - 12) * 64:2048 + (cc - 11) * 64]
                                k.mm(k.bank(b)[hh * 64:(hh + 1) * 64, 0:cn], lhs, k.AB[:, kk, c0:c0 + cn], kk == 0, kk == 7, [("W", kk)], [k.bk(b)])
                    for kk in range(8):
                        if cc < 4:
                            lhs = W[:, kk, cc * 128:(cc + 1) * 128]
                        elif cc < 8:
                            lhs = W[:, kk, 512 + (cc - 4) * 128:512 + (cc - 3) * 128]
                        elif cc < 12:
                            lhs = W[:, kk, 1536 + (cc - 8) * 128:1536 + (cc - 7) * 128]
                        else:
                            break
                        k.mm(k.bank(b)[:, 0:cn], lhs, k.AB[:, kk, c0:c0 + cn], kk == 0, kk == 7, [("W", kk)], [k.bk(b)])
                    src = k.bank(b)[:, 0:cn]
                    if cc < 4:
                        k.act(QTa[:, cc, c0:c0 + cn], src, AF.Copy, [k.bk(b)], [("QTa", cc, c0)], scale=0.125)
                        continue
                    if cc < 8:
                        k.cp("dve", KTa[:, cc - 4, c0:c0 + cn], src, [k.bk(b)], [("KTa", cc, c0)])
                        continue
                    is_q = cc < 12
                    dest = QTb[:, cc - 8, c0:c0 + cn] if is_q else KTb[:, cc - 12, c0:c0 + cn]
                    dk = [("QKb", cc, c0)]
                    gain, gkeys = (gq, gqk) if is_q else (gk, gkk)
                    r = nrot % 2
                    nrot += 1
                    tk = lambda n: ("at", {"rstd": "sd", "t2": "sq"}.get(n, n), r)
                    tv = lambda n: tmp[n][r][:, 0:cn]
                    k.act(tv("sq"), src, AF.Square, [k.bk(b)], [tk("sq")])
                    b2 = brr % 8
                    brr += 1
                    k.mm(k.bank(b2)[:, 0:cn], blk[:], tv("sq"), True, True, [tk("sq"), "blk"], [k.bk(b2)])
                    k.act(tv("sd"), k.bank(b2)[:, 0:cn], AF.Sqrt, [k.bk(b2), "epsnm"], [tk("sd")], scale=1.0 / 64, bias=k.epsnm[:, 0:1])
                    P.add("dve", lambda e, o=tv("rstd"), a=tv("sd"): e.reciprocal(out=o, in_=a), [tk("sd")], [tk("sd")])
                    k.act(tv("xg"), src, AF.Identity, [k.bk(b)] + gkeys, [tk("xg")], scale=gain[:, 0:1])
                    if c0 < S:
                        k.tt("pool", tv("xn"), tv("xg"), tv("rstd"), ALU.mult, [tk("xg"), tk("rstd")], [tk("xn")])
                        b3 = brr % 8
                        brr += 1
                        k.mm(k.bank(b3)[:, 0:cn], pm[:], tv("xn"), True, True, [tk("xn"), "pm"], [k.bk(b3)])
                        k.tt("pool", tv("t1"), tv("xn"), ropec[:, c0:c0 + cn], ALU.mult, [tk("xn"), "ropec"], [tk("t1")])
                        k.tt("dve", tv("t2"), k.bank(b3)[:, 0:cn], ropes[:, c0:c0 + cn], ALU.mult, [k.bk(b3), "ropes"], [tk("t2")])
                        k.tt("pool", dest, tv("t1"), tv("t2"), ALU.add, [tk("t1"), tk("t2")], dk)
                    else:
                        k.tt("pool", dest, tv("xg"), tv("rstd"), ALU.mult, [tk("xg"), tk("rstd")], dk)
            for t in range(NT):
                tl = slice(t * 128, (t + 1) * 128)
                b = brr % 8
                brr += 1
                for kk in range(8):
                    k.mm(k.bank(b)[:, 0:512], k.AB[:, kk, tl], W[:, kk, 1024:1536], kk == 0, kk == 7, [("W", kk)], [k.bk(b)])
                k.cp("act", Va[:, t, :], k.bank(b)[:, 0:512], [k.bk(b)], [("Va", t)])
                b = brr % 8
                brr += 1
                for kk in range(8):
                    k.mm(k.bank(b)[:, 0:128], k.AB[:, kk, tl], W[:, kk, 2176:2304], kk == 0, kk == 7, [("W", kk)], [k.bk(b)])
                k.cp("dve", Vb[:, t, :], k.bank(b)[:, 0:128], [k.bk(b)], [("Vb", t)])
        with P.phase() as st:
            tabE = P.sbuf("tabE", [128, 3840], F32, st)
            tabO = P.sbuf("tabO", [128, 2560], F32, st)
            k.dma("sp", tabE[:], I["na_tabE"][j], [], ["tabE"])
            k.dma("sp", tabO[:], I["na_tabO"][j], [], ["tabO"])
            Sb = [P.sbuf(f"naS{r}", [128, 896], F32, st) for r in range(2)]
            Pe = [P.sbuf(f"naP{r}", [128, 896], BF16, st) for r in range(2)]
            Pn = [P.sbuf(f"naN{r}", [128, 896], BF16, st) for r in range(2)]
            PT = [P.sbuf(f"naT{r}", [128, 896], BF16, st) for r in range(2)]
            sm = {n: [P.sbuf(f"na_{n}{r}", [128, 1], F32, st) for r in range(2)] for n in ("mx", "nmx", "rs", "ri")}
            units = []
            for r_ in range(32):
                sr = min(max(r_ - 4, 0), 24)
                if sr % 2 == 0:
                    a0, nrow, odd, d0 = sr, 8, False, sr - r_ + 7
                else:
                    a0, nrow, odd, d0 = sr - 1, 10, True, 0
                units.append((r_ * 64, a0 * 64, nrow * 64, odd, d0))
            if keep_ctx:
                for cq in range(4):
                    units.append((S + cq * 64, 0, 0, False, 0))
            ulist = [(q0, k0, nlat, odd, d0, hp) for (q0, k0, nlat, odd, d0) in units for hp in range(4)]

            def na_s(n):
                q0, k0, nlat, odd, d0, hp = ulist[n]
                nk = nlat + NCTX
                r = n % 2
                kS = ("naS", r)
                for hh in range(2):
                    ps = slice(hh * 64, (hh + 1) * 64)
                    bA, bB = 2 * hh, 2 * hh + 1
                    q = QTa[ps, hp, q0:q0 + 64]
                    if nlat:
                        k.mm(k.bank(bA)[ps, 0:512], q, KTa[ps, hp, k0:k0 + 512], True, True, [], [k.bk(bA)])
                        if nlat > 512:
                            k.mm(k.bank(bB)[ps, 0:128], q, KTa[ps, hp, k0 + 512:k0 + 640], True, True, [], [k.bk(bB)])
                        xo_ = nlat - 512
                        k.mm(k.bank(bB)[ps, xo_:xo_ + NCTX], q, KTa[ps, hp, S:T], True, True, [], [k.bk(bB)])
                        if odd:
                            bias = tabO[ps, hp * 640:(hp + 1) * 640]
                        else:
                            bias = tabE[ps, (hp * 15 + d0) * 64:(hp * 15 + d0 + 8) * 64]
                        k.tt("dve", Sb[r][ps, 0:512], k.bank(bA)[ps, 0:512], bias[:, 0:512], ALU.add,
                             [k.bk(bA), "tabE", "tabO"], [(kS, hh, 0)])
                        if nlat > 512:
                            k.tt("dve", Sb[r][ps, 512:640], k.bank(bB)[ps, 0:128], bias[:, 512:640], ALU.add,
                                 [k.bk(bB), "tabO"], [(kS, hh, 1)])
                        k.cp("dve", Sb[r][ps, nlat:nk], k.bank(bB)[ps, xo_:xo_ + NCTX], [k.bk(bB)], [(kS, hh, 2)])
                    else:
                        k.mm(k.bank(bA)[ps, 0:NCTX], q, KTa[ps, hp, S:T], True, True, [], [k.bk(bA)])
                        k.cp("act", Sb[r][ps, 0:NCTX], k.bank(bA)[ps, 0:NCTX], [k.bk(bA)], [(kS, hh, 2)])

            def na_rest(n):
                q0, k0, nlat, odd, d0, hp = ulist[n]
                nk = nlat + NCTX
                r = n % 2
                kS, kP, kN, kT = ("naS", r), ("naP", r), ("naN", r), ("naT", r)
                sk = [(kS, hh, x) for hh in range(2) for x in range(3)]
                M = {nm: sm[nm][r] for nm in sm}
                mk = lambda nm: ("nasm", nm, r)
                P.add("dve", lambda e, o=M["mx"], a=Sb[r], nk=nk: e.reduce_max(out=o[:], in_=a[:, 0:nk], axis=AX.X), sk, [mk("mx")])
                k.ts("dve", M["nmx"][:], M["mx"][:], -1.0, None, ALU.mult, ALU.bypass, [mk("mx")], [mk("nmx")])
                P.add("act", lambda e, o=Pe[r], a=Sb[r], nk=nk, nm=M["nmx"], rs=M["rs"]: e.activation(
                    out=o[:, 0:nk], in_=a[:, 0:nk], func=AF.Exp, bias=nm[:, 0:1], scale=1.0, accum_out=rs[:, 0:1]),
                    sk + [mk("nmx")], [kP, mk("rs")])
                P.add("dve", lambda e, o=M["ri"], a=M["rs"]: e.reciprocal(out=o[:], in_=a[:]), [mk("rs")], [mk("ri")])
                k.ts("dve", Pn[r][:, 0:nk], Pe[r][:, 0:nk], M["ri"][:, 0:1], None, ALU.mult, ALU.bypass, [kP, mk("ri")], [kN])
                nch = nk // 128
                bT = 4 + (n % 2)
                ptb = k.bank(bT).bitcast(BF16)
                for c in range(nch):
                    k.tr(ptb[:, c * 128:(c + 1) * 128], Pn[r][:, c * 128:(c + 1) * 128], k.identb[:], [kN], [k.bk(bT)])
                k.cp("act", PT[r][:, 0:nk], ptb[:, 0:nk], [k.bk(bT)], [kT])
                bO = 6 + (n % 2)
                for hh in range(2):
                    ps = slice(hh * 64, (hh + 1) * 64)
                    h = 2 * hp + hh
                    for c in range(nch):
                        if c < nlat // 128:
                            vt = k0 // 128 + c
                        else:
                            vt = 16 + (c - nlat // 128)
                        k.mm(k.bank(bO)[ps, 0:64], Va[:, vt, h * 64:(h + 1) * 64], PT[r][:, c * 128 + hh * 64:c * 128 + hh * 64 + 64],
                             c == 0, c == nch - 1, [kT], [k.bk(bO)])
                k.cp("act", k.AB[:, hp, q0:q0 + 64], k.bank(bO)[:, 0:64], [k.bk(bO)], [("mT", hp, q0)])

            na_s(0)
            for n in range(len(ulist)):
                if n + 1 < len(ulist):
                    na_s(n + 1)
                na_rest(n)
        with P.phase() as st:
            E = [[P.sbuf(f"gqE{r}{hh}", [128, 512], BF16, st) for hh in range(2)] for r in range(2)]
            rc = P.sbuf("gqrc", [128, 512], F32, st)
            gunits = [(hp, c * 512, 512, list(range(NT))) for hp in range(4) for c in range(4)]
            if keep_ctx:
                gunits += [(hp, S, NCTX, [16, 17]) for hp in range(4)]
            n = 0
            for (hp, q0, nq, kts) in gunits:
                g = hp // 2
                def gq_s(ki, r):
                    kt = kts[ki]
                    for hh in range(2):
                        ps = slice(hh * 64, (hh + 1) * 64)
                        bS = 2 * r + hh
                        k.mm(k.bank(bS)[:, 0:nq], KTb[ps, g, kt * 128:(kt + 1) * 128], QTb[ps, hp, q0:q0 + nq], True, True, [], [k.bk(bS)])
                        k.act(E[r][hh][:, 0:nq], k.bank(bS)[:, 0:nq], AF.Exp, [k.bk(bS)], [("gqE", r, hh)])

                def gq_pv(ki, r):
                    kt = kts[ki]
                    for hh in range(2):
                        ps = slice(hh * 64, (hh + 1) * 64)
                        k.mm(k.bank(4 + hh)[ps, 0:nq], Vb[:, kt, g * 64:(g + 1) * 64], E[r][hh][:, 0:nq], ki == 0, ki == len(kts) - 1,
                             [("gqE", r, hh)], [k.bk(4 + hh)])
                        k.mm(k.bank(6 + hh)[ps, 0:nq], k.onesb[:, 0:64], E[r][hh][:, 0:nq], ki == 0, ki == len(kts) - 1,
                             [("gqE", r, hh), "onesb"], [k.bk(6 + hh)])

                gq_s(0, n % 2)
                for ki in range(len(kts)):
                    r = n % 2
                    n += 1
                    if ki + 1 < len(kts):
                        gq_s(ki + 1, n % 2)
                    gq_pv(ki, r)
                for hh in range(2):
                    ps = slice(hh * 64, (hh + 1) * 64)
                    P.add("dve", lambda e, o=rc[ps, 0:nq], a=k.bank(6 + hh)[ps, 0:nq]: e.reciprocal(out=o, in_=a), [k.bk(6 + hh)], [("gqrc", hh)])
                    k.tt("dve", k.AB[ps, 4 + hp, q0:q0 + nq], k.bank(4 + hh)[ps, 0:nq], rc[ps, 0:nq], ALU.mult,
                         [k.bk(4 + hh), ("gqrc", hh)], [("mT", 4 + hp, q0, hh)])


_CACHE = {}


def make_in_maps(inputs):
    f = lambda a: np.ascontiguousarray(np.asarray(a, dtype=np.float32))
    shared = {}
    for n in ("ada_w", "ada_b", "ln_g", "ln_b", "mix_w_out", "att_w_in", "qk_gain", "rec_w_in", "rwkv_mu", "rwkv_w0",
              "rwkv_w1", "rwkv_w2", "rwkv_a0", "rwkv_a1", "rwkv_a2", "rwkv_g1", "rwkv_g2", "rwkv_kvec", "rwkv_gn",
              "mlstm_norm", "moe_router", "moe_bias", "moe_w1", "moe_w3", "moe_w2", "shared_w1", "shared_w3", "shared_w2"):
        shared[n] = f(inputs[n])
    shared["rwkv_rk"] = f(inputs["rwkv_rk"]).reshape(2, 512)
    shared["mlstm_gate_b"] = f(inputs["mlstm_gate_b"]).reshape(2, 16)
    rpb = f(inputs["na_rpb"])
    tabs = [na_bias_tables(rpb[j]) for j in range(rpb.shape[0])]
    shared["na_tabE"] = np.ascontiguousarray(np.stack([t[0] for t in tabs]))
    shared["na_tabO"] = np.ascontiguousarray(np.stack([t[1] for t in tabs]))
    hc = host_constants()
    for cn in CONST_NAMES:
        shared["c_" + cn] = np.ascontiguousarray(hc[cn])
    x = f(inputs["x"]); c = f(inputs["c"]); ctx = f(inputs["ctx"]); c_ctx = f(inputs["c_ctx"])
    maps = []
    for b in range(x.shape[0]):
        m = dict(shared)
        m["x"] = np.ascontiguousarray(x[b])
        m["ctx"] = np.ascontiguousarray(ctx[b])
        m["c2"] = np.ascontiguousarray(np.stack([c[b], c_ctx]))
        maps.append(m)
    return maps


def kernel(**inputs):
    maps = make_in_maps(inputs)
    if "nc" not in _CACHE:
        _CACHE["nc"] = build_program()
    nc = _CACHE["nc"]
    res = run_bass_kernel_spmd(nc, maps, core_ids=list(range(len(maps))))
    return np.stack([np.asarray(r["out"], dtype=np.float32) for r in res.results])


def tokview(t2d, row_elems, base, start, step, n, parts=128, p0=0):
    return bass.AP(t2d, p0 * row_elems + base + start, [[row_elems, parts], [step, n]])


def proc_tiles(d):
    if d == 0:
        return [(t * 128, 1) for t in (16, 17)] + [(t * 128, 1) for t in range(16)]
    return [(T - 1 - u * 128, -1) for u in range(NT)]


def recurrent_layer(k, i):
    P, I = k.P, k.I
    with P.phase() as sl:
        mTt = P.sbuf("mTt", [128, 8, T], BF16, sl) if False else None
        mTm = P.sbuf("mTm", [128, 4, T], BF16, sl)
        cm = {}
        for cn in ("m_le", "m_lt", "m_le128", "m_gt128", "ones", "blk64"):
            cm[cn] = P.sbuf("c" + cn, [128, 128], F32, sl)
            k.dma("sp", cm[cn][:], I[cn], [], [("cm", cn)])
        P.flush(barrier=True)
        mlstm_mixer(k, i, mTm, cm)
        rwkv_mixer(k, i, cm)
        with P.phase():
            for c in range(4):
                k.cp("pool" if c % 2 else "dve", k.AB[:, 4 + c, :], mTm[:, c, :], [], [("AB", 4 + c)])


def mlstm_mixer(k, i, mTt, cm):
    P, I = k.P, k.I
    j = i // 2
    Wr = I["rec_w_in"][j]
    ABt = k.AB
    RE = 8 * T
    with P.phase() as sm:
        Wg = P.sbuf("Wg", [128, 8, 16], BF16, sm)
        k.dma("pool", Wg[:], Wr[:, 4096:4112].rearrange("(k p) n -> p k n", p=128), [], ["Wg"])
        gb = P.sbuf("gateb", [128, 16], F32, sm)
        k.dma("sp", gb[:], bc_rows(I["mlstm_gate_b"][j, :], 128), [], ["gateb"])
        ng = P.sbuf("normg", [128, 4], F32, sm)
        k.dma("sp", ng[:], I["mlstm_norm"][j, :].rearrange("(h p) -> p h", p=128), [], ["normg"], allow_slow_non_contiguous=True)
        IG = [P.sbuf(f"IG{d}", [128, NT, 4], F32, sm) for d in range(2)]
        LF = [P.sbuf(f"LF{d}", [128, NT, 4], F32, sm) for d in range(2)]
        WI = [P.sbuf(f"WI{d}", [128, NT, 4], F32, sm) for d in range(2)]
        WS = [P.sbuf(f"WS{d}", [128, NT, 4], F32, sm) for d in range(2)]
        WC = [P.sbuf(f"WC{d}", [128, NT, 4], F32, sm) for d in range(2)]
        gt = [P.sbuf(f"gtmp{r}", [128, 8], F32, sm) for r in range(2)]
        hTr = P.sbuf("hTr", [128, 8, T], BF16, sm)
        for kk in range(8):
            k.cp("pool" if kk % 2 else "dve", hTr[:, kk, :], tokview(ABt, RE, kk * T, T - 1, -1, T), [], [("hTr", kk)])
        hkeys = [("hTr", kk) for kk in range(8)]

        def hview(d, u, st0, kk):
            return k.AB[:, kk, st0:st0 + 128] if d == 0 else hTr[:, kk, u * 128:(u + 1) * 128]
        n = 0
        for d in range(2):
            for u, (st0, step) in enumerate(proc_tiles(d)):
                r = n % 2
                b = n % 8
                n += 1
                for kk in range(8):
                    k.mm(k.bank(b)[:, 0:16], hview(d, u, st0, kk), Wg[:, kk, :], kk == 0, kk == 7, ["Wg"] + hkeys, [k.bk(b)])
                c0 = 8 * d
                k.tt("dve", IG[d][:, u, :], k.bank(b)[:, c0:c0 + 4], gb[:, c0:c0 + 4], ALU.add, [k.bk(b), "gateb"], [("IG", d, u)])
                k.tt("dve", gt[r][:, 0:4], k.bank(b)[:, c0 + 4:c0 + 8], gb[:, c0 + 4:c0 + 8], ALU.add, [k.bk(b), "gateb"], [("gt", r)])
                k.act(gt[r][:, 4:8], gt[r][:, 0:4], AF.Exp, [("gt", r)], [("gt2", r)], scale=-1.0)
                k.act(gt[r][:, 0:4], gt[r][:, 4:8], AF.Ln, [("gt2", r), ("gt", r)], [("gt", r)], bias=1.0, scale=1.0)
                k.ts("dve", LF[d][:, u, :], gt[r][:, 0:4], -1.0, None, ALU.mult, ALU.bypass, [("gt", r)], [("LF", d, u)])
                b2 = n % 8
                n += 1
                k.mm(k.bank(b2)[:, 0:4], cm["m_le128"][:], LF[d][:, u, :], True, True, [("LF", d, u), ("cm", "m_le128")], [k.bk(b2)])
                k.mm(k.bank(b2)[:, 4:8], cm["ones"][:], LF[d][:, u, :], True, True, [("LF", d, u), ("cm", "ones")], [k.bk(b2)])
                k.act(WI[d][:, u, :], k.bank(b2)[:, 0:4], AF.Exp, [k.bk(b2)], [("WI", d, u)])
                k.act(WC[d][:, u, :], k.bank(b2)[:, 4:8], AF.Exp, [k.bk(b2)], [("WC", d, u)])
                k.act(gt[r][:, 4:8], k.bank(b2)[:, 4:8], AF.Copy, [k.bk(b2), ("gt2", r)], [("gt2", r)])
                k.act(gt[r][:, 0:4], k.bank(b2)[:, 0:4], AF.Copy, [k.bk(b2), ("gt", r)], [("gt", r)])
                k.tt("dve", gt[r][:, 4:8], gt[r][:, 4:8], gt[r][:, 0:4], ALU.subtract, [("gt", r), ("gt2", r)], [("gt2", r)])
                k.tt("dve", gt[r][:, 4:8], gt[r][:, 4:8], IG[d][:, u, :], ALU.add, [("gt2", r), ("IG", d, u)], [("gt2", r)])
                k.act(WS[d][:, u, :], gt[r][:, 4:8], AF.Exp, [("gt2", r)], [("WS", d, u)])
        P.flush(barrier=True)
        for h in range(4):
            with P.phase() as st:
                Wq = P.sbuf("mWq", [128, 8, 128], BF16, st)
                Wk = P.sbuf("mWk", [128, 8, 128], BF16, st)
                Wv = P.sbuf("mWv", [128, 8, 128], BF16, st)
                Wo = P.sbuf("mWo", [128, 8, 128], BF16, st)
                for (w, off, nm) in ((Wq, 2048, "q"), (Wk, 2560, "k"), (Wv, 3072, "v"), (Wo, 3584, "o")):
                    k.dma("pool", w[:], Wr[:, off + h * 128:off + (h + 1) * 128].rearrange("(k p) n -> p k n", p=128), [], [("mW", nm)])
                QT = P.sbuf("mQT", [128, T], BF16, st)
                KT = P.sbuf("mKT", [128, T], BF16, st)
                SO = P.sbuf("mSO", [128, T], BF16, st)
                HS = P.sbuf("mHS", [128, T], F32, st)
                Kt = [P.sbuf(f"mKt{d}", [128, NT, 128], BF16, st) for d in range(2)]
                Vt = [P.sbuf(f"mVt{d}", [128, NT, 129], BF16, st) for d in range(2)]
                Cs = [P.sbuf(f"mC{d}", [128, 129], F32, st) for d in range(2)]
                Cbs = [P.sbuf(f"mCb{d}", [128, 129], BF16, st) for d in range(2)]
                tm = {nm: [P.sbuf(f"mt_{nm}{r}", [128, 132], F32, st) for r in range(2)] for nm in ("R", "eD", "eDm", "tmp", "num", "hq")}
                tb = {nm: [P.sbuf(f"mtb_{nm}{r}", [128, 128], BF16, st) for r in range(2)] for nm in ("Sg", "Kw")}
                sm1 = {nm: [P.sbuf(f"ms_{nm}{r}", [128, 1], F32, st) for r in range(2)] for nm in ("dn", "rdn")}
                fz = {nm: P.sbuf(f"mf_{nm}", [128, 512], F32, st) for nm in ("sq", "sd", "o1")}
                brr = 0
                for (c0, cn) in [(0, 512), (512, 512), (1024, 512), (1536, 512), (2048, 256)]:
                    for (w, nm) in ((Wq, "q"), (Wk, "k"), (Wo, "o")):
                        b = brr % 8
                        brr += 1
                        for kk in range(8):
                            k.mm(k.bank(b)[:, 0:cn], w[:, kk, :], k.AB[:, kk, c0:c0 + cn], kk == 0, kk == 7, [("mW", nm)], [k.bk(b)])
                        if nm == "q":
                            k.act(QT[:, c0:c0 + cn], k.bank(b)[:, 0:cn], AF.Copy, [k.bk(b)], [("mQT", c0)], scale=float(128 ** -0.5))
                        elif nm == "k":
                            k.cp("dve", KT[:, c0:c0 + cn], k.bank(b)[:, 0:cn], [k.bk(b)], [("mKT", c0)])
                        else:
                            k.act(SO[:, c0:c0 + cn], k.bank(b)[:, 0:cn], AF.Sigmoid, [k.bk(b)], [("mSO", c0)])
                for d in range(2):
                    P.add("pool", lambda e, d=d: e.memset(Vt[d][:, :, 128:129], 1.0), [], [("mVt1", d)])
                    for u, (st0, step) in enumerate(proc_tiles(d)):
                        for (w, nm) in ((Wk, "k"), (Wv, "v")):
                            b = brr % 8
                            brr += 1
                            for kk in range(8):
                                k.mm(k.bank(b)[:, 0:128], hview(d, u, st0, kk), w[:, kk, :], kk == 0, kk == 7, [("mW", nm)], [k.bk(b)])
                            if nm == "k":
                                k.cp("act", Kt[d][:, u, :], k.bank(b)[:, 0:128], [k.bk(b)], [("mKt", d, u)])
                            else:
                                k.cp("dve", Vt[d][:, u, 0:128], k.bank(b)[:, 0:128], [k.bk(b)], [("mVt", d, u)])
                qk_keys = [("mQT", c0) for c0 in (0, 512, 1024, 1536, 2048)] + [("mKT", c0) for c0 in (0, 512, 1024, 1536, 2048)]
                QTr = P.sbuf("mQTr", [128, T], BF16, st)
                KTr = P.sbuf("mKTr", [128, T], BF16, st)
                k.cp("pool", QTr[:], tokview(QT, T, 0, T - 1, -1, T), qk_keys, ["mQTr"])
                k.cp("pool", KTr[:], tokview(KT, T, 0, T - 1, -1, T), qk_keys, ["mKTr"])
                qk_keys = qk_keys + ["mQTr", "mKTr"]
                P.add("pool", lambda e: e.memset(HS[:], 0.0), [], [("mHS", t_) for t_ in range(NT)])
                for d in range(2):
                    P.add("dve", lambda e, d=d: e.memset(Cs[d][:], 0.0), [], [("mC", d)])
                    P.add("pool", lambda e, d=d: e.memset(Cbs[d][:], 0.0), [], [("mCb", d)])
                ptl = [proc_tiles(0), proc_tiles(1)]
                for u in range(NT):
                    for d in range(2):
                        st0, step = ptl[d][u]
                        r = d
                        C, Cb = Cs[d], Cbs[d]
                        stile = (st0 // 128) if d == 0 else (NT - 1 - u)
                        tk = lambda nm, r=r: ("mt", nm, r)
                        if d == 0:
                            qv, kv = QT[:, st0:st0 + 128], KT[:, st0:st0 + 128]
                        else:
                            qv, kv = QTr[:, u * 128:(u + 1) * 128], KTr[:, u * 128:(u + 1) * 128]
                        bS, bD, bN, bI = 4 * d, 4 * d + 1, 4 * d + 2, 4 * d + 3
                        bT, bC = bD, bS
                        k.mm(k.bank(bS)[:, 0:128], kv, qv, True, True, qk_keys, [k.bk(bS)])
                        k.ts("dve", tm["R"][r][:, 0:128], cm["m_le128"][:], LF[d][:, u, h:h + 1], None, ALU.mult, ALU.bypass, [], [tk("R")])
                        k.mm(k.bank(bD)[:, 0:128], cm["m_gt128"][:], tm["R"][r][:, 0:128], True, True, [tk("R")], [k.bk(bD)])
                        k.act(tm["eD"][r][:, 0:128], k.bank(bD)[:, 0:128], AF.Exp, [k.bk(bD)], [tk("eD")], bias=IG[d][:, u, h:h + 1], scale=1.0)
                        k.tt("pool", tm["eDm"][r][:, 0:128], tm["eD"][r][:, 0:128], cm["m_le128"][:], ALU.mult, [tk("eD")], [tk("eDm")])
                        k.tt("dve", tb["Sg"][r][:], k.bank(bS)[:, 0:128], tm["eDm"][r][:, 0:128], ALU.mult, [k.bk(bS), tk("eDm")], [tk("Sg")])
                        k.mm(k.bank(bN)[:, 0:129], tb["Sg"][r][:], Vt[d][:, u, :], True, True, [tk("Sg"), ("mVt", d, u), ("mVt1", d)], [k.bk(bN)])
                        k.mm(k.bank(bI)[:, 0:129], qv, Cb[:], True, True, qk_keys + [("mCb", d)], [k.bk(bI)])
                        k.ts("dve", tm["tmp"][r][:, 0:129], k.bank(bI)[:, 0:129], WI[d][:, u, h:h + 1], None, ALU.mult, ALU.bypass, [k.bk(bI)], [tk("tmp")])
                        k.tt("dve", tm["num"][r][:, 0:129], k.bank(bN)[:, 0:129], tm["tmp"][r][:, 0:129], ALU.add, [k.bk(bN), tk("tmp")], [tk("num")])
                        k.act(sm1["dn"][r][:], tm["num"][r][:, 128:129], AF.Abs, [tk("num")], [tk("dn")])
                        k.ts("dve", sm1["dn"][r][:], sm1["dn"][r][:], 1.0, None, ALU.max, ALU.bypass, [tk("dn")], [tk("dn")])
                        P.add("dve", lambda e, o=sm1["rdn"][r], a=sm1["dn"][r]: e.reciprocal(out=o[:], in_=a[:]), [tk("dn")], [tk("rdn")])
                        k.ts("dve", tm["hq"][r][:, 0:128], tm["num"][r][:, 0:128], sm1["rdn"][r][:, 0:1], None, ALU.mult, ALU.bypass, [tk("num"), tk("rdn")], [tk("hq")])
                        k.tr(k.bank(bT)[:, 0:128], tm["hq"][r][:, 0:128], k.identf[:], [tk("hq")], [k.bk(bT)])
                        hv = tokview(HS, T, 0, st0, step, 128)
                        k.tt("dve", hv, k.bank(bT)[:, 0:128], hv, ALU.add, [k.bk(bT), ("mHS", stile)], [("mHS", stile)])
                        k.ts("pool", tb["Kw"][r][:], Kt[d][:, u, :], WS[d][:, u, h:h + 1], None, ALU.mult, ALU.bypass, [("mKt", d, u)], [tk("Kw")])
                        k.mm(k.bank(bC)[:, 0:129], tb["Kw"][r][:], Vt[d][:, u, :], True, True, [tk("Kw"), ("mVt", d, u), ("mVt1", d)], [k.bk(bC)])
                        k.stt("dve", C[:], C[:], WC[d][:, u, h:h + 1], k.bank(bC)[:, 0:129], ALU.mult, ALU.add, [("mC", d), k.bk(bC)], [("mC", d)])
                        k.cp("pool", Cb[:], C[:], [("mC", d)], [("mCb", d)])
                hk = [("mHS", u) for u in range(NT)]
                for (c0, cn) in [(0, 512), (512, 512), (1024, 512), (1536, 512), (2048, 256)]:
                    b = brr % 2
                    brr += 1
                    k.act(fz["sq"][:, 0:cn], HS[:, c0:c0 + cn], AF.Square, hk, ["mfsq"])
                    k.mm(k.bank(b)[:, 0:cn], cm["ones"][:], fz["sq"][:, 0:cn], True, True, ["mfsq"], [k.bk(b)])
                    k.act(fz["sd"][:, 0:cn], k.bank(b)[:, 0:cn], AF.Sqrt, [k.bk(b)], ["mfsd"], scale=1.0 / 128, bias=k.epsnm[:, 0:1])
                    P.add("dve", lambda e, o=fz["sd"][:, 0:cn]: e.reciprocal(out=o, in_=o), ["mfsd"], ["mfsd"])
                    k.tt("pool", fz["o1"][:, 0:cn], HS[:, c0:c0 + cn], fz["sd"][:, 0:cn], ALU.mult, hk + ["mfsd"], ["mfo1"])
                    k.stt("dve", mTt[:, h, c0:c0 + cn], fz["o1"][:, 0:cn], ng[:, h:h + 1], SO[:, c0:c0 + cn], ALU.mult, ALU.mult,
                          ["mfo1", ("mSO", c0)], [("mTt", 4 + h, c0)])


DECAY_K = -math.exp(-0.5)


def col_vec(ap2d):
    return ap2d.rearrange("n (c p) -> p n c", p=128)


def rwkv_mixer(k, i, cm):
    P, I = k.P, k.I
    j = i // 2
    Wr = I["rec_w_in"][j]
    TCS = [(0, 512), (512, 512), (1024, 512), (1536, 512), (2048, 256)]
    with P.phase() as sr:
        L1 = P.sbuf("rL1", [128, T], BF16, sr)
        L2 = P.sbuf("rL2", [128, T], BF16, sr)
        L3 = P.sbuf("rL3", [128, T], BF16, sr)
        with P.phase() as st:
            W = P.sbuf("rW", [128, 8, 2048], BF16, st)
            for kk in range(8):
                k.dma("pool", W[:, kk, :], Wr[kk * 128:(kk + 1) * 128, 0:2048], [], [("rW", kk)])
            hd = P.sbuf("rhd", [128, 8, T], BF16, st)
            tmp = [P.sbuf(f"rhtmp{r}", [128, S], BF16, st) for r in range(2)]
            muT = P.sbuf("rmuT", [128, 6, 4], F32, st)
            k.dma("sp", muT[:], col_vec(I["rwkv_mu"][j]), [], ["muT"], allow_slow_non_contiguous=True)
            w1b = P.sbuf("rw1b", [128, 2, 4, 32], BF16, st)
            a1b = P.sbuf("ra1b", [128, 2, 4, 32], BF16, st)
            g1b = P.sbuf("rg1b", [128, 4, 96], BF16, st)
            for d in range(2):
                k.dma("pool", w1b[:, d], I["rwkv_w1"][j, d].rearrange("(c p) r -> p c r", p=128), [], [("w1b", d)])
                k.dma("pool", a1b[:, d], I["rwkv_a1"][j, d].rearrange("(c p) r -> p c r", p=128), [], [("a1b", d)])
            k.dma("pool", g1b[:], I["rwkv_g1"][j].rearrange("(c p) r -> p c r", p=128), [], ["g1b"])
            n = 0
            for kk in range(8):
                for (a, b) in ((0, S), (S, T)):
                    r = n % 2
                    e1 = "pool" if n % 2 else "dve"
                    n += 1
                    nn = b - a
                    k.tt(e1, tmp[r][:, 0:nn - 2], k.AB[:, kk, a:b - 2], k.AB[:, kk, a + 2:b], ALU.add, [], [("rhtmp", r)])
                    k.stt("dve", hd[:, kk, a + 1:b - 1], tmp[r][:, 0:nn - 2], 0.5, k.AB[:, kk, a + 1:b - 1], ALU.mult, ALU.subtract, [("rhtmp", r)], [("hd", kk, a, 0)])
                    k.stt("dve", hd[:, kk, a:a + 1], k.AB[:, kk, a + 1:a + 2], 0.5, k.AB[:, kk, a:a + 1], ALU.mult, ALU.subtract, [], [("hd", kk, a, 1)])
                    k.stt("dve", hd[:, kk, b - 1:b], k.AB[:, kk, b - 2:b - 1], 0.5, k.AB[:, kk, b - 1:b], ALU.mult, ALU.subtract, [], [("hd", kk, a, 2)])
            P.flush(barrier=True)
            ps = [P.sbuf(f"rps{r}", [128, 512], F32, st) for r in range(2)]
            zt = {nm: [P.sbuf(f"rz{nm}{r}", [128, 512], BF16, st) for r in range(2)] for nm in ("w", "a", "g")}
            o32 = [P.sbuf(f"ro32{r}", [128, 512], F32, st) for r in range(2)]
            n = 0
            for (c0, cn) in TCS:
                for c in range(4):
                    r = n % 2
                    n += 1
                    for kk in range(8):
                        k.mm(k.bank(0)[:, 0:cn], W[:, kk, 1536 + c * 128:1536 + (c + 1) * 128], k.AB[:, kk, c0:c0 + cn], kk == 0, kk == 7, [("rW", kk)], [k.bk(0)])
                    for kk in range(8):
                        k.mm(k.bank(1)[:, 0:cn], W[:, kk, 1536 + c * 128:1536 + (c + 1) * 128], hd[:, kk, c0:c0 + cn], kk == 0, kk == 7, [("rW", kk)], [k.bk(1)])
                    k.cp("act", ps[r][:, 0:cn], k.bank(0)[:, 0:cn], [k.bk(0)], [("rps", r)])
                    for (nm, m) in (("w", 3), ("a", 4), ("g", 5)):
                        k.stt("dve", zt[nm][r][:, 0:cn], k.bank(1)[:, 0:cn], muT[:, m, c:c + 1], ps[r][:, 0:cn], ALU.mult, ALU.add,
                              [k.bk(1), ("rps", r), "muT"], [("rz", nm, r)])
                    f, l = (c == 0), (c == 3)
                    k.mm(k.bank(2)[0:32, 0:cn], w1b[:, 0, c, :], zt["w"][r][:, 0:cn], f, l, [("rz", "w", r), ("w1b", 0)], [k.bk(2)])
                    k.mm(k.bank(3)[32:64, 0:cn], w1b[:, 1, c, :], zt["w"][r][:, 0:cn], f, l, [("rz", "w", r), ("w1b", 1)], [k.bk(3)])
                    k.mm(k.bank(4)[64:96, 0:cn], a1b[:, 0, c, :], zt["a"][r][:, 0:cn], f, l, [("rz", "a", r), ("a1b", 0)], [k.bk(4)])
                    k.mm(k.bank(5)[0:32, 0:cn], a1b[:, 1, c, :], zt["a"][r][:, 0:cn], f, l, [("rz", "a", r), ("a1b", 1)], [k.bk(5)])
                    k.mm(k.bank(6)[0:96, 0:cn], g1b[:, c, :], zt["g"][r][:, 0:cn], f, l, [("rz", "g", r), "g1b"], [k.bk(6)])
                k.act(L1[0:32, c0:c0 + cn], k.bank(2)[0:32, 0:cn], AF.Tanh, [k.bk(2)], [("L1", 0, c0)])
                k.act(L1[32:64, c0:c0 + cn], k.bank(3)[32:64, 0:cn], AF.Tanh, [k.bk(3)], [("L1", 1, c0)])
                k.cp("act", L1[64:96, c0:c0 + cn], k.bank(4)[64:96, 0:cn], [k.bk(4)], [("L1", 2, c0)])
                k.cp("act", L3[0:32, c0:c0 + cn], k.bank(5)[0:32, 0:cn], [k.bk(5)], [("L3", c0)])
                k.act(L2[0:96, c0:c0 + cn], k.bank(6)[0:96, 0:cn], AF.Sigmoid, [k.bk(6)], [("L2", c0)])
            for g in range(3):
                for (c0, cn) in TCS:
                    for c in range(4):
                        r = n % 2
                        n += 1
                        b0, b1 = (0, 1) if r == 0 else (2, 3)
                        for kk in range(8):
                            k.mm(k.bank(b0)[:, 0:cn], W[:, kk, g * 512 + c * 128:g * 512 + (c + 1) * 128], k.AB[:, kk, c0:c0 + cn], kk == 0, kk == 7, [("rW", kk)], [k.bk(b0)])
                        for kk in range(8):
                            k.mm(k.bank(b1)[:, 0:cn], W[:, kk, g * 512 + c * 128:g * 512 + (c + 1) * 128], hd[:, kk, c0:c0 + cn], kk == 0, kk == 7, [("rW", kk)], [k.bk(b1)])
                        k.cp("act", ps[r][:, 0:cn], k.bank(b0)[:, 0:cn], [k.bk(b0)], [("rps", r)])
                        k.stt("dve", o32[r][:, 0:cn], k.bank(b1)[:, 0:cn], muT[:, g, c:c + 1], ps[r][:, 0:cn], ALU.mult, ALU.add,
                              [k.bk(b1), ("rps", r), "muT"], [("ro32", r)])
                        k.dma("sp", k.rkv_d[g, c * 128:(c + 1) * 128, c0:c0 + cn], o32[r][:, 0:cn], [("ro32", r)], [("rkv_d", g, c, c0)])
        for hp in range(4):
            rwkv_stage_b(k, i, hp, cm, L1, L2, L3)


def rwkv_stage_b(k, i, hp, cm, L1, L2, L3):
    P, I = k.P, k.I
    j = i // 2
    TCS = [(0, 512), (512, 512), (1024, 512), (1536, 512), (2048, 256)]
    BN = 512
    cs_ = slice(hp * 128, (hp + 1) * 128)
    with P.phase() as st:
        pv = {}
        for nm, src, nrow in (("w0", "rwkv_w0", 2), ("a0", "rwkv_a0", 2), ("kvec", "rwkv_kvec", 2), ("gn", "rwkv_gn", 2)):
            pv[nm] = P.sbuf("rp_" + nm, [128, nrow], F32, st)
            k.dma("sp", pv[nm][:], I[src][j][:, cs_].rearrange("n p -> p n"), [], [("rp", nm)], allow_slow_non_contiguous=True)
        pv["rk"] = P.sbuf("rp_rk", [128, 1], F32, st)
        k.dma("sp", pv["rk"][:], I["rwkv_rk"][j, cs_].rearrange("(p o) -> p o", o=1), [], [("rp", "rk")], allow_slow_non_contiguous=True)
        pv["omk"] = P.sbuf("rp_omk", [128, 1], F32, st)
        k.ts("dve", pv["omk"][:], pv["kvec"][:, 1:2], -1.0, 1.0, ALU.mult, ALU.add, [("rp", "kvec")], [("rp", "omk")])
        pv["gne"] = P.sbuf("rp_gne", [128, 1], F32, st)
        P.add("pool", lambda e: e.memset(pv["gne"][:], RWKV_GN_EPS), [], [("rp", "gne")])
        w2b = P.sbuf("rw2b", [128, 128], BF16, st)
        a2b1 = P.sbuf("ra2b1", [32, 128], BF16, st)
        g2b = P.sbuf("rg2b", [96, 128], BF16, st)
        k.dma("pool", w2b[0:32, :], I["rwkv_w2"][j, 0][:, cs_], [], [("w2b", 0)])
        k.dma("pool", w2b[32:64, :], I["rwkv_w2"][j, 1][:, cs_], [], [("w2b", 1)])
        k.dma("pool", w2b[64:96, :], I["rwkv_a2"][j, 0][:, cs_], [], [("w2b", 2)])
        k.dma("pool", a2b1[:], I["rwkv_a2"][j, 1][:, cs_], [], ["a2b1"])
        k.dma("pool", g2b[:], I["rwkv_g2"][j][:, cs_], [], ["g2b"])
        R = P.sbuf("rR", [128, T], BF16, st)
        Kx = P.sbuf("rK", [128, T], BF16, st)
        V = P.sbuf("rV", [128, T], BF16, st)
        k.dma("pool", R[:], k.rkv_d[0, cs_, :], [], ["rR"])
        k.dma("pool", Kx[:], k.rkv_d[1, cs_, :], [], ["rK"])
        k.dma("pool", V[:], k.rkv_d[2, cs_, :], [], ["rV"])
        KK = P.sbuf("rKK", [128, T], F32, st)
        GA = P.sbuf("rGA", [128, T], BF16, st)
        LWr = [P.sbuf(f"rlw{d}", [128, T], F32, st) for d in range(2)]
        Aa = [P.sbuf(f"raa{d}", [128, T], BF16, st) for d in range(2)]
        YS = P.sbuf("rYS", [128, T], F32, st)
        ft = {nm: P.sbuf("rf_" + nm, [128, 512], F32, st) for nm in ("a", "b", "c")}
        brr = [0]

        def nb():
            brr[0] += 1
            return brr[0] % 8

        for (c0, cn) in TCS:
            sl = slice(c0, c0 + cn)
            k.act(ft["a"][:, 0:cn], Kx[:, sl], AF.Square, ["rK", ("rp", "kvec")], ["rfa"], scale=pv["kvec"][:, 0:1])
            b = nb()
            k.mm(k.bank(b)[:, 0:cn], cm["blk64"][:], ft["a"][:, 0:cn], True, True, ["rfa"], [k.bk(b)])
            k.ts("dve", ft["b"][:, 0:cn], k.bank(b)[:, 0:cn], 1e-24, None, ALU.max, ALU.bypass, [k.bk(b)], ["rfb"])
            k.act(ft["b"][:, 0:cn], ft["b"][:, 0:cn], AF.Sqrt, ["rfb"], ["rfb"])
            P.add("dve", lambda e, o=ft["b"][:, 0:cn]: e.reciprocal(out=o, in_=o), ["rfb"], ["rfb"])
            k.stt("dve", KK[:, sl], Kx[:, sl], pv["kvec"][:, 0:1], ft["b"][:, 0:cn], ALU.mult, ALU.mult, ["rK", "rfb", ("rp", "kvec")], [("rKK", c0)])
            b = nb()
            k.mm(k.bank(b)[:, 0:cn], g2b[:], L2[0:96, sl], True, True, ["g2b"], [k.bk(b)])
            k.cp("act", GA[:, sl], k.bank(b)[:, 0:cn], [k.bk(b)], [("rGA", c0)])
            for d in range(2):
                b = nb()
                k.mm(k.bank(b)[:, 0:cn], w2b[32 * d:32 * d + 32, :], L1[32 * d:32 * d + 32, sl], True, True, [("w2b", d)], [k.bk(b)])
                k.act(ft["c"][:, 0:cn], k.bank(b)[:, 0:cn], AF.Sigmoid, [k.bk(b), ("rp", "w0")], ["rfc"], bias=pv["w0"][:, d:d + 1], scale=1.0)
                k.ts("dve", LWr[d][:, sl], ft["c"][:, 0:cn], float(DECAY_K), None, ALU.mult, ALU.bypass, ["rfc"], [("rlw", d, c0)])
                b = nb()
                if d == 0:
                    k.mm(k.bank(b)[:, 0:cn], w2b[64:96, :], L1[64:96, sl], True, True, [("w2b", 2)], [k.bk(b)])
                else:
                    k.mm(k.bank(b)[:, 0:cn], a2b1[:], L3[0:32, sl], True, True, ["a2b1"], [k.bk(b)])
                k.act(Aa[d][:, sl], k.bank(b)[:, 0:cn], AF.Sigmoid, [k.bk(b), ("rp", "a0")], [("raa", d, c0)], bias=pv["a0"][:, d:d + 1], scale=1.0)
        P.flush(barrier=True)
        rst = P.sbuf("rrst", [128, BN], BF16, st)
        P.add("pool", lambda e: e.memset(rst[:], 1.0), [], ["rst"])
        P.add("pool", lambda e: e.memset(bass.AP(rst, 0, [[BN, 128], [64, BN // 64]]), 0.0), ["rst"], ["rst"])
        bl = {nm: P.sbuf("rb_" + nm, [128, BN], F32, st) for nm in ("LW", "E", "kka", "ke", "AH", "KH", "AW", "KW", "YT", "t", "Vb")}
        BR = P.sbuf("rBR", [128, BN // 128, 2, 128], F32, st)
        Hs = P.sbuf("rH", [128, 64], F32, st)
        tl = {nm: [P.sbuf(f"rt_{nm}{r}", [128, 128], F32 if nm in ("R1T", "Y0T") else BF16, st) for r in range(4)]
              for nm in ("Mm", "nAT", "Bm", "Btm", "MT", "Pv", "Q", "QT", "G", "nG2", "R1T", "Y0T")}
        TMs = [P.sbuf(f"rTM{r}", [128, 4, 128], BF16, st) for r in range(2)]
        FTs = [P.sbuf(f"rFT{r}", [128, 2, 64], F32, st) for r in range(4)]
        Zs = [P.sbuf(f"rZs{r}", [128, 2, 64], F32, st) for r in range(4)]
        for d in range(2):
            if d == 0:
                blocks = [(S, 1, NCTX), (0, 1, 512), (512, 1, 512), (1024, 1, 512), (1536, 1, 512)]
            else:
                blocks = [(T - 1, -1, NCTX), (S - 1, -1, 512), (S - 513, -1, 512), (S - 1025, -1, 512), (S - 1537, -1, 512)]
            P.add("dve", lambda e: e.memset(Hs[:], 0.0), [], [("rH", 0), ("rH", 1)])
            for (bs, step, bn) in blocks:
                Vw = lambda arr, bs=bs, step=step, bn=bn: tokview(arr, T, 0, bs, step, bn)
                B_ = lambda nm, bn=bn: bl[nm][:, 0:bn]
                nch = bn // 64
                P.add("dve", lambda e, o=B_("LW"), a=rst[:, 0:bn], b=Vw(LWr[d]): e.tensor_tensor_scan(
                    out=o, data0=a, data1=b, initial=0.0, op0=ALU.mult, op1=ALU.add), ["rst"], ["bLW"])
                lwend = bass.AP(bl["LW"], 63, [[BN, 128], [64, nch], [0, 64]])
                lw3 = bl["LW"][:, 0:bn].rearrange("p (c t) -> p c t", t=64)
                a_v, kk_v, k_v, r_v = Vw(Aa[d]), Vw(KK), Vw(Kx), Vw(R)
                k.cp("pool", B_("Vb"), Vw(V), [], ["bVb"])
                k.tt("pool", B_("kka"), kk_v, a_v, ALU.mult, [], ["bkka"])
                k.ts("dve", B_("t"), a_v, pv["kvec"][:, 1:2], pv["omk"][:, 0:1], ALU.mult, ALU.add, [("rp", "omk")], ["bt"])
                k.tt("pool", B_("ke"), B_("t"), k_v, ALU.mult, ["bt"], ["bke"])
                k.act(B_("E"), B_("LW"), AF.Exp, ["bLW"], ["bE"], scale=-1.0)
                k.tt("dve", B_("AH"), B_("kka"), B_("E"), ALU.mult, ["bkka", "bE"], ["bAH"])
                k.tt("pool", B_("KH"), B_("ke"), B_("E"), ALU.mult, ["bke", "bE"], ["bKH"])
                k.tt("dve", B_("t").rearrange("p (c t) -> p c t", t=64), lwend, lw3, ALU.subtract, ["bLW", "bt"], ["bt"])
                k.act(B_("E"), B_("t"), AF.Exp, ["bt", "bE", "bAH", "bKH"], ["bE"])
                k.tt("dve", B_("AW"), B_("kka"), B_("E"), ALU.mult, ["bkka", "bE"], ["bAW"])
                k.tt("pool", B_("KW"), B_("ke"), B_("E"), ALU.mult, ["bke", "bE"], ["bKW"])
                k.tt("dve", B_("t"), B_("LW"), Vw(LWr[d]), ALU.subtract, ["bLW", "bt"], ["bt"])
                k.act(B_("E"), B_("t"), AF.Exp, ["bt", "bE", "bAW", "bKW"], ["bE"])
                ntile = bn // 128
                brv = lambda q, ntile=ntile: bass.AP(BR, q * 128, [[(BN // 128) * 256, 128], [256, ntile], [1, 128]])
                k.tt("dve", brv(0), bass.AP(kk_v.tensor, kk_v.offset, [[T, 128], [128 * step, ntile], [step, 128]]),
                     B_("E").rearrange("p (c t) -> p c t", t=128), ALU.mult, ["bE"], ["bBR0"])
                k.act(B_("E"), B_("LW"), AF.Exp, ["bLW", "bE", "bBR0"], ["bE"])
                k.tt("pool", brv(1), bass.AP(r_v.tensor, r_v.offset, [[T, 128], [128 * step, ntile], [step, 128]]),
                     B_("E").rearrange("p (c t) -> p c t", t=128), ALU.mult, ["bE"], ["bBR1"])
                k.act(bl["t"][:, 0:nch], bass.AP(bl["LW"], 63, [[BN, 128], [64, nch]]), AF.Exp, ["bLW", "bt", "bE"], ["bWC", "bt"])
                for tg0 in range(0, ntile, 2):
                    tis = [ti for ti in (tg0, tg0 + 1) if ti < ntile]
                    chains = [(ti, hh) for ti in tis for hh in range(2)]
                    for ti in tis:
                        tsl = slice(ti * 128, (ti + 1) * 128)
                        r = ti % 2
                        bT = nb()
                        srcs = [BR[:, ti, 0, :], bl["Vb"][:, tsl], bl["AW"][:, tsl], bl["KW"][:, tsl]]
                        for q, s_ in enumerate(srcs):
                            k.tr(k.bank(bT)[:, q * 128:(q + 1) * 128], s_, k.identf[:], ["bBR0", "bAW", "bKW", "bVb"], [k.bk(bT)])
                        k.cp("act", TMs[r][:], k.bank(bT)[:, 0:512].rearrange("p (q n) -> p q n", q=4), [k.bk(bT)], [("rTM", r)])
                    CH = []
                    for x, (ti, hh) in enumerate(chains):
                        CH.append(dict(x=x, ti=ti, hh=hh, r=ti % 2, ps=slice(hh * 64, (hh + 1) * 64), tsl=slice(ti * 128, (ti + 1) * 128),
                                       L={nm: tl[nm][x] for nm in tl}, lk=(lambda nm, x=x: ("rtl", nm, x))))
                    for c_ in CH:
                        L, lk, ps_, tsl, ti = c_["L"], c_["lk"], c_["ps"], c_["tsl"], c_["ti"]
                        bX = nb()
                        k.mm(k.bank(bX)[:, 0:256], bl["AH"][ps_, tsl], BR[ps_, ti, :, :], True, True, ["bAH", "bBR0", "bBR1"], [k.bk(bX)])
                        k.mm(k.bank(bX)[:, 256:512], bl["KH"][ps_, tsl], BR[ps_, ti, :, :], True, True, ["bKH", "bBR0", "bBR1"], [k.bk(bX)])
                        k.tt("dve", L["Mm"][:], k.bank(bX)[:, 0:128], cm["m_lt"][:], ALU.mult, [k.bk(bX)], [lk("Mm")])
                        k.stt("dve", L["nAT"][:], k.bank(bX)[:, 128:256], -1.0, cm["m_le"][:], ALU.mult, ALU.mult, [k.bk(bX)], [lk("nAT")])
                        k.tt("dve", L["Bm"][:], k.bank(bX)[:, 256:384], cm["m_lt"][:], ALU.mult, [k.bk(bX)], [lk("Bm")])
                        k.tt("dve", L["Btm"][:], k.bank(bX)[:, 384:512], cm["m_le"][:], ALU.mult, [k.bk(bX)], [lk("Btm")])
                    for c_ in CH:
                        L, lk = c_["L"], c_["lk"]
                        bY = nb()
                        pyb = k.bank(bY).bitcast(BF16)
                        k.tr(pyb[:, 0:128], L["Mm"][:], k.identb[:], [lk("Mm")], [k.bk(bY)])
                        k.cp("act", L["MT"][:], pyb[:, 0:128], [k.bk(bY)], [lk("MT")])
                        k.tt("pool", L["Pv"][:], k.identf[:], L["Mm"][:], ALU.subtract, [lk("Mm")], [lk("Pv")])
                        c_["Q"], c_["QT"], c_["qk"], c_["qtk"] = L["Mm"], L["MT"], lk("Mm"), lk("MT")
                    for lvl in range(1, 6):
                        for c_ in CH:
                            L, lk = c_["L"], c_["lk"]
                            bq = nb()
                            c_["bq"] = bq
                            if lvl < 5:
                                k.mm(k.bank(bq)[:, 0:128], c_["QT"][:], c_["Q"][:], True, True, [c_["qk"], c_["qtk"]], [k.bk(bq)])
                            k.mm(k.bank(bq)[:, 128:256], c_["Q"][:], c_["QT"][:], True, True, [c_["qk"], c_["qtk"]], [k.bk(bq)])
                        for c_ in CH:
                            L, lk, bq = c_["L"], c_["lk"], c_["bq"]
                            if lvl < 5:
                                k.cp("act", L["Q"][:], k.bank(bq)[:, 0:128], [k.bk(bq)], [lk("Q")])
                            k.cp("act", L["QT"][:], k.bank(bq)[:, 128:256], [k.bk(bq)], [lk("QT")])
                            c_["Q"], c_["QT"], c_["qk"], c_["qtk"] = L["Q"], L["QT"], lk("Q"), lk("QT")
                        for c_ in CH:
                            L, lk = c_["L"], c_["lk"]
                            bp = nb()
                            c_["bp"] = bp
                            k.mm(k.bank(bp)[:, 0:128], c_["QT"][:], L["Pv"][:], True, True, [c_["qtk"], lk("Pv")], [k.bk(bp)])
                        for c_ in CH:
                            L, lk, bp = c_["L"], c_["lk"], c_["bp"]
                            k.tt("dve", L["Pv"][:], L["Pv"][:], k.bank(bp)[:, 0:128], ALU.add, [k.bk(bp), lk("Pv")], [lk("Pv")])
                    for c_ in CH:
                        L, lk, ps_, r = c_["L"], c_["lk"], c_["ps"], c_["r"]
                        bb = nb()
                        k.mm(k.bank(bb)[:, 0:64], L["Bm"][:], TMs[r][:, 1, ps_], True, True, [lk("Bm"), ("rTM", r)], [k.bk(bb)])
                        k.cp("act", L["Mm"][:, 64:128], k.bank(bb)[:, 0:64], [k.bk(bb)], [lk("Mm")])
                        k.cp("pool", L["Mm"][:, 0:64], TMs[r][:, 0, ps_], [("rTM", r)], [lk("Mm")])
                    for c_ in CH:
                        L, lk = c_["L"], c_["lk"]
                        bg = nb()
                        k.mm(k.bank(bg)[:, 0:128], L["Pv"][:], L["Mm"][:], True, True, [lk("Pv"), lk("Mm")], [k.bk(bg)])
                        k.cp("act", L["G"][:], k.bank(bg)[:, 0:128], [k.bk(bg)], [lk("G")])
                        k.ts("pool", L["nG2"][:, 0:64], L["G"][:, 64:128], -1.0, None, ALU.mult, ALU.bypass, [lk("G")], [lk("nG2")])
                    for c_ in CH:
                        L, lk, ps_, r, ti = c_["L"], c_["lk"], c_["ps"], c_["r"], c_["ti"]
                        b1 = nb()
                        k.mm(k.bank(b1)[ps_, 0:128], L["G"][:, 0:64], L["nAT"][:], True, True, [lk("G"), lk("nAT")], [k.bk(b1)])
                        k.tt("dve", L["R1T"][ps_, :], k.bank(b1)[ps_, 0:128], BR[ps_, ti, 1, :], ALU.add, [k.bk(b1), "bBR1"], [lk("R1T")])
                        b2 = nb()
                        k.mm(k.bank(b2)[ps_, 0:128], TMs[r][:, 1, ps_], L["Btm"][:], True, False, [("rTM", r), lk("Btm")], [k.bk(b2)])
                        k.mm(k.bank(b2)[ps_, 0:128], L["G"][:, 64:128], L["nAT"][:], False, True, [lk("G"), lk("nAT")], [k.bk(b2)])
                        k.cp("act", L["Y0T"][ps_, :], k.bank(b2)[ps_, 0:128], [k.bk(b2)], [lk("Y0T")])
                    for c in range(2):
                        rows = slice(c * 64, (c + 1) * 64)
                        for c_ in CH:
                            L, lk, ps_, r, ti, hh, x = c_["L"], c_["lk"], c_["ps"], c_["r"], c_["ti"], c_["hh"], c_["x"]
                            bf_ = nb()
                            k.mm(k.bank(bf_)[ps_, 0:64], L["G"][rows, 0:64], TMs[r][rows, 2, ps_], True, True, [lk("G"), ("rTM", r)], [k.bk(bf_)])
                            wc = bl["t"][ps_, ti * 2 + c:ti * 2 + c + 1]
                            k.stt("dve", FTs[x][ps_, c, :], k.identf[ps_, hh * 64:(hh + 1) * 64], wc, k.bank(bf_)[ps_, 0:64], ALU.mult, ALU.subtract,
                                  [k.bk(bf_), "bWC"], [("rFT", x, c)])
                            bz = nb()
                            k.mm(k.bank(bz)[ps_, 0:64], TMs[r][rows, 3, ps_], TMs[r][rows, 1, ps_], True, False, [("rTM", r)], [k.bk(bz)])
                            k.mm(k.bank(bz)[ps_, 0:64], TMs[r][rows, 2, ps_], L["nG2"][rows, 0:64], False, True, [("rTM", r), lk("nG2")], [k.bk(bz)])
                            k.cp("act", Zs[x][ps_, c, :], k.bank(bz)[ps_, 0:64], [k.bk(bz)], [("rZs", x, c)])
                    for ti in tis:
                        for c in range(2):
                            cc = slice(c * 64, (c + 1) * 64)
                            for c_ in CH:
                                if c_["ti"] != ti:
                                    continue
                                L, lk, ps_, hh, x = c_["L"], c_["lk"], c_["ps"], c_["hh"], c_["x"]
                                by = nb()
                                k.mm(k.bank(by)[ps_, 0:64], Hs[ps_, :], L["R1T"][ps_, cc], True, True, [("rH", hh), lk("R1T")], [k.bk(by)])
                                k.tt("dve", bl["YT"][ps_, ti * 128 + c * 64:ti * 128 + (c + 1) * 64], k.bank(by)[ps_, 0:64], L["Y0T"][ps_, cc], ALU.add,
                                     [k.bk(by), lk("Y0T")], [("bYT", hh, ti, c)])
                                bh = nb()
                                k.mm(k.bank(bh)[ps_, 0:64], FTs[x][ps_, c, :], Hs[ps_, :], True, True, [("rH", hh), ("rFT", x, c)], [k.bk(bh)])
                                k.tt("dve", Hs[ps_, :], k.bank(bh)[ps_, 0:64], Zs[x][ps_, c, :], ALU.add, [k.bk(bh), ("rZs", x, c), ("rH", hh)], [("rH", hh)])
                yk = [("bYT", hh, ti, c) for hh in range(2) for ti in range(ntile) for c in range(2)]
                ysv = Vw(YS)
                if d == 0:
                    k.cp("pool", ysv, B_("YT"), yk, [("rYS", bs)])
                else:
                    k.tt("pool", ysv, ysv, B_("YT"), ALU.add, yk, [("rYS", bs)])
                P.flush(barrier=True)
        for (c0, cn) in TCS:
            sl = slice(c0, c0 + cn)
            fa, fb, fc = ft["a"][:, 0:cn], ft["b"][:, 0:cn], ft["c"][:, 0:cn]
            b = nb()
            k.mm(k.bank(b)[:, 0:cn], cm["blk64"][:], YS[:, sl], True, True, [], [k.bk(b)])
            k.stt("dve", fa, k.bank(b)[:, 0:cn], -1.0 / 64, YS[:, sl], ALU.mult, ALU.add, [k.bk(b)], ["ffa"])
            k.act(fb, fa, AF.Square, ["ffa"], ["ffb"])
            b = nb()
            k.mm(k.bank(b)[:, 0:cn], cm["blk64"][:], fb, True, True, ["ffb"], [k.bk(b)])
            k.act(fb, k.bank(b)[:, 0:cn], AF.Sqrt, [k.bk(b), ("rp", "gne")], ["ffb"], scale=1.0 / 64, bias=pv["gne"][:, 0:1])
            P.add("dve", lambda e, o=fb: e.reciprocal(out=o, in_=o), ["ffb"], ["ffb"])
            k.tt("pool", fa, fa, fb, ALU.mult, ["ffa", "ffb"], ["ffa"])
            k.ts("dve", fa, fa, pv["gn"][:, 0:1], pv["gn"][:, 1:2], ALU.mult, ALU.add, ["ffa", ("rp", "gn")], ["ffa"])
            k.tt("pool", fc, Aa[0][:, sl], Aa[1][:, sl], ALU.add, [], ["ffc"])
            k.ts("dve", fc, fc, pv["kvec"][:, 1:2], pv["omk"][:, 0:1], ALU.mult, ALU.add, ["ffc"], ["ffc"])
            k.stt("dve", fc, fc, pv["omk"][:, 0:1], Kx[:, sl], ALU.add, ALU.mult, ["ffc"], ["ffc"])
            k.stt("dve", fc, R[:, sl], pv["rk"][:, 0:1], fc, ALU.mult, ALU.mult, ["ffc", ("rp", "rk")], ["ffc"])
            b = nb()
            k.mm(k.bank(b)[:, 0:cn], cm["blk64"][:], fc, True, True, ["ffc"], [k.bk(b)])
            k.tt("dve", fb, k.bank(b)[:, 0:cn], V[:, sl], ALU.mult, [k.bk(b), "ffb"], ["ffb"])
            k.tt("pool", fa, fa, fb, ALU.add, ["ffa", "ffb"], ["ffa"])
            k.tt("dve", k.AB[:, hp, sl], fa, GA[:, sl], ALU.mult, ["ffa"], [("ABo", hp, c0)])
```
